# Optimizing a Trainium2 kernel written in Bass

```python
import math
import jax
import jax.numpy as jnp
from jax import lax
import numpy as np

D_MODEL = 1024
BATCH = 2
SEQ = 8192
DEPTH = 2

GRID_W = 64
CTX_LEN = 256
EPS = 1e-6
NEG_INF = -1e30
ROPE_THETA = 10000.0
Q_BLOCK = 128

DA_HEADS = 4
DA_HEAD_DIM = 64
DA_QK_WIDTH = DA_HEADS * 2 * DA_HEAD_DIM
DA_V_WIDTH = DA_HEADS * 2 * DA_HEAD_DIM
ROPE_AXIS = DA_HEAD_DIM // 2
ROPE_HALF = ROPE_AXIS // 2

SG_CHUNK = 128
SG_GROUPS = 4
SG_WIDTH = 512
SG_GROUP_DIM = SG_WIDTH // SG_GROUPS

NA_HEADS = 8
NA_HEAD_DIM = 64
NA_WIDTH = NA_HEADS * NA_HEAD_DIM
NA_ROWS_MAX = 8
NA_COLS = 16

N_BRANCH = 3
BRANCH_WIDTH = 512

OFF_KA = 0
OFF_VA = OFF_KA + DA_QK_WIDTH
OFF_KC = OFF_VA + DA_V_WIDTH
OFF_VC = OFF_KC + NA_WIDTH
KV_COLS = OFF_VC + NA_WIDTH
OFF_QA = KV_COLS
OFF_QC = OFF_QA + DA_QK_WIDTH
OFF_ZB = OFF_QC + NA_WIDTH
OFF_GATE = OFF_ZB + 2 * SG_WIDTH
IN_COLS = OFF_GATE + N_BRANCH * D_MODEL

N_GROUPS = 4
EXPERTS_PER_GROUP = 8
N_EXPERTS = N_GROUPS * EXPERTS_PER_GROUP
TOP_K = 2
D_EXPERT = 512
MOE_BLOCK = 128

kernel_name = 'hybrid_diffattn_gmlp_natten_hmoe_dit'


def rms_norm(x, g):
    xf = x.astype(jnp.float32)
    y = xf * lax.rsqrt(jnp.mean(xf * xf, axis=-1, keepdims=True) + EPS)
    return (y * g.astype(jnp.float32)).astype(x.dtype)


def layer_norm(x, g, b):
    xf = x.astype(jnp.float32)
    mu = jnp.mean(xf, axis=-1, keepdims=True)
    var = jnp.mean(jnp.square(xf - mu), axis=-1, keepdims=True)
    y = (xf - mu) * lax.rsqrt(var + EPS)
    return (y * g.astype(jnp.float32) + b.astype(jnp.float32)).astype(x.dtype)


def ada_modulation(cond, w, b):
    m = cond @ w + b
    return jnp.split(m[:, None, :], 6, axis=-1)


def modulate(xn, shift, scale):
    return xn * (1.0 + scale) + shift


def da_qk_heads(t):
    return t.reshape(*t.shape[:-1], DA_HEADS, 2, DA_HEAD_DIM)


def da_v_heads(t):
    return t.reshape(*t.shape[:-1], DA_HEADS, 2 * DA_HEAD_DIM)


def na_heads(t):
    return t.reshape(*t.shape[:-1], NA_HEADS, NA_HEAD_DIM)


def axial_rope_tables(n_tok):
    t = jnp.arange(n_tok, dtype=jnp.int32)
    row = (t // GRID_W).astype(jnp.float32)
    col = (t % GRID_W).astype(jnp.float32)
    inv = ROPE_THETA ** (-jnp.arange(ROPE_HALF, dtype=jnp.float32) / ROPE_HALF)
    ar = row[:, None] * inv
    ac = col[:, None] * inv
    ang = jnp.concatenate([ar, ar, ac, ac], axis=-1)
    return jnp.cos(ang), jnp.sin(ang)


def apply_axial_rope(x, cos, sin):
    x1, x2, x3, x4 = jnp.split(x, 4, axis=-1)
    rot = jnp.concatenate([-x2, x1, -x4, x3], axis=-1)
    cos = cos[None, :, None, None, :].astype(x.dtype)
    sin = sin[None, :, None, None, :].astype(x.dtype)
    return x * cos + rot * sin


def diff_softmax_mix(q, k, v, lam):
    s = jnp.einsum('bqhmd,bkhmd->bhmqk', q, k).astype(jnp.float32) * (DA_HEAD_DIM ** -0.5)
    p = jax.nn.softmax(s, axis=-1)
    w = (p[:, :, 0] - lam * p[:, :, 1]).astype(v.dtype)
    return jnp.einsum('bhqk,bkhe->bqhe', w, v)


def diff_attention_latent(q, k, v, k_ctx, v_ctx, lam):
    b, n = q.shape[:2]
    k_all = jnp.concatenate([k_ctx, k], axis=1)
    v_all = jnp.concatenate([v_ctx, v], axis=1)
    qb = q.reshape(b, n // Q_BLOCK, Q_BLOCK, DA_HEADS, 2, DA_HEAD_DIM).swapaxes(0, 1)
    o = lax.map(lambda qq: diff_softmax_mix(qq, k_all, v_all, lam), qb)
    return o.swapaxes(0, 1).reshape(b, n, DA_HEADS, 2 * DA_HEAD_DIM)


def diff_post(o, g, lam_init):
    y = rms_norm(o, g) * (1.0 - lam_init)
    return y.reshape(*o.shape[:2], DA_V_WIDTH)


def spatial_gating(z, ln_g, ln_b, w_s, b_s):
    z = jax.nn.gelu(z)
    u, vv = jnp.split(z, 2, axis=-1)
    vv = layer_norm(vv, ln_g, ln_b)
    b, n, _ = vv.shape
    vv = vv.reshape(b, n // SG_CHUNK, SG_CHUNK, SG_GROUPS, SG_GROUP_DIM)
    s = jnp.einsum('gpq,bnqgc->bnpgc', w_s, vv) + b_s.T[None, None, :, :, None]
    return u * s.reshape(b, n, SG_WIDTH)


def neighbourhood_attention(q, k, v, k_ctx, v_ctx, rpb, rows):
    b, n, nh, dh = q.shape
    kr = min(NA_ROWS_MAX, rows)
    n_cb = GRID_W // NA_COLS
    half = NA_COLS // 2
    band_w = 2 * NA_COLS
    scale = dh ** -0.5

    def grid_padded(t):
        t = t.reshape(b, rows, GRID_W, nh, dh)
        return jnp.pad(t, ((0, 0), (0, 0), (half, half), (0, 0), (0, 0)))

    kg, vg = grid_padded(k), grid_padded(v)
    qcol = np.arange(GRID_W).reshape(n_cb, NA_COLS)
    kcol = np.arange(n_cb)[:, None] * NA_COLS - half + np.arange(band_w)[None, :]
    cstart = np.clip(qcol - half, 0, GRID_W - NA_COLS)
    col_ok = jnp.asarray((kcol[:, None, :] >= cstart[..., None]) & (kcol[:, None, :] < cstart[..., None] + NA_COLS))
    dc_idx = np.clip(kcol[:, None, :] - qcol[:, :, None] + NA_COLS - 1, 0, 2 * NA_COLS - 2)
    rpb_c = rpb.astype(jnp.float32)[:, :, dc_idx]

    def row_step(args):
        r, qr = args
        rs = jnp.clip(r - kr // 2, 0, rows - kr)

        def band(t):
            t = lax.dynamic_slice_in_dim(t, rs, kr, axis=1).reshape(b, kr, n_cb + 1, NA_COLS, nh, dh)
            return jnp.concatenate([t[:, :, :-1], t[:, :, 1:]], axis=3)

        kb, vb = band(kg), band(vg)
        qb = qr.reshape(b, n_cb, NA_COLS, nh, dh)
        s_win = jnp.einsum('bjqhd,brjkhd->bhjqrk', qb, kb).astype(jnp.float32) * scale
        dr_idx = rs + jnp.arange(kr) - r + NA_ROWS_MAX - 1
        bias = jnp.take(rpb_c, dr_idx, axis=1).transpose(0, 2, 3, 1, 4)
        s_win = jnp.where(col_ok[:, :, None, :], s_win + bias[None], NEG_INF)
        s_ctx = jnp.einsum('bjqhd,bkhd->bhjqk', qb, k_ctx).astype(jnp.float32) * scale
        nw = kr * band_w
        s = jnp.concatenate([s_win.reshape(b, nh, n_cb, NA_COLS, nw), s_ctx], axis=-1)
        p = jax.nn.softmax(s, axis=-1).astype(v.dtype)
        p_win = p[..., :nw].reshape(b, nh, n_cb, NA_COLS, kr, band_w)
        o = (jnp.einsum('bhjqrk,brjkhd->bjqhd', p_win, vb)
             + jnp.einsum('bhjqk,bkhd->bjqhd', p[..., nw:], v_ctx))
        return o.reshape(b, GRID_W, nh, dh)

    q_rows = q.reshape(b, rows, GRID_W, nh, dh).swapaxes(0, 1)
    o = lax.map(row_step, (jnp.arange(rows, dtype=jnp.int32), q_rows))
    return o.swapaxes(0, 1).reshape(b, n, nh * dh)


def ctx_attention(q, k, v):
    s = jnp.einsum('bqhd,bkhd->bhqk', q, k).astype(jnp.float32) * (q.shape[-1] ** -0.5)
    p = jax.nn.softmax(s, axis=-1).astype(v.dtype)
    o = jnp.einsum('bhqk,bkhd->bqhd', p, v)
    return o.reshape(*o.shape[:2], -1)


def merge_branches(p_gate, y_a, y_b, y_c, w_branch, w_out):
    ys = jnp.stack([y_a, y_b, y_c], axis=2)
    yb = jnp.einsum('bnie,ied->bnid', ys, w_branch)
    g = jax.nn.sigmoid(p_gate.reshape(*p_gate.shape[:-1], N_BRANCH, D_MODEL))
    return jnp.sum(g * yb, axis=2) @ w_out


def hier_moe(x, w_group, b_group, w_router, b_router, w_gate, w_up, w_down):
    t = x.shape[0]
    g_logits = (x @ w_group).astype(jnp.float32) + b_group.astype(jnp.float32)
    g_prob = jax.nn.softmax(g_logits, axis=-1)
    g_idx = jnp.argmax(g_logits, axis=-1)
    g_w = jnp.take_along_axis(g_prob, g_idx[:, None], axis=1)
    e_logits = ((x @ w_router).astype(jnp.float32) + b_router.astype(jnp.float32)).reshape(t, N_GROUPS, EXPERTS_PER_GROUP)
    e_logits = e_logits[jnp.arange(t), g_idx]
    top_l, top_i = lax.top_k(e_logits, TOP_K)
    top_w = jax.nn.softmax(top_l, axis=-1) * g_w
    expert = (g_idx[:, None] * EXPERTS_PER_GROUP + top_i).astype(jnp.int32)

    a = t * TOP_K
    flat_e = expert.reshape(a)
    order = jnp.argsort(flat_e).astype(jnp.int32)
    sorted_e = flat_e[order]
    counts = jnp.bincount(flat_e, length=N_EXPERTS)
    padded = (counts + MOE_BLOCK - 1) // MOE_BLOCK * MOE_BLOCK
    pad_end = jnp.cumsum(padded)
    pad_start = pad_end - padded
    start = jnp.cumsum(counts) - counts
    dest = pad_start[sorted_e] + jnp.arange(a, dtype=jnp.int32) - start[sorted_e]
    n_blocks = -(-(a + N_EXPERTS * (MOE_BLOCK - 1)) // MOE_BLOCK)
    n_slots = n_blocks * MOE_BLOCK
    slot_token = jnp.full((n_slots,), t, jnp.int32).at[dest].set(order // TOP_K)
    x_pad = jnp.concatenate([x, jnp.zeros((1, x.shape[1]), x.dtype)], axis=0)
    xs = x_pad[slot_token].reshape(n_blocks, MOE_BLOCK, x.shape[1])
    block_expert = jnp.minimum(jnp.searchsorted(pad_end, jnp.arange(n_blocks) * MOE_BLOCK, side='right'), N_EXPERTS - 1)

    def expert_block(args):
        xb, e = args
        hdn = jax.nn.silu(xb @ w_gate[e]) * (xb @ w_up[e])
        return hdn @ w_down[e]

    ys = lax.map(expert_block, (xs, block_expert)).reshape(n_slots, -1)
    w_sorted = top_w.reshape(a)[order].astype(ys.dtype)
    return jax.ops.segment_sum(ys[dest] * w_sorted[:, None], order // TOP_K, num_segments=t)


def setup_inputs(seed: int = 0) -> dict:
    key = jax.random.key(seed)
    ks = jax.random.split(key, 32)
    f32 = jnp.float32
    D = D_MODEL

    def nrm(k, shape, scale):
        return jax.random.normal(k, shape, f32) * scale

    return {
        'x': nrm(ks[0], (BATCH, SEQ, D), 1.0),
        'c': nrm(ks[1], (BATCH, D), 1.0),
        'ctx': nrm(ks[2], (BATCH, CTX_LEN, D), 1.0),
        'c_ctx': nrm(ks[3], (D,), 1.0),
        'w_ada': nrm(ks[4], (DEPTH, D, 6 * D), 0.5 * D ** -0.5),
        'b_ada': nrm(ks[5], (DEPTH, 6 * D), 0.02),
        'g_norm_mix': 1.0 + nrm(ks[6], (DEPTH, D), 0.02),
        'g_norm_ffn': 1.0 + nrm(ks[7], (DEPTH, D), 0.02),
        'w_in': nrm(ks[8], (DEPTH, D, IN_COLS), D ** -0.5),
        'da_lambda': nrm(ks[9], (DEPTH, 4, DA_HEAD_DIM), 0.1),
        'da_subln_g': 1.0 + nrm(ks[10], (DEPTH, 2 * DA_HEAD_DIM), 0.02),
        'sg_ln_g': 1.0 + nrm(ks[11], (DEPTH, SG_WIDTH), 0.02),
        'sg_ln_b': nrm(ks[12], (DEPTH, SG_WIDTH), 0.02),
        'sg_w': nrm(ks[13], (DEPTH, SG_GROUPS, SG_CHUNK, SG_CHUNK), SG_CHUNK ** -0.5),
        'sg_b': 1.0 + nrm(ks[14], (DEPTH, SG_GROUPS, SG_CHUNK), 0.02),
        'na_rpb': nrm(ks[15], (DEPTH, NA_HEADS, 2 * NA_ROWS_MAX - 1, 2 * NA_COLS - 1), 0.1),
        'w_branch': nrm(ks[16], (DEPTH, N_BRANCH, BRANCH_WIDTH, D), BRANCH_WIDTH ** -0.5),
        'w_out': nrm(ks[17], (DEPTH, D, D), D ** -0.5),
        'moe_w_group': nrm(ks[18], (DEPTH, D, N_GROUPS), D ** -0.5),
        'moe_b_group': nrm(ks[19], (DEPTH, N_GROUPS), 0.01),
        'moe_w_router': nrm(ks[20], (DEPTH, D, N_EXPERTS), D ** -0.5),
        'moe_b_router': nrm(ks[21], (DEPTH, N_EXPERTS), 0.01),
        'moe_w_gate': nrm(ks[22], (DEPTH, N_EXPERTS, D, D_EXPERT), D ** -0.5),
        'moe_w_up': nrm(ks[23], (DEPTH, N_EXPERTS, D, D_EXPERT), D ** -0.5),
        'moe_w_down': nrm(ks[24], (DEPTH, N_EXPERTS, D_EXPERT, D), D_EXPERT ** -0.5),
        'g_final': 1.0 + nrm(ks[25], (D,), 0.02),
    }


def reference(x, c, ctx, c_ctx, w_ada, b_ada, g_norm_mix, g_norm_ffn, w_in, da_lambda, da_subln_g,
              sg_ln_g, sg_ln_b, sg_w, sg_b, na_rpb, w_branch, w_out, moe_w_group, moe_b_group,
              moe_w_router, moe_b_router, moe_w_gate, moe_w_up, moe_w_down, g_final):
    b, n_lat, _ = x.shape
    n_ctx = ctx.shape[1]
    rows = n_lat // GRID_W
    cos, sin = axial_rope_tables(n_lat)
    cond_lat = jax.nn.silu(c)
    cond_ctx = jax.nn.silu(c_ctx)[None]
    h, hc = x, ctx
    for l in range(DEPTH):
        last = l == DEPTH - 1
        lam_init = 0.8 - 0.6 * math.exp(-0.3 * l)
        lq1, lk1, lq2, lk2 = da_lambda[l].astype(jnp.float32)
        lam = jnp.exp(jnp.sum(lq1 * lk1)) - jnp.exp(jnp.sum(lq2 * lk2)) + lam_init
        sh1, sc1, gt1, sh2, sc2, gt2 = ada_modulation(cond_lat, w_ada[l], b_ada[l])
        csh1, csc1, cgt1, csh2, csc2, cgt2 = ada_modulation(cond_ctx, w_ada[l], b_ada[l])

        xn = modulate(rms_norm(h, g_norm_mix[l]), sh1, sc1)
        xc = modulate(rms_norm(hc, g_norm_mix[l]), csh1, csc1)
        p = xn @ w_in[l]
        pc = xc @ (w_in[l][:, :KV_COLS] if last else w_in[l])
        ka_c = da_qk_heads(pc[..., OFF_KA:OFF_VA])
        va_c = da_v_heads(pc[..., OFF_VA:OFF_KC])
        kc_c = na_heads(pc[..., OFF_KC:OFF_VC])
        vc_c = na_heads(pc[..., OFF_VC:KV_COLS])

        qa = apply_axial_rope(da_qk_heads(p[..., OFF_QA:OFF_QC]), cos, sin)
        ka = apply_axial_rope(da_qk_heads(p[..., OFF_KA:OFF_VA]), cos, sin)
        va = da_v_heads(p[..., OFF_VA:OFF_KC])
        y_a = diff_post(diff_attention_latent(qa, ka, va, ka_c, va_c, lam), da_subln_g[l], lam_init)
        y_b = spatial_gating(p[..., OFF_ZB:OFF_GATE], sg_ln_g[l], sg_ln_b[l], sg_w[l], sg_b[l])
        y_c = neighbourhood_attention(na_heads(p[..., OFF_QC:OFF_ZB]), na_heads(p[..., OFF_KC:OFF_VC]),
                                      na_heads(p[..., OFF_VC:KV_COLS]), kc_c, vc_c, na_rpb[l], rows)
        h = h + gt1 * merge_branches(p[..., OFF_GATE:], y_a, y_b, y_c, w_branch[l], w_out[l])
        if not last:
            ya_c = diff_post(diff_softmax_mix(da_qk_heads(pc[..., OFF_QA:OFF_QC]), ka_c, va_c, lam), da_subln_g[l], lam_init)
            yb_c = spatial_gating(pc[..., OFF_ZB:OFF_GATE], sg_ln_g[l], sg_ln_b[l], sg_w[l], sg_b[l])
            yc_c = ctx_attention(na_heads(pc[..., OFF_QC:OFF_ZB]), kc_c, vc_c)
            hc = hc + cgt1 * merge_branches(pc[..., OFF_GATE:], ya_c, yb_c, yc_c, w_branch[l], w_out[l])

        xn2 = modulate(rms_norm(h, g_norm_ffn[l]), sh2, sc2).reshape(b * n_lat, D_MODEL)
        if last:
            y = hier_moe(xn2, moe_w_group[l], moe_b_group[l], moe_w_router[l], moe_b_router[l],
                         moe_w_gate[l], moe_w_up[l], moe_w_down[l])
            h = h + gt2 * y.reshape(b, n_lat, D_MODEL)
        else:
            xc2 = modulate(rms_norm(hc, g_norm_ffn[l]), csh2, csc2).reshape(b * n_ctx, D_MODEL)
            y = hier_moe(jnp.concatenate([xn2, xc2], axis=0), moe_w_group[l], moe_b_group[l],
                         moe_w_router[l], moe_b_router[l], moe_w_gate[l], moe_w_up[l], moe_w_down[l])
            h = h + gt2 * y[:b * n_lat].reshape(b, n_lat, D_MODEL)
            hc = hc + cgt2 * y[b * n_lat:].reshape(b, n_ctx, D_MODEL)
    return rms_norm(h, g_final)
```

```python
import math
from contextlib import ExitStack

import numpy as np
import concourse.bass as bass
import concourse.mybir as mybir
from concourse.bass_utils import run_bass_kernel_spmd

F32 = mybir.dt.float32
BF16 = mybir.dt.bfloat16
AF = mybir.ActivationFunctionType
ALU = mybir.AluOpType
AX = mybir.AxisListType

NCORES = 8
D = 1024
SEQ = 8192
NCTX = 256
GRID_W = 64
TL = 1024
NLAT = 2 * TL
NTOK = NLAT + 2 * NCTX
EPS = 1e-6
OFF_KA, OFF_VA, OFF_KC, OFF_VC, OFF_QA, OFF_QC, OFF_ZB, OFF_GATE = 0, 512, 1024, 1536, 2048, 2560, 3072, 4096
IN_COLS = 7168
NEXP = 32
DEXP = 512
MOE_CAP = 512
SEC_KA, SEC_VA, SEC_KCH, SEC_VCH, SEC_B = 0, 512, 1024, 1280, 1536
SEND_ROWS = 2 * SEC_B


class Buf:
    __slots__ = ("name", "w", "r")

    def __init__(self, name=""):
        self.name = name
        self.w = None
        self.r = {}


class Prog:
    def __init__(self, nc, es, n_dma_sems=(28, 16)):
        self.nc = nc
        self.engs = {"pe": nc.tensor, "act": nc.scalar, "dve": nc.vector, "pool": nc.gpsimd, "sp": nc.sync}
        self.semobj = {}
        self.cnt = {}
        for e in ["pe", "act", "dve", "pool"]:
            self.semobj[e] = es.enter_context(nc.semaphore("s_" + e))
            self.cnt[e] = 0
        self.dpool = {"sp": [], "pool": []}
        for q, n in zip(["sp", "pool"], n_dma_sems):
            for i in range(n):
                k = "d_%s_%d" % (q, i)
                self.semobj[k] = es.enter_context(nc.semaphore(k))
                self.cnt[k] = 0
                self.dpool[q].append(k)
        self.semobj["cc"] = es.enter_context(nc.semaphore("s_cc"))
        self.cnt["cc"] = 0
        self.drr = {"sp": 0, "pool": 0}
        self.waited = {e: {} for e in self.engs}
        self.nins = 0

    def _collect(self, reads, writes):
        need = {}

        def add(k, v):
            if need.get(k, 0) < v:
                need[k] = v

        for b in reads:
            if b.w is not None:
                add(*b.w)
        for b in writes:
            if b.w is not None:
                add(*b.w)
            for k, v in b.r.items():
                add(k, v)
        return need

    def _emit_waits(self, eng, need):
        w = self.waited[eng]
        e = self.engs[eng]
        for k, v in need.items():
            if eng == "pe" and k == "pe":
                continue
            if w.get(k, 0) < v:
                e.wait_ge(self.semobj[k], v)
                w[k] = v
                self.nins += 1

    def _update(self, tok, reads, writes):
        k, v = tok
        for b in reads:
            if b.r.get(k, 0) < v:
                b.r[k] = v
        for b in writes:
            b.w = tok
            b.r = {}

    def op(self, eng, fn, reads=(), writes=(), signal=True):
        self._emit_waits(eng, self._collect(reads, writes))
        ins = fn(self.engs[eng])
        self.nins += 1
        if signal:
            ins.then_inc(self.semobj[eng], 1)
            self.cnt[eng] += 1
            tok = (eng, self.cnt[eng])
        else:
            tok = (eng, self.cnt[eng] + 1)
        self._update(tok, reads, writes)
        return tok

    def dma(self, q, out, in_, reads=(), writes=(), **kw):
        need = self._collect(reads, writes)
        pool = self.dpool[q]
        k = pool[self.drr[q] % len(pool)]
        self.drr[q] += 1
        if self.cnt[k] > 0 and need.get(k, 0) < self.cnt[k]:
            need[k] = self.cnt[k]
        self._emit_waits(q, need)
        ins = self.engs[q].dma_start(out=out, in_=in_, **kw)
        self.nins += 1
        ins.then_inc(self.semobj[k], 16)
        self.cnt[k] += 16
        tok = (k, self.cnt[k])
        self._update(tok, reads, writes)
        return tok

    def indirect(self, kind, dram, idx, sb_ap, bound, reads=(), writes=()):
        q = "pool"
        need = self._collect(reads, writes)
        pool = self.dpool[q]
        k = pool[self.drr[q] % len(pool)]
        self.drr[q] += 1
        if self.cnt[k] > 0 and need.get(k, 0) < self.cnt[k]:
            need[k] = self.cnt[k]
        self._emit_waits(q, need)
        off = bass.IndirectOffsetOnAxis(ap=idx, axis=0)
        if not hasattr(self, "_bregs"):
            self._bregs = {}
        if bound not in self._bregs:
            self._bregs[bound] = self.nc.gpsimd.to_reg(bound)
        bound = self._bregs[bound]
        if kind == "scatter":
            ins = self.nc.gpsimd.indirect_dma_start(out=dram[:, :], out_offset=off, in_=sb_ap, in_offset=None, bounds_check=bound,
                                                    oob_is_err=False)
        else:
            ins = self.nc.gpsimd.indirect_dma_start(out=sb_ap, out_offset=None, in_=dram[:, :], in_offset=off, bounds_check=bound,
                                                    oob_is_err=False)
        self.nins += 1
        ins.then_inc(self.semobj[k], 16)
        self.cnt[k] += 16
        tok = (k, self.cnt[k])
        self._update(tok, reads, writes)
        return tok

    def collective(self, in_ap, out_ap, reads=(), writes=()):
        self._emit_waits("pool", self._collect(reads, writes))
        ins = self.nc.gpsimd.collective_compute("AllGather", ALU.bypass, replica_groups=[list(range(NCORES))],
                                                ins=[in_ap.opt()], outs=[out_ap.opt()])
        self.nins += 1
        ins.then_inc(self.semobj["cc"], 1)
        self.cnt["cc"] += 1
        tok = ("cc", self.cnt["cc"])
        self._update(tok, reads, writes)
        return tok

    def all_counts(self):
        return {k: v for k, v in self.cnt.items() if v > 0}

    def barrier(self):
        need = self.all_counts()
        for e in self.engs:
            self._emit_waits(e, need)

    def finish(self):
        self._emit_waits("sp", self.all_counts())


class KB:
    def __init__(self, launch, dbg=None):
        self.launch = launch
        self.dbg = dbg or {}
        self.nc = bass.Bass("TRN2", target_bir_lowering=False)
        self.in_names = []
        self.out_names = []

    def din(self, name, shape, dt=F32):
        self.in_names.append(name)
        return self.nc.dram_tensor(name, list(shape), dt, kind="ExternalInput").ap()

    def dout(self, name, shape, dt=F32):
        self.out_names.append(name)
        return self.nc.dram_tensor(name, list(shape), dt, kind="ExternalOutput").ap()

    def dscr(self, name, shape, dt=F32):
        return self.nc.dram_tensor(name, list(shape), dt).ap()

    def sb(self, es, name, shape, dt=F32):
        self._uid = getattr(self, "_uid", 0) + 1
        return es.enter_context(self.nc.sbuf_tensor("sb%d_%s" % (self._uid, name), list(shape), dt))

    def mm(self, out, lhsT, rhs, start, stop, rd, wr, signal=None):
        if signal is None:
            signal = stop
        return self.P.op("pe", lambda e: e.matmul(out, lhsT=lhsT, rhs=rhs, start=start, stop=stop), reads=rd, writes=wr,
                         signal=signal)

    def tr(self, out, in_, ident, rd, wr, signal=True):
        return self.P.op("pe", lambda e: e.transpose(out=out, in_=in_, identity=ident), reads=rd, writes=wr, signal=signal)

    def act(self, out, in_, func, rd, wr, bias=None, scale=None, accum_out=None):
        kw = {}
        if bias is not None:
            kw["bias"] = bias
        if scale is not None:
            kw["scale"] = scale
        if accum_out is not None:
            kw["accum_out"] = accum_out
        return self.P.op("act", lambda e: e.activation(out=out, in_=in_, func=func, **kw), reads=rd, writes=wr)

    def tt(self, eng, out, in0, in1, op, rd, wr):
        return self.P.op(eng, lambda e: e.tensor_tensor(out=out, in0=in0, in1=in1, op=op), reads=rd, writes=wr)

    def ts(self, eng, out, in0, s1, s2, op0, op1, rd, wr):
        if op1 is None:
            return self.P.op(eng, lambda e: e.tensor_scalar(out=out, in0=in0, scalar1=s1, scalar2=None, op0=op0), reads=rd,
                             writes=wr)
        return self.P.op(eng, lambda e: e.tensor_scalar(out=out, in0=in0, scalar1=s1, scalar2=s2, op0=op0, op1=op1),
                         reads=rd, writes=wr)

    def stt(self, eng, out, in0, scalar, in1, op0, op1, rd, wr):
        return self.P.op(eng, lambda e: e.scalar_tensor_tensor(out=out, in0=in0, scalar=scalar, in1=in1, op0=op0, op1=op1),
                         reads=rd, writes=wr)

    def cp(self, eng, out, in_, rd, wr):
        if eng == "act":
            return self.P.op("act", lambda e: e.copy(out=out, in_=in_), reads=rd, writes=wr)
        return self.P.op(eng, lambda e: e.tensor_copy(out=out, in_=in_), reads=rd, writes=wr)

    def recip(self, out, in_, rd, wr):
        return self.P.op("dve", lambda e: e.reciprocal(out=out, in_=in_), reads=rd, writes=wr)

    def memset(self, eng, ap, val, wr):
        return self.P.op(eng, lambda e: e.memset(ap, val), writes=wr)

    def rsqrt_inplace(self, ap, mult, add, buf):
        self.ts("dve", ap, ap, mult, add, ALU.mult, ALU.add, [buf], [buf])
        self.act(ap, ap, AF.Sqrt, [buf], [buf])
        self.recip(ap, ap, [buf], [buf])

    def setup(self, es, layers):
        nc = self.nc
        d = {}
        d["condT"] = self.din("condT", [128, 8, 3])
        d["ident"] = self.din("ident", [128, 128])
        d["pm"] = self.din("pm", [128, 128])
        d["ropec"] = self.din("ropec", [128, TL])
        d["ropes"] = self.din("ropes", [128, TL])
        d["selw"] = self.din("selw", [128, 16])
        d["tri"] = self.din("tri", [128, 128])
        d["ecb"] = self.din("ecb", [128, NEXP])
        for l in layers:
            d["w_ada_%d" % l] = self.din("w_ada_%d" % l, [D, 6 * D])
            d["b_adaT_%d" % l] = self.din("b_adaT_%d" % l, [128, 48])
            d["gmixT_%d" % l] = self.din("gmixT_%d" % l, [128, 8])
            d["gffnT_%d" % l] = self.din("gffnT_%d" % l, [128, 8])
            d["w_in_%d" % l] = self.din("w_in_%d" % l, [D, IN_COLS])
            if (l, "full") in self.need:
                d["dalam_%d" % l] = self.din("dalam_%d" % l, [1, 256])
                d["dasub_%d" % l] = self.din("dasub_%d" % l, [128, 1])
                d["sglng_%d" % l] = self.din("sglng_%d" % l, [1, 512])
                d["sglnb_%d" % l] = self.din("sglnb_%d" % l, [1, 512])
                d["sgwT_%d" % l] = self.din("sgwT_%d" % l, [4, 128, 128])
                d["sgb_%d" % l] = self.din("sgb_%d" % l, [1, 512])
                d["nabias_%d" % l] = self.din("nabias_%d" % l, [2, 8, 8, 128, 512])
                d["w_branch_%d" % l] = self.din("w_branch_%d" % l, [3, 512, D])
                d["w_out_%d" % l] = self.din("w_out_%d" % l, [D, D])
                d["wr_%d" % l] = self.din("wr_%d" % l, [D, 36])
                d["br_%d" % l] = self.din("br_%d" % l, [1, 36])
                d["wg_%d" % l] = self.din("wg_%d" % l, [NEXP, D, DEXP])
                d["wu_%d" % l] = self.din("wu_%d" % l, [NEXP, D, DEXP])
                d["wd_%d" % l] = self.din("wd_%d" % l, [NEXP, DEXP, D])
        self.d = d
        P = self.P
        self.bank = [es.enter_context(nc.psum_tensor("bank%d" % i, [128, 512], F32)) for i in range(8)]
        self.bankB = [Buf("bank%d" % i) for i in range(8)]
        self.ident = self.sb(es, "ident_f", [128, 128])
        self.pm = self.sb(es, "pm_f", [128, 128])
        self.ones_f = self.sb(es, "ones_f", [128, 128])
        self.ones_b = self.sb(es, "ones_b", [128, 128], BF16)
        self.ident_b = self.sb(es, "ident_b", [128, 128], BF16)
        self.cB = Buf("consts")
        P.dma("sp", self.ident[:], d["ident"], writes=[self.cB])
        P.dma("sp", self.pm[:], d["pm"], writes=[self.cB])
        self.memset("dve", self.ones_f[:], 1.0, [self.cB])
        self.memset("dve", self.ones_b[:], 1.0, [self.cB])
        self.cp("dve", self.ident_b[:], self.ident[:], [self.cB], [self.cB])
        self.selw = self.sb(es, "selw", [128, 16])
        P.dma("sp", self.selw[:], d["selw"], writes=[self.cB])
        self.condS = self.sb(es, "condS", [128, 8, 3])
        self.condSB = Buf("condS")
        P.dma("sp", self.condS[:], d["condT"], writes=[self.condSB])
        self.act(self.condS[:], self.condS[:], AF.Silu, [self.condSB], [self.condSB])
        self.modT = {}
        self.modTB = {}
        self.s1T = {}
        self.s2T = {}
        for l in layers:
            self.modT[l] = self.sb(es, "modT%d" % l, [128, 48, 3])
            self.s1T[l] = self.sb(es, "s1T%d" % l, [128, 8, 3])
            self.s2T[l] = self.sb(es, "s2T%d" % l, [128, 8, 3])
            self.modTB[l] = Buf("modT%d" % l)
        self.xnT = self.sb(es, "xnT", [128, 8, NTOK], BF16)
        self.xnTB = [Buf("xnT%d" % i) for i in range(NTOK // 128)]
        self.hbuf = self.dscr("hbuf", [NTOK, D])
        self.hB = [Buf("h%d" % i) for i in range(NTOK // 128)]

    @staticmethod
    def variant(tile):
        return 0 if tile < 8 else (1 if tile < 16 else 2)

    def phase_cond(self, l):
        P = self.P
        d = self.d
        with ExitStack() as ph:
            wblk = [self.sb(ph, "wada%d" % i, [128, 8, 512]) for i in range(2)]
            wB = [Buf() for _ in range(2)]
            bT = self.sb(ph, "badaT", [128, 48])
            g1 = self.sb(ph, "g1T", [128, 8])
            g2 = self.sb(ph, "g2T", [128, 8])
            sB = Buf()
            P.dma("sp", bT[:], d["b_adaT_%d" % l], writes=[sB])
            P.dma("sp", g1[:], d["gmixT_%d" % l], writes=[sB])
            P.dma("sp", g2[:], d["gffnT_%d" % l], writes=[sB])
            ps, psB = self.bank[0], self.bankB[0]
            for cb in range(12):
                w, B = wblk[cb % 2], wB[cb % 2]
                P.dma("sp", w[:], d["w_ada_%d" % l][:, cb * 512:(cb + 1) * 512].rearrange("(k p) c -> p k c", p=128),
                      writes=[B])
                for j in range(4):
                    cc = cb * 4 + j
                    for k in range(8):
                        self.mm(ps[:, cc * 3:(cc + 1) * 3], w[:, k, j * 128:(j + 1) * 128], self.condS[:, k, :], k == 0, k == 7,
                                [B, self.condSB], [psB])
            modT, mB = self.modT[l], self.modTB[l]
            self.tt("dve", modT[:], ps[:, 0:144].rearrange("p (c v) -> p c v", v=3),
                    bT[:].unsqueeze(2).to_broadcast([128, 48, 3]), ALU.add, [psB, sB], [mB])
            self.stt("dve", self.s1T[l][:], modT[:, 8:16, :], 1.0, g1[:].unsqueeze(2).to_broadcast([128, 8, 3]), ALU.add,
                     ALU.mult, [mB, sB], [mB])
            self.stt("dve", self.s2T[l][:], modT[:, 32:40, :], 1.0, g2[:].unsqueeze(2).to_broadcast([128, 8, 3]), ALU.add,
                     ALU.mult, [mB, sB], [mB])
            P.barrier()

    def phase_norm(self, l, sub, ntok, route=None):
        P = self.P
        ntile = ntok // 128
        scaleT = self.s1T[l] if sub == 1 else self.s2T[l]
        sh0 = 0 if sub == 1 else 24
        mB = self.modTB[l]
        with ExitStack() as ph:
            ht = [self.sb(ph, "ht%d" % i, [128, D]) for i in range(2)]
            htB = [Buf() for _ in range(2)]
            hn = [self.sb(ph, "hn%d" % i, [128, D]) for i in range(2)]
            hnB = [Buf() for _ in range(2)]
            tmp = [self.sb(ph, "ntmp%d" % i, [128, 4, 128]) for i in range(2)]
            tmpB = [Buf() for _ in range(2)]
            junk = self.sb(ph, "junk", [128, D])
            junkB = Buf()
            ss = self.sb(ph, "ss", [128, 32])
            ssB = Buf()
            self.memset("dve", ss[:], 0.0, [ssB])
            for t in range(ntile):
                i = t % 2
                P.dma("sp", ht[i][:], self.hbuf[t * 128:(t + 1) * 128, :], reads=[self.hB[t]], writes=[htB[i]])
                self.act(junk[:], ht[i][:], AF.Square, [htB[i], ssB], [junkB, ssB], accum_out=ss[:, t:t + 1])
            self.rsqrt_inplace(ss[:, 0:ntile], 1.0 / D, EPS, ssB)
            if route is not None:
                route["begin"](ph)
            for t in range(ntile):
                i = t % 2
                v = self.variant(t)
                P.dma("sp", ht[i][:], self.hbuf[t * 128:(t + 1) * 128, :], reads=[self.hB[t]], writes=[htB[i]])
                self.act(hn[i][:], ht[i][:], AF.Copy, [htB[i], ssB], [hnB[i]], scale=ss[:, t:t + 1])
                for half in range(2):
                    bk, bB = self.bank[half], self.bankB[half]
                    for c in range(4):
                        cc = half * 4 + c
                        self.tr(bk[:, c * 128:(c + 1) * 128], hn[i][:, cc * 128:(cc + 1) * 128], self.ident[:], [hnB[i], self.cB],
                                [bB], signal=(c == 3))
                    c0 = half * 4
                    j = half
                    self.tt("dve", tmp[j][:], bk[:].rearrange("p (c t) -> p c t", c=4),
                            scaleT[:, c0:c0 + 4, v].unsqueeze(2).to_broadcast([128, 4, 128]), ALU.mult, [bB, mB], [tmpB[j]])
                    shift = self.modT[l][:, sh0 + c0:sh0 + c0 + 4, v].unsqueeze(2).to_broadcast([128, 4, 128])
                    if route is None:
                        self.tt("pool", self.xnT[:, c0:c0 + 4, t * 128:(t + 1) * 128], tmp[j][:], shift, ALU.add, [tmpB[j], mB],
                                [self.xnTB[t]])
                    else:
                        self.tt("dve", tmp[j][:], tmp[j][:], shift, ALU.add, [tmpB[j], mB], [tmpB[j]])
                        self.cp("pool", self.xnT[:, c0:c0 + 4, t * 128:(t + 1) * 128], tmp[j][:], [tmpB[j]], [self.xnTB[t]])
                        route["tile"](t, half, tmp[j], tmpB[j])
            P.barrier()

    def load_w(self, dst, dstB, src, nsplit=1):
        n = src.shape[1]
        step = n // nsplit
        for s in range(nsplit):
            self.P.dma("pool", dst[:, :, s * step:(s + 1) * step],
                       src[:, s * step:(s + 1) * step].rearrange("(k p) c -> p k c", p=128), writes=[dstB])

    def proj_fm(self, bank, bankB, W, WB, col0, tok0, ntok, ncol=128):
        rd = [WB] + self.xnTB[tok0 // 128:(tok0 + ntok + 127) // 128]
        for k in range(8):
            self.mm(bank[0:ncol, 0:ntok], W[:, k, col0:col0 + ncol], self.xnT[:, k, tok0:tok0 + ntok], k == 0, k == 7, rd, [bankB])

    def proj_tm(self, bank, bankB, W, WB, col0, ncol, tok0):
        rd = [WB, self.xnTB[tok0 // 128]]
        for k in range(8):
            self.mm(bank[:, 0:ncol], self.xnT[:, k, tok0:tok0 + 128], W[:, k, col0:col0 + ncol], k == 0, k == 7, rd, [bankB])

    def rope_evac(self, ph_state, bank, bankB, rbank, rbankB, dst, dstB, pos0, n):
        st = ph_state
        i = st["i"] = (st.get("i", -1) + 1) % 2
        xs, xsB = st["xs"][i], st["xsB"][i]
        t1, t1B = st["t1"][i], st["t1B"][i]
        self.cp("act", xs[:, 0:n], bank[:, 0:n], [bankB], [xsB])
        self.mm(rbank[:, 0:n], self.pm[:], xs[:, 0:n], True, True, [self.cB, xsB], [rbankB])
        self.tt("dve", t1[:, 0:n], xs[:, 0:n], self.ropec[:, pos0:pos0 + n], ALU.mult, [xsB, self.ropeB], [t1B])
        self.tt("dve", xs[:, 0:n], rbank[:, 0:n], self.ropes[:, pos0:pos0 + n], ALU.mult, [rbankB, self.ropeB, xsB], [xsB])
        self.tt("pool", dst, t1[:, 0:n], xs[:, 0:n], ALU.add, [t1B, xsB], [dstB])

    def rope_state(self, ph):
        self.ropec = self.sb(ph, "ropec", [128, TL])
        self.ropes = self.sb(ph, "ropes", [128, TL])
        self.ropeB = Buf("rope")
        self.P.dma("sp", self.ropec[:], self.d["ropec"], writes=[self.ropeB])
        self.P.dma("sp", self.ropes[:], self.d["ropes"], writes=[self.ropeB])
        return {"xs": [self.sb(ph, "rxs%d" % i, [128, 512]) for i in range(2)], "xsB": [Buf() for _ in range(2)],
                "t1": [self.sb(ph, "rt1%d" % i, [128, 512]) for i in range(2)], "t1B": [Buf() for _ in range(2)]}

    def sec(self, buf, rank, b, sec0, nrows):
        r0 = rank * SEND_ROWS + b * SEC_B + sec0
        return buf[r0:r0 + nrows, :]

    def phase_kv(self, l, send, sendB):
        P = self.P
        d = self.d
        with ExitStack() as ph:
            W = self.sb(ph, "wkv", [128, 8, 2048], BF16)
            WB = Buf()
            self.load_w(W, WB, d["w_in_%d" % l][:, 0:2048], nsplit=4)
            rs = self.rope_state(ph)
            st = [self.sb(ph, "kvst%d" % i, [128, 512], BF16) for i in range(4)]
            stB = [Buf() for _ in range(4)]
            si = 0
            for b in range(2):
                tok0 = b * TL
                ctok0 = NLAT + b * NCTX
                for cc in range(4):
                    for half in range(2):
                        bk, bB = self.bank[si % 2], self.bankB[si % 2]
                        s, sB = st[si % 4], stB[si % 4]
                        self.proj_fm(bk, bB, W, WB, OFF_KA + cc * 128, tok0 + half * 512, 512)
                        self.rope_evac(rs, bk, bB, self.bank[2 + si % 2], self.bankB[2 + si % 2], s[:], sB, half * 512, 512)
                        P.dma("sp", self.sec(send, 0, b, SEC_KA + cc * 128, 128)[:, half * 512:(half + 1) * 512], s[:],
                              reads=[sB], writes=[sendB])
                        si += 1
                    bk, bB = self.bank[si % 2], self.bankB[si % 2]
                    s, sB = st[si % 4], stB[si % 4]
                    self.proj_fm(bk, bB, W, WB, OFF_KA + cc * 128, ctok0, NCTX)
                    self.cp("act", s[:, 0:NCTX], bk[:, 0:NCTX], [bB], [sB])
                    P.dma("sp", self.ckaT[b][cc * 128:(cc + 1) * 128, :], s[:, 0:NCTX], reads=[sB], writes=[self.ckvB])
                    si += 1
                for cc in range(4):
                    for half in range(2):
                        bk, bB = self.bank[si % 2], self.bankB[si % 2]
                        s, sB = st[si % 4], stB[si % 4]
                        self.proj_fm(bk, bB, W, WB, OFF_KC + cc * 128, tok0 + half * 512, 512)
                        self.cp("act", s[:], bk[:], [bB], [sB])
                        P.dma("sp", self.kcT[b][cc * 128:(cc + 1) * 128, half * 512:(half + 1) * 512], s[:], reads=[sB],
                              writes=[self.kvownB])
                        hsec = self.sec(send, 0, b, SEC_KCH, 256).rearrange("a (two t) -> (a two) t", two=2)
                        src = s[:, 0:256] if half == 0 else s[:, 256:512]
                        P.dma("sp", hsec[cc * 128:(cc + 1) * 128, half * 256:(half + 1) * 256], src, reads=[sB], writes=[sendB])
                        si += 1
                    bk, bB = self.bank[si % 2], self.bankB[si % 2]
                    s, sB = st[si % 4], stB[si % 4]
                    self.proj_fm(bk, bB, W, WB, OFF_KC + cc * 128, ctok0, NCTX)
                    self.cp("act", s[:, 0:NCTX], bk[:, 0:NCTX], [bB], [sB])
                    P.dma("sp", self.ckcT[b][cc * 128:(cc + 1) * 128, :], s[:, 0:NCTX], reads=[sB], writes=[self.ckvB])
                    si += 1
                vasec = self.sec(send, 0, b, SEC_VA, 512).rearrange("r c -> (r c)").rearrange("(h t e) -> t h e", h=4, t=TL)
                vchsec = self.sec(send, 0, b, SEC_VCH, 256).rearrange("a (two e) -> (a two) e", two=2)
                for t in range(10):
                    lat = t < 8
                    tk = tok0 + t * 128 if lat else ctok0 + (t - 8) * 128
                    for which in range(2):
                        bk, bB = self.bank[si % 2], self.bankB[si % 2]
                        s, sB = st[si % 4], stB[si % 4]
                        self.proj_tm(bk, bB, W, WB, OFF_VA if which == 0 else OFF_VC, 512, tk)
                        self.cp("act" if which == 0 else "dve", s[:], bk[:], [bB], [sB])
                        if which == 0:
                            if lat:
                                P.dma("sp", vasec[t * 128:(t + 1) * 128, :, :], s[:].rearrange("p (h e) -> p h e", h=4),
                                      reads=[sB], writes=[sendB])
                            else:
                                P.dma("sp", self.cva[b][(t - 8) * 128:(t - 7) * 128, :], s[:], reads=[sB], writes=[self.ckvB])
                        else:
                            if lat:
                                P.dma("sp", self.vc[b][t * 128:(t + 1) * 128, :], s[:], reads=[sB], writes=[self.kvownB])
                                if t < 2:
                                    P.dma("sp", vchsec[t * 128:(t + 1) * 128, :], s[:], reads=[sB], writes=[sendB])
                                elif t >= 6:
                                    P.dma("sp", vchsec[(t - 4) * 128:(t - 3) * 128, :], s[:], reads=[sB], writes=[sendB])
                            else:
                                P.dma("sp", self.cvc[b][(t - 8) * 128:(t - 7) * 128, :], s[:], reads=[sB], writes=[self.ckvB])
                        si += 1
            P.barrier()

    def alloc_kv_scratch(self):
        self.ckaT = [self.dscr("ckaT%d" % b, [512, NCTX], BF16) for b in range(2)]
        self.ckcT = [self.dscr("ckcT%d" % b, [512, NCTX], BF16) for b in range(2)]
        self.cva = [self.dscr("cva%d" % b, [NCTX, 512], BF16) for b in range(2)]
        self.cvc = [self.dscr("cvc%d" % b, [NCTX, 512], BF16) for b in range(2)]
        self.kcT = [self.dscr("kcT%d" % b, [512, TL], BF16) for b in range(2)]
        self.vc = [self.dscr("vc%d" % b, [TL, 512], BF16) for b in range(2)]
        self.ckvB = Buf("ckv")
        self.kvownB = Buf("kvown")

    def bcast_rows(self, ph, vecT_fn, nchunk, dst, dstB, rdB):
        tmpd = [self.sb(ph, "bct%d" % i, [128, 128]) for i in range(2)]
        tB = [Buf() for _ in range(2)]
        for c in range(nchunk):
            i = c % 2
            bk, bB = self.bank[(c // 4) % 2], self.bankB[(c // 4) % 2]
            self.ts("dve", tmpd[i][:], self.ident[:], vecT_fn(c), None, ALU.mult, None, [self.cB] + rdB, [tB[i]])
            self.mm(bk[:, (c % 4) * 128:(c % 4 + 1) * 128], self.ones_f[:], tmpd[i][:], True, True, [self.cB, tB[i]], [bB])
            if c % 4 == 3 or c == nchunk - 1:
                c0 = (c // 4) * 4
                n = (c - c0 + 1) * 128
                self.cp("act", dst[:, c0 * 128:c0 * 128 + n], bk[:, 0:n], [bB], [dstB])

    def merge_setup(self, ph, l, i, K):
        nparts = 512 // K
        Wb = self.sb(ph, "wb%d" % i, [K, nparts, D], BF16)
        WbB = Buf()
        self.P.dma("pool", Wb[:], self.d["w_branch_%d" % l][i].rearrange("(j p) c -> p j c", p=K), writes=[WbB])
        Wg = self.sb(ph, "wgate%d" % i, [128, 8, D], BF16)
        WgB = Buf()
        self.load_w(Wg, WgB, self.d["w_in_%d" % l][:, OFF_GATE + i * D:OFF_GATE + (i + 1) * D], nsplit=2)
        sg = [self.sb(ph, "sgt%d_%d" % (i, j), [128, 512]) for j in range(2)]
        sgB = [Buf() for _ in range(2)]
        return {"Wb": Wb, "WbB": WbB, "Wg": Wg, "WgB": WgB, "K": K, "nparts": nparts, "sg": sg, "sgB": sgB, "n": 0}

    def merge(self, ms, yget, yB, tok0, ntok, first, banks=(6, 7)):
        K, nparts = ms["K"], ms["nparts"]
        for s0 in range(0, ntok, 512):
            n = min(512, ntok - s0)
            t0 = tok0 + s0
            mB = self.mTB[t0 // 128:(t0 + n + 127) // 128]
            for fc in range(8):
                bA, bAB = self.bank[banks[0]], self.bankB[banks[0]]
                bG, bGB = self.bank[banks[1]], self.bankB[banks[1]]
                for j in range(nparts):
                    self.mm(bA[:, 0:n], ms["Wb"][0:K, j, fc * 128:(fc + 1) * 128], yget(j, s0, n), j == 0, j == nparts - 1,
                            [ms["WbB"]] + yB, [bAB])
                self.proj_fm(bG, bGB, ms["Wg"], ms["WgB"], fc * 128, t0, n)
                i = ms["n"] = (ms["n"] + 1) % 2
                sg, sgB = ms["sg"][i], ms["sgB"][i]
                self.act(sg[:, 0:n], bG[:, 0:n], AF.Sigmoid, [bGB], [sgB])
                if first:
                    self.tt("dve", self.mT[:, fc, t0:t0 + n], bA[:, 0:n], sg[:, 0:n], ALU.mult, [bAB, sgB], mB)
                else:
                    self.tt("dve", sg[:, 0:n], bA[:, 0:n], sg[:, 0:n], ALU.mult, [bAB, sgB], [sgB])
                    self.tt("pool", self.mT[:, fc, t0:t0 + n], self.mT[:, fc, t0:t0 + n], sg[:, 0:n], ALU.add, [sgB] + mB, mB)

    def mixer_b(self, l, last):
        P, d = self.P, self.d
        nchunk = (NLAT if last else NTOK) // 128
        with ExitStack() as ph:
            W = self.sb(ph, "wzb", [128, 8, 1024], BF16)
            WB = Buf()
            self.load_w(W, WB, d["w_in_%d" % l][:, OFF_ZB:OFF_ZB + 1024], nsplit=2)
            wsT = self.sb(ph, "wsT", [128, 4, 128], BF16)
            cB = Buf()
            P.dma("pool", wsT[:], d["sgwT_%d" % l].rearrange("g q p -> q g p"), writes=[cB])
            bsbc = self.sb(ph, "bsbc", [128, 512])
            lng = self.sb(ph, "lng", [128, 512])
            lnb = self.sb(ph, "lnb", [128, 512])
            P.dma("sp", bsbc[:], d["sgb_%d" % l].partition_broadcast(128), writes=[cB])
            P.dma("sp", lng[:], d["sglng_%d" % l].partition_broadcast(128), writes=[cB])
            P.dma("sp", lnb[:], d["sglnb_%d" % l].partition_broadcast(128), writes=[cB])
            ms = self.merge_setup(ph, l, 1, 128)
            uT = [self.sb(ph, "uT%d" % i, [128, 512]) for i in range(2)]
            uB = [Buf() for _ in range(2)]
            vs = [self.sb(ph, "vs%d" % i, [128, 512]) for i in range(2)]
            vB = [Buf() for _ in range(2)]
            vvb = [self.sb(ph, "vvb%d" % i, [128, 512], BF16) for i in range(2)]
            vvB = [Buf() for _ in range(2)]
            junk = self.sb(ph, "sgjunk", [128, 512])
            jB = Buf()
            st = self.sb(ph, "sgst", [128, 8])
            stB = Buf()
            ybT = [self.sb(ph, "ybT%d" % i, [128, 4, 512], BF16) for i in range(2)]
            ybB = [Buf() for _ in range(2)]
            blocks = [(0, 4), (4, 4), (8, 4), (12, 4)] + ([] if last else [(16, 2), (18, 2)])
            for bi, (ch0, nch) in enumerate(blocks):
                yb, yB = ybT[bi % 2], ybB[bi % 2]
                for cj in range(nch):
                    ch = ch0 + cj
                    i = ch % 2
                    tok0 = ch * 128
                    bu, buB = self.bank[0 + i], self.bankB[0 + i]
                    bv, bvB = self.bank[2 + i], self.bankB[2 + i]
                    bs, bsB = self.bank[4 + i], self.bankB[4 + i]
                    for g in range(4):
                        rd = [WB, self.xnTB[ch]]
                        for k in range(8):
                            self.mm(bu[:, g * 128:(g + 1) * 128], W[:, k, g * 128:(g + 1) * 128], self.xnT[:, k, tok0:tok0 + 128],
                                    k == 0, k == 7, rd, [buB], signal=(k == 7 and g == 3))
                    self.act(uT[i][:], bu[:], AF.Gelu_apprx_tanh, [buB], [uB[i]])
                    self.proj_tm(bv, bvB, W, WB, 512, 512, tok0)
                    self.memset("pool", st[:], 0.0, [stB])
                    self.act(vs[i][:], bv[:], AF.Gelu_apprx_tanh, [bvB, stB], [vB[i], stB], accum_out=st[:, 0:1])
                    self.ts("dve", st[:, 1:2], st[:, 0:1], -1.0 / 512, None, ALU.mult, None, [stB], [stB])
                    self.ts("dve", vs[i][:], vs[i][:], st[:, 1:2], None, ALU.add, None, [vB[i], stB], [vB[i]])
                    self.act(junk[:], vs[i][:], AF.Square, [vB[i], stB], [jB, stB], accum_out=st[:, 2:3])
                    self.rsqrt_inplace(st[:, 2:3], 1.0 / 512, EPS, stB)
                    self.stt("dve", vs[i][:], vs[i][:], st[:, 2:3], lng[:], ALU.mult, ALU.mult, [vB[i], stB, cB], [vB[i]])
                    self.tt("pool", vvb[i][:], vs[i][:], lnb[:], ALU.add, [vB[i], cB], [vvB[i]])
                    for g in range(4):
                        self.mm(bs[:, g * 128:(g + 1) * 128], vvb[i][:, g * 128:(g + 1) * 128], wsT[:, g, :], True, True,
                                [vvB[i], cB], [bsB], signal=(g == 3))
                    self.tt("dve", vs[i][:], bs[:], bsbc[:], ALU.add, [bsB, cB, vB[i]], [vB[i]])
                    self.tt("pool", yb[:, :, cj * 128:(cj + 1) * 128], vs[i][:].rearrange("p (g t) -> p g t", g=4),
                            uT[i][:].rearrange("p (g t) -> p g t", g=4), ALU.mult, [vB[i], uB[i]], [yB])
                self.merge(ms, lambda j, s0, n, yb=yb: yb[:, j, s0:s0 + n], [yB], ch0 * 128, nch * 128, True)
            P.barrier()

    def mixer_a(self, l, last, gath, gathB):
        P, d = self.P, self.d
        lam_init = 0.8 - 0.6 * math.exp(-0.3 * l)
        with ExitStack() as ph:
            W = self.sb(ph, "wqa", [128, 8, 512], BF16)
            WB = Buf()
            self.load_w(W, WB, d["w_in_%d" % l][:, OFF_QA:OFF_QA + 512])
            rs = self.rope_state(ph)
            ms = self.merge_setup(ph, l, 0, 128)
            lamt = self.sb(ph, "lamt", [128, 256])
            lamp = self.sb(ph, "lamp", [128, 128])
            lams = self.sb(ph, "lams", [128, 8])
            lB = Buf()
            P.dma("sp", lamt[:], d["dalam_%d" % l].partition_broadcast(128), writes=[lB])
            P.dma("sp", lams[:, 4:5], d["dasub_%d" % l], writes=[lB])
            self.tt("dve", lamp[:].rearrange("p (m d) -> p m d", m=2), lamt[:].rearrange("p (m k d) -> p m k d", m=2, k=2)[:, :, 0, :],
                    lamt[:].rearrange("p (m k d) -> p m k d", m=2, k=2)[:, :, 1, :], ALU.mult, [lB], [lB])
            P.op("dve", lambda e: e.reduce_sum(out=lams[:, 0:2], in_=lamp[:].rearrange("p (m d) -> p m d", m=2), axis=AX.X),
                 reads=[lB], writes=[lB])
            self.act(lams[:, 0:2], lams[:, 0:2], AF.Exp, [lB], [lB])
            self.tt("dve", lams[:, 2:3], lams[:, 1:2], lams[:, 0:1], ALU.subtract, [lB], [lB])
            self.ts("dve", lams[:, 3:4], lams[:, 2:3], -lam_init, None, ALU.add, None, [lB], [lB])
            self.ts("dve", lams[:, 5:6], lams[:, 4:5], 1.0 - lam_init, None, ALU.mult, None, [lB], [lB])
            neglam, gsc = lams[:, 3:4], lams[:, 5:6]
            KT = self.sb(ph, "KT", [128, NCTX + SEQ], BF16)
            KTB = Buf()
            V = self.sb(ph, "Vh", [128, 66, 128], BF16)
            VB = Buf()
            QhT = self.sb(ph, "QhT", [128, TL + NCTX], BF16)
            QB = Buf()
            yaT = self.sb(ph, "yaT", [128, 4, TL + NCTX], BF16)
            yaB = Buf()
            NE = 6
            E = [self.sb(ph, "E%d" % i, [128, 512], BF16) for i in range(NE)]
            EB = [Buf() for _ in range(NE)]
            accD = self.sb(ph, "accD", [128, 2, 512])
            accDB = [Buf() for _ in range(2)]
            r1 = self.sb(ph, "fr1", [128, 512])
            r2 = self.sb(ph, "fr2", [128, 512])
            o = self.sb(ph, "fo", [128, 512])
            sq = self.sb(ph, "fsq", [128, 512])
            fB = Buf()
            for b in range(2):
                for h in range(4):
                    P.dma("sp", KT[:, 0:NCTX], self.ckaT[b][h * 128:(h + 1) * 128, :], reads=[self.ckvB], writes=[KTB])
                    P.dma("sp", V[:, 0:2, :], self.cva[b][:, h * 128:(h + 1) * 128].rearrange("(t p) e -> p t e", p=128),
                          reads=[self.ckvB], writes=[VB])
                    for r in range(NCORES):
                        P.dma("sp", KT[:, NCTX + r * TL:NCTX + (r + 1) * TL], self.sec(gath, r, b, SEC_KA + h * 128, 128),
                              reads=[gathB], writes=[KTB])
                        vsec = self.sec(gath, r, b, SEC_VA, 512).rearrange("r c -> (r c)").rearrange("(h t e) -> h t e", h=4, t=TL)
                        P.dma("sp", V[:, 2 + r * 8:2 + (r + 1) * 8, :], vsec[h].rearrange("(p j) e -> p j e", j=8),
                              reads=[gathB], writes=[VB])
                    for half in range(2):
                        bk, bB = self.bank[half], self.bankB[half]
                        self.proj_fm(bk, bB, W, WB, h * 128, b * TL + half * 512, 512)
                        self.rope_evac(rs, bk, bB, self.bank[2 + half], self.bankB[2 + half], QhT[:, half * 512:(half + 1) * 512], QB,
                                       half * 512, 512)
                    if not last:
                        self.proj_fm(self.bank[0], self.bankB[0], W, WB, h * 128, NLAT + b * NCTX, NCTX)
                        self.cp("act", QhT[:, TL:TL + NCTX], self.bank[0][:, 0:NCTX], [self.bankB[0]], [QB])
                    lat_tiles = [(t, slice(t * 128, (t + 1) * 128)) for t in range(2)] + [
                        (2 + r * 8 + j, slice(NCTX + r * TL + j, NCTX + (r + 1) * TL, 8)) for r in range(NCORES) for j in range(8)]
                    qblocks = [(0, 512, lat_tiles), (512, 512, lat_tiles)] + ([] if last else [(TL, NCTX, lat_tiles[0:2])])
                    for (q0, n, tiles) in qblocks:
                        O1, O2, D1, D2 = self.bank[4], self.bank[5], self.bank[6], self.bank[7]
                        O1B, O2B, D1B, D2B = self.bankB[4], self.bankB[5], self.bankB[6], self.bankB[7]
                        nt = len(tiles)
                        def emit_s(ti):
                            vt, ksl = tiles[ti]
                            for m in range(2):
                                si = (2 * ti + m) % 4
                                ei = (2 * ti + m) % NE
                                S, SB = self.bank[si], self.bankB[si]
                                self.mm(S[:, 0:n], KT[m * 64:(m + 1) * 64, ksl], QhT[m * 64:(m + 1) * 64, q0:q0 + n], True, True,
                                        [KTB, QB], [SB])
                                self.act(E[ei][:, 0:n], S[:, 0:n], AF.Exp, [SB], [EB[ei]], scale=0.125)

                        def emit_pv(ti):
                            vt, ksl = tiles[ti]
                            for m in range(2):
                                ei = (2 * ti + m) % NE
                                Om, OmB = (O1, O1B) if m == 0 else (O2, O2B)
                                self.mm(Om[:, 0:n], V[:, vt, :], E[ei][:, 0:n], ti == 0, ti == nt - 1, [VB, EB[ei]], [OmB])
                                if m == 0:
                                    self.mm(D1[:, 0:n], self.ones_b[:], E[ei][:, 0:n], ti == 0, ti == nt - 1, [self.cB, EB[ei]], [D1B])
                                else:
                                    a = ti % 2
                                    eng = "dve" if a == 0 else "pool"
                                    if ti < 2:
                                        self.cp(eng, accD[:, a, 0:n], E[ei][:, 0:n], [EB[ei]], [accDB[a]])
                                    else:
                                        self.tt(eng, accD[:, a, 0:n], accD[:, a, 0:n], E[ei][:, 0:n], ALU.add, [EB[ei], accDB[a]], [accDB[a]])

                        emit_s(0)
                        for ti in range(nt):
                            if ti + 1 < nt:
                                emit_s(ti + 1)
                            emit_pv(ti)
                        self.mm(D2[:, 0:n], self.ones_f[:], accD[:, 0, 0:n], True, False, [self.cB, accDB[0]], [D2B], signal=False)
                        self.mm(D2[:, 0:n], self.ones_f[:], accD[:, 1, 0:n], False, True, [self.cB, accDB[1]], [D2B])
                        self.recip(r1[:, 0:n], D1[:, 0:n], [D1B], [fB])
                        self.recip(r2[:, 0:n], D2[:, 0:n], [D2B], [fB])
                        self.tt("dve", o[:, 0:n], O1[:, 0:n], r1[:, 0:n], ALU.mult, [O1B, fB], [fB])
                        self.tt("dve", r2[:, 0:n], O2[:, 0:n], r2[:, 0:n], ALU.mult, [O2B, fB], [fB])
                        self.stt("dve", o[:, 0:n], r2[:, 0:n], neglam, o[:, 0:n], ALU.mult, ALU.add, [fB, lB], [fB])
                        self.act(sq[:, 0:n], o[:, 0:n], AF.Square, [fB], [fB])
                        self.mm(D1[:, 0:n], self.ones_f[:], sq[:, 0:n], True, True, [self.cB, fB], [D1B])
                        self.ts("dve", r1[:, 0:n], D1[:, 0:n], 1.0 / 128, EPS, ALU.mult, ALU.add, [D1B, fB], [fB])
                        self.act(r1[:, 0:n], r1[:, 0:n], AF.Sqrt, [fB], [fB])
                        self.recip(r1[:, 0:n], r1[:, 0:n], [fB], [fB])
                        self.stt("dve", yaT[:, h, q0:q0 + n], o[:, 0:n], gsc, r1[:, 0:n], ALU.mult, ALU.mult, [fB, lB], [yaB])
                self.merge(ms, lambda j, s0, n: yaT[:, j, s0:s0 + n], [yaB], b * TL, TL, False, banks=(0, 1))
                if not last:
                    self.merge(ms, lambda j, s0, n: yaT[:, j, TL + s0:TL + s0 + n], [yaB], NLAT + b * NCTX, NCTX, False, banks=(0, 1))
            P.barrier()

    def mixer_c(self, l, last, gath, gathB):
        P, d = self.P, self.d
        with ExitStack() as ph:
            W = self.sb(ph, "wqc", [128, 8, 512], BF16)
            WB = Buf()
            self.load_w(W, WB, d["w_in_%d" % l][:, OFF_QC:OFF_QC + 512])
            ms = self.merge_setup(ph, l, 2, 64)
            selm = self.sb(ph, "selm", [128, 16, 128], BF16)
            selB = Buf()
            for j in range(16):
                self.ts("dve", selm[:, j, :], self.ident_b[:], self.selw[:, j:j + 1], None, ALU.mult, None, [self.cB], [selB])
            KCx = self.sb(ph, "KCx", [128, 4, 24 * 64], BF16)
            KCB = Buf()
            VCx = self.sb(ph, "VCx", [128, 12, 512], BF16)
            VCB = Buf()
            KCc = self.sb(ph, "KCc", [128, 4, NCTX], BF16)
            VCc = self.sb(ph, "VCc", [128, 2, 512], BF16)
            ccB = Buf()
            QcT = self.sb(ph, "QcT", [128, 4, TL + NCTX], BF16)
            QB = Buf()
            ycT = [self.sb(ph, "ycT%d" % i, [64, 8, 512], BF16) for i in range(1)]
            ycB = [Buf() for _ in range(1)]
            E = [self.sb(ph, "Ec%d" % i, [128, 512], BF16) for i in range(4)]
            EB = [Buf() for _ in range(4)]
            NBT = 8
            bt = [self.sb(ph, "nabt%d" % i, [128, 512], BF16) for i in range(NBT)]
            btB = [Buf() for _ in range(NBT)]
            nqb = 2
            bias_uses = [(g_, hd_, i_) for b_ in range(2) for g_ in range(nqb) for hd_ in range(8) for i_ in range(8)]
            bias_state = {"k": 0}

            def bias_prefetch(k):
                if k < len(bias_uses):
                    g_, hd_, i_ = bias_uses[k]
                    P.dma("pool", bt[k % NBT][:], d["nabias_%d" % l][g_, hd_, i_], writes=[btB[k % NBT]])

            for k in range(NBT):
                bias_prefetch(k)
            rr = [self.sb(ph, "ncr%d" % i, [64, 512]) for i in range(2)]
            rrB = [Buf() for _ in range(2)]
            cand = [self.sb(ph, "cand%d" % i, [128, 8, 512], BF16) for i in range(1)]
            candB = [Buf() for _ in range(1)]
            yi = 0
            for b in range(2):
                P.dma("sp", KCx[:, :, 256:256 + TL], self.kcT[b].rearrange("(c p) t -> p c t", p=128), reads=[self.kvownB], writes=[KCB])
                P.dma("sp", VCx[:, 2:10, :], self.vc[b].rearrange("(t p) e -> p t e", p=128), reads=[self.kvownB], writes=[VCB])
                P.dma("sp", KCc[:], self.ckcT[b].rearrange("(c p) t -> p c t", p=128), reads=[self.ckvB], writes=[ccB])
                P.dma("sp", VCc[:], self.cvc[b].rearrange("(t p) e -> p t e", p=128), reads=[self.ckvB], writes=[ccB])
                ci = 0
                for c in range(4):
                    cd, cdB = cand[0], candB[0]
                    ci += 1
                    for r in range(NCORES):
                        hsec = self.sec(gath, r, b, SEC_KCH, 256).rearrange("a (two t) -> (a two) t", two=2)
                        P.dma("sp", cd[:, r, :], hsec[c * 128:(c + 1) * 128, :], reads=[gathB], writes=[cdB])
                    for side in range(2):
                        bk, bB = self.bank[side], self.bankB[side]
                        for r in range(NCORES):
                            src = cd[:, r, 256:512] if side == 0 else cd[:, r, 0:256]
                            self.mm(bk[:, 0:256], selm[:, side * 8 + r, :], src, r == 0, r == NCORES - 1, [selB, cdB], [bB])
                        dst = KCx[:, c, 0:256] if side == 0 else KCx[:, c, 256 + TL:512 + TL]
                        self.cp("act", dst, bk[:, 0:256], [bB], [KCB])
                for side in range(2):
                    for a in range(2):
                        cd, cdB = cand[0], candB[0]
                        ci += 1
                        for r in range(NCORES):
                            vsec = self.sec(gath, r, b, SEC_VCH, 256).rearrange("a (two e) -> (a two) e", two=2)
                            t0 = (256 + a * 128) if side == 0 else a * 128
                            P.dma("sp", cd[:, r, :], vsec[t0:t0 + 128, :], reads=[gathB], writes=[cdB])
                        bk, bB = self.bank[2 + a], self.bankB[2 + a]
                        for r in range(NCORES):
                            self.mm(bk[:, :], selm[:, side * 8 + r, :], cd[:, r, :], r == 0, r == NCORES - 1, [selB, cdB], [bB])
                        self.cp("dve", VCx[:, (0 if side == 0 else 10) + a, :], bk[:, :], [bB], [VCB])
                for c in range(4):
                    for half in range(2):
                        bk, bB = self.bank[(2 * c + half) % 4], self.bankB[(2 * c + half) % 4]
                        self.proj_fm(bk, bB, W, WB, c * 128, b * TL + half * 512, 512)
                        self.act(QcT[:, c, half * 512:(half + 1) * 512], bk[:, :], AF.Copy, [bB], [QB], scale=0.125)
                    if not last:
                        bk, bB = self.bank[c % 4], self.bankB[c % 4]
                        self.proj_fm(bk, bB, W, WB, c * 128, NLAT + b * NCTX, NCTX)
                        self.act(QcT[:, c, TL:TL + NCTX], bk[:, 0:NCTX], AF.Copy, [bB], [QB], scale=0.125)
                ctx_tiles = [("c", t) for t in range(2)]
                qblocks = [(0, 512, [("w", 0 + i) for i in range(8)] + ctx_tiles, 0),
                           (512, 512, [("w", 4 + i) for i in range(8)] + ctx_tiles, 1)]
                if not last:
                    qblocks.append((TL, NCTX, ctx_tiles, None))
                si = 0
                for (q0, n, tiles, g) in qblocks:
                    yc, yB = ycT[0], ycB[0]
                    yi += 1
                    for hd in range(8):
                        c, po = hd // 2, (hd % 2) * 64
                        O, OB = self.bank[4 + 2 * (hd % 2)], self.bankB[4 + 2 * (hd % 2)]
                        Dn, DB = self.bank[5 + 2 * (hd % 2)], self.bankB[5 + 2 * (hd % 2)]
                        nt = len(tiles)
                        def emit_s(ti, sidx):
                            kind, j = tiles[ti]
                            S, SB = self.bank[sidx % 4], self.bankB[sidx % 4]
                            e, eB = E[sidx % 4], EB[sidx % 4]
                            if kind == "w":
                                i = j - 4 * g
                                k = bias_state["k"]
                                assert bias_uses[k] == (g, hd, i)
                                btile, btB_ = bt[k % NBT], btB[k % NBT]
                                self.mm(S[:, 0:n], KCx[po:po + 64, c, j * 128:(j + 1) * 128], QcT[po:po + 64, c, q0:q0 + n], True, False,
                                        [KCB, QB], [SB], signal=False)
                                self.mm(S[:, 0:n], self.ident_b[:], btile[:, 0:n], False, True, [self.cB, btB_], [SB])
                                bias_prefetch(k + NBT)
                                bias_state["k"] = k + 1
                            else:
                                self.mm(S[:, 0:n], KCc[po:po + 64, c, j * 128:(j + 1) * 128], QcT[po:po + 64, c, q0:q0 + n], True, True,
                                        [ccB, QB], [SB])
                            self.act(e[:, 0:n], S[:, 0:n], AF.Exp, [SB], [eB])

                        def emit_pv(ti, sidx):
                            kind, j = tiles[ti]
                            e, eB = E[sidx % 4], EB[sidx % 4]
                            if kind == "w":
                                vap, vrd = VCx[:, j, hd * 64:(hd + 1) * 64], VCB
                            else:
                                vap, vrd = VCc[:, j, hd * 64:(hd + 1) * 64], ccB
                            self.mm(O[0:64, 0:n], vap, e[:, 0:n], ti == 0, ti == nt - 1, [vrd, eB], [OB])
                            self.mm(Dn[0:64, 0:n], self.ones_b[:, 0:64], e[:, 0:n], ti == 0, ti == nt - 1, [self.cB, eB], [DB])

                        emit_s(0, si)
                        for ti in range(nt):
                            if ti + 1 < nt:
                                emit_s(ti + 1, si + ti + 1)
                            emit_pv(ti, si + ti)
                        si += nt
                        r_, rB_ = rr[hd % 2], rrB[hd % 2]
                        self.recip(r_[:, 0:n], Dn[0:64, 0:n], [DB], [rB_])
                        self.tt("dve", yc[:, hd, 0:n], O[0:64, 0:n], r_[:, 0:n], ALU.mult, [OB, rB_], [yB])
                    tok0 = b * TL + q0 if g is not None else NLAT + b * NCTX
                    self.merge(ms, lambda j, s0, n_, yc=yc: yc[0:64, j, s0:s0 + n_], [yB], tok0, n, False, banks=(0, 1))
            P.barrier()

    def gate_bcast(self, ph, l, c0, v, dst, dstB):
        self.bcast_rows(ph, lambda c: self.modT[l][:, c0 + c, v:v + 1], 8, dst, dstB, [self.modTB[l]])

    def out_proj(self, l, last):
        P, d = self.P, self.d
        ntile = (NLAT if last else NTOK) // 128
        with ExitStack() as ph:
            W = self.sb(ph, "wout", [128, 8, D], BF16)
            WB = Buf()
            self.load_w(W, WB, d["w_out_%d" % l][:, :], nsplit=2)
            gbc = self.sb(ph, "gbc", [128, D])
            gB = Buf()
            ht = [self.sb(ph, "oht%d" % i, [128, D]) for i in range(2)]
            htB = [Buf() for _ in range(2)]
            hn = [self.sb(ph, "ohn%d" % i, [128, D]) for i in range(2)]
            hnB = [Buf() for _ in range(2)]
            curv = -1
            for t in range(ntile):
                v = self.variant(t)
                if v != curv:
                    self.gate_bcast(ph, l, 16, v, gbc, gB)
                    curv = v
                i = t % 2
                P.dma("sp", ht[i][:], self.hbuf[t * 128:(t + 1) * 128, :], reads=[self.hB[t]], writes=[htB[i]])
                for half in range(2):
                    bk, bB = self.bank[2 + 2 * i + half], self.bankB[2 + 2 * i + half]
                    for fc in range(8):
                        self.mm(bk[:, :], self.mT[:, fc, t * 128:(t + 1) * 128], W[:, fc, half * 512:(half + 1) * 512], fc == 0, fc == 7,
                                [self.mTB[t], WB], [bB])
                    self.tt("dve", hn[i][:, half * 512:(half + 1) * 512], bk[:, :], gbc[:, half * 512:(half + 1) * 512], ALU.mult,
                            [bB, gB], [hnB[i]])
                self.tt("pool", hn[i][:], hn[i][:], ht[i][:], ALU.add, [hnB[i], htB[i]], [hnB[i]])
                P.dma("sp", self.hbuf[t * 128:(t + 1) * 128, :], hn[i][:], reads=[hnB[i]], writes=[self.hB[t]])
            P.barrier()

    def phase_mix(self, l, last, gath, gathB):
        with ExitStack() as ph:
            self.mT = self.sb(ph, "mT", [128, 8, NTOK], BF16)
            self.mTB = [Buf("mT%d" % i) for i in range(NTOK // 128)]
            self.mixer_b(l, last)
            self.mixer_a(l, last, gath, gathB)
            self.mixer_c(l, last, gath, gathB)
            self.out_proj(l, last)

    def phase_moe_dense(self, l, last):
        P, d = self.P, self.d
        ntok = NLAT if last else NTOK
        ntile = ntok // 128
        with ExitStack() as ph:
            wts = self.sb(ph, "wts", [128, NTOK // 128, NEXP])
            wtsB = Buf()
            rstate = {}

            def r_begin(ph2):
                rstate["wrT"] = self.sb(ph2, "wrT", [128, 8, 36])
                rstate["br"] = self.sb(ph2, "brbc", [128, 36])
                rstate["B"] = Buf()
                P.dma("sp", rstate["wrT"][:], d["wr_%d" % l].rearrange("(k p) n -> p k n", p=128), writes=[rstate["B"]])
                P.dma("sp", rstate["br"][:], d["br_%d" % l].partition_broadcast(128), writes=[rstate["B"]])
                rstate["lg"] = self.sb(ph2, "rlg", [128, 36])
                rstate["em"] = self.sb(ph2, "rem", [128, 32])
                rstate["oh"] = self.sb(ph2, "roh", [128, 32])
                rstate["sm"] = self.sb(ph2, "rsm", [128, 16])
                rstate["ge"] = self.sb(ph2, "rge", [128, 4])
                rstate["tB"] = Buf()

            def r_tile(t, half, tmp, tmpB):
                L, LB = self.bank[2], self.bankB[2]
                for c in range(4):
                    self.mm(L[:, 0:36], tmp[:, c, :], rstate["wrT"][:, half * 4 + c, :], half == 0 and c == 0, half == 1 and c == 3,
                            [tmpB, rstate["B"]], [LB], signal=(c == 3))
                if half == 0:
                    return
                lg, em, oh, sm, ge, tB = rstate["lg"], rstate["em"], rstate["oh"], rstate["sm"], rstate["ge"], rstate["tB"]
                rB = [tB]
                self.tt("dve", lg[:], L[:, 0:36], rstate["br"][:], ALU.add, [LB, rstate["B"]], rB)
                P.op("dve", lambda e: e.reduce_max(out=sm[:, 0:1], in_=lg[:, 0:4], axis=AX.X), reads=rB, writes=rB)
                self.ts("dve", oh[:, 0:4], lg[:, 0:4], sm[:, 0:1], None, ALU.is_equal, None, rB, rB)
                self.ts("dve", sm[:, 1:2], sm[:, 0:1], -1.0, None, ALU.mult, None, rB, rB)
                self.memset("dve", sm[:, 2:3], 0.0, rB)
                self.act(ge[:], lg[:, 0:4], AF.Exp, rB, rB, bias=sm[:, 1:2], accum_out=sm[:, 2:3])
                self.recip(sm[:, 3:4], sm[:, 2:3], rB, rB)
                self.ts("dve", oh[:, 4:8], oh[:, 0:4], 1e30, -1e30, ALU.mult, ALU.add, rB, rB)
                self.tt("dve", em[:].rearrange("p (g e) -> p g e", g=4), lg[:, 4:36].rearrange("p (g e) -> p g e", g=4),
                        oh[:, 4:8].unsqueeze(2).to_broadcast([128, 4, 8]), ALU.add, rB, rB)
                P.op("dve", lambda e: e.reduce_max(out=sm[:, 4:5], in_=em[:], axis=AX.X), reads=rB, writes=rB)
                self.ts("dve", oh[:], em[:], sm[:, 4:5], None, ALU.is_equal, None, rB, rB)
                self.stt("dve", em[:], oh[:], -1e30, em[:], ALU.mult, ALU.add, rB, rB)
                P.op("dve", lambda e: e.reduce_max(out=sm[:, 5:6], in_=em[:], axis=AX.X), reads=rB, writes=rB)
                self.tt("dve", sm[:, 6:7], sm[:, 4:5], sm[:, 5:6], ALU.subtract, rB, rB)
                self.act(sm[:, 6:7], sm[:, 6:7], AF.Sigmoid, rB, rB)
                self.tt("dve", sm[:, 7:8], sm[:, 6:7], sm[:, 3:4], ALU.mult, rB, rB)
                self.tt("dve", sm[:, 8:9], sm[:, 3:4], sm[:, 7:8], ALU.subtract, rB, rB)
                self.ts("dve", wts[:, t, :], oh[:], sm[:, 7:8], None, ALU.mult, None, rB, [wtsB])
                self.ts("dve", oh[:], em[:], sm[:, 5:6], None, ALU.is_equal, None, rB, rB)
                self.stt("dve", wts[:, t, :], oh[:], sm[:, 8:9], wts[:, t, :], ALU.mult, ALU.add, rB + [wtsB], [wtsB])

            self.phase_norm(l, 2, ntok, route={"begin": r_begin, "tile": r_tile})

            acc = self.sb(ph, "acc", [128, ntile, D])
            accB = [Buf() for _ in range(ntile)]
            for t in range(ntile):
                self.memset("pool", acc[:, t, :], 0.0, [accB[t]])
            wg = [self.sb(ph, "wg%d" % i, [128, 8, DEXP], BF16) for i in range(2)]
            wu = [self.sb(ph, "wu%d" % i, [128, 8, DEXP], BF16) for i in range(2)]
            wd = [self.sb(ph, "wd%d" % i, [128, 4, D], BF16) for i in range(2)]
            wB = [Buf() for _ in range(2)]
            sgb = [self.sb(ph, "msg%d" % i, [128, 512], BF16) for i in range(2)]
            sgB = [Buf() for _ in range(2)]
            hdT = self.sb(ph, "hdT", [128, 4, 512], BF16)
            hdB = [Buf() for _ in range(4)]

            def load_expert(e):
                i = e % 2
                self.load_w(wg[i], wB[i], d["wg_%d" % l][e])
                self.load_w(wu[i], wB[i], d["wu_%d" % l][e])
                self.load_w(wd[i], wB[i], d["wd_%d" % l][e])

            load_expert(0)
            bi = 0
            for e in range(NEXP):
                if e + 1 < NEXP:
                    load_expert(e + 1)
                i = e % 2
                for tb in range(ntok // 512):
                    tok0 = tb * 512
                    for fcb in range(4):
                        G, GB = self.bank[2 * (fcb % 2)], self.bankB[2 * (fcb % 2)]
                        U, UB = self.bank[2 * (fcb % 2) + 1], self.bankB[2 * (fcb % 2) + 1]
                        self.proj_fm(G, GB, wg[i], wB[i], fcb * 128, tok0, 512)
                        self.proj_fm(U, UB, wu[i], wB[i], fcb * 128, tok0, 512)
                        sg_, sgB_ = sgb[fcb % 2], sgB[fcb % 2]
                        self.act(sg_[:], G[:, :], AF.Silu, [GB], [sgB_])
                        self.tt("dve", hdT[:, fcb, :], U[:, :], sg_[:], ALU.mult, [UB, sgB_], [hdB[fcb]])
                    for tt_ in range(4):
                        t = tb * 4 + tt_
                        for half in range(2):
                            Y, YB = self.bank[4 + bi % 4], self.bankB[4 + bi % 4]
                            bi += 1
                            for fcb in range(4):
                                self.mm(Y[:, :], hdT[:, fcb, tt_ * 128:(tt_ + 1) * 128], wd[i][:, fcb, half * 512:(half + 1) * 512],
                                        fcb == 0, fcb == 3, [hdB[fcb], wB[i]], [YB])
                            self.stt("dve", acc[:, t, half * 512:(half + 1) * 512], Y[:, :], wts[:, t, e:e + 1],
                                     acc[:, t, half * 512:(half + 1) * 512], ALU.mult, ALU.add, [YB, wtsB, accB[t]], [accB[t]])
            gbc = self.sb(ph, "gbc2", [128, D])
            gB = Buf()
            ht = [self.sb(ph, "mht%d" % i, [128, D]) for i in range(2)]
            htB = [Buf() for _ in range(2)]
            curv = -1
            for t in range(ntile):
                v = self.variant(t)
                if v != curv:
                    self.gate_bcast(ph, l, 40, v, gbc, gB)
                    curv = v
                i = t % 2
                P.dma("sp", ht[i][:], self.hbuf[t * 128:(t + 1) * 128, :], reads=[self.hB[t]], writes=[htB[i]])
                self.tt("pool", acc[:, t, :], acc[:, t, :], gbc[:], ALU.mult, [accB[t], gB], [accB[t]])
                self.tt("dve", acc[:, t, :], acc[:, t, :], ht[i][:], ALU.add, [accB[t], htB[i]], [accB[t]])
                P.dma("sp", self.hbuf[t * 128:(t + 1) * 128, :], acc[:, t, :], reads=[accB[t], htB[i]], writes=[self.hB[t]])
            P.barrier()

    def phase_moe(self, l, last):
        P, d = self.P, self.d
        ntok = NLAT if last else NTOK
        ntile = ntok // 128
        C = MOE_CAP
        NSLOT = NEXP * C
        I32 = mybir.dt.int32
        xs = self.dscr("xs%d" % l, [NSLOT, D], BF16)
        ys = self.dscr("ys%d" % l, [NSLOT, D], F32)
        scaleT = self.s2T[l]
        mB = self.modTB[l]
        with ExitStack() as ph:
            didx = self.sb(ph, "didx", [128, NTOK // 128, 2], I32)
            wts2 = self.sb(ph, "wts2", [128, NTOK // 128, 2])
            rtB = Buf()
            with ExitStack() as p1:
                tri = self.sb(p1, "tri", [128, 128], BF16)
                ecb = self.sb(p1, "ecb", [128, NEXP])
                wrT = self.sb(p1, "wrT", [128, 8, 36])
                brb = self.sb(p1, "brbc", [128, 36])
                kB = Buf()
                P.dma("pool", tri[:], d["tri"], writes=[kB])
                P.dma("sp", ecb[:], d["ecb"], writes=[kB])
                P.dma("sp", wrT[:], d["wr_%d" % l].rearrange("(k p) n -> p k n", p=128), writes=[kB])
                P.dma("sp", brb[:], d["br_%d" % l].partition_broadcast(128), writes=[kB])
                base = self.sb(p1, "rbase", [128, NEXP])
                baseB = Buf()
                self.memset("dve", base[:], 0.0, [baseB])
                sbc = self.sb(p1, "sbc", [128, D])
                shbc = self.sb(p1, "shbc", [128, D])
                bcB = Buf()
                ht = [self.sb(p1, "ht%d" % i, [128, D]) for i in range(2)]
                htB = [Buf() for _ in range(2)]
                xf = [self.sb(p1, "xf%d" % i, [128, D]) for i in range(2)]
                xfB = [Buf() for _ in range(2)]
                xtm = [self.sb(p1, "xtm%d" % i, [128, D], BF16) for i in range(3)]
                xtmB = [Buf() for _ in range(3)]
                xT = [self.sb(p1, "xT%d" % i, [128, 4, 128]) for i in range(2)]
                xTB = [Buf() for _ in range(2)]
                junk = self.sb(p1, "junk", [128, D])
                junkB = Buf()
                ss = self.sb(p1, "ss", [128, 32])
                ssB = Buf()
                lg = self.sb(p1, "rlg", [128, 36])
                em = self.sb(p1, "rem", [128, 32])
                oh1 = self.sb(p1, "roh1", [128, 32])
                oh2 = self.sb(p1, "roh2", [128, 32])
                ohg = self.sb(p1, "rohg", [128, 8])
                Ab = self.sb(p1, "rAb", [128, 32], BF16)
                slot = self.sb(p1, "rslot", [128, 32])
                tmp32 = self.sb(p1, "rtmp32", [128, 32])
                sm = self.sb(p1, "rsm", [128, 16])
                ge = self.sb(p1, "rge", [128, 4])
                tB = Buf()
                rB = [tB]
                NT = ntile
                xall = self.sb(p1, "xall", [128, NT, D], BF16)
                xallB = [Buf() for _ in range(NT)]
                lgall = self.sb(p1, "lgall", [128, NT, 36])
                emA = self.sb(p1, "emA", [128, NT, 32])
                o1A = self.sb(p1, "o1A", [128, NT, 32])
                o2A = self.sb(p1, "o2A", [128, NT, 32])
                slA = self.sb(p1, "slA", [128, NT, 32])
                bsA = self.sb(p1, "bsA", [128, NT, 32])
                AbA = self.sb(p1, "AbA", [128, NT, 32], BF16)
                g4 = self.sb(p1, "g4", [128, NT, 4])
                p4 = self.sb(p1, "p4", [128, NT, 4])
                sv = self.sb(p1, "sv", [128, 12, NT])
                self.memset("dve", ss[:], 0.0, [ssB])
                for t in range(ntile):
                    i = t % 2
                    P.dma("sp", ht[i][:], self.hbuf[t * 128:(t + 1) * 128, :], reads=[self.hB[t]], writes=[htB[i]])
                    self.act(junk[:], ht[i][:], AF.Square, [htB[i], ssB], [junkB, ssB], accum_out=ss[:, t:t + 1])
                self.rsqrt_inplace(ss[:, 0:ntile], 1.0 / D, EPS, ssB)
                curv = -1
                for t in range(ntile):
                    i = t % 2
                    v = self.variant(t)
                    if v != curv:
                        self.bcast_rows(p1, lambda c, v=v: scaleT[:, c, v:v + 1], 8, sbc, bcB, [mB])
                        self.bcast_rows(p1, lambda c, v=v: self.modT[l][:, 24 + c, v:v + 1], 8, shbc, bcB, [mB])
                        curv = v
                    P.dma("sp", ht[i][:], self.hbuf[t * 128:(t + 1) * 128, :], reads=[self.hB[t]], writes=[htB[i]])
                    self.act(xf[i][:], ht[i][:], AF.Copy, [htB[i], ssB], [xfB[i]], scale=ss[:, t:t + 1])
                    self.tt("dve", xf[i][:], xf[i][:], sbc[:], ALU.mult, [xfB[i], bcB], [xfB[i]])
                    self.tt("pool", xf[i][:], xf[i][:], shbc[:], ALU.add, [xfB[i], bcB], [xfB[i]])
                    self.cp("pool", xall[:, t, :], xf[i][:], [xfB[i]], [xallB[t]])
                    L, LB = self.bank[2 + t % 2], self.bankB[2 + t % 2]
                    for half in range(2):
                        bk, bB = self.bank[half], self.bankB[half]
                        for c in range(4):
                            cc = half * 4 + c
                            self.tr(bk[:, c * 128:(c + 1) * 128], xf[i][:, cc * 128:(cc + 1) * 128], self.ident[:], [xfB[i], self.cB], [bB],
                                    signal=(c == 3))
                        self.cp("act", xT[half][:], bk[:].rearrange("p (c t) -> p c t", c=4), [bB], [xTB[half]])
                        for c in range(4):
                            self.mm(L[:, 0:36], xT[half][:, c, :], wrT[:, half * 4 + c, :], half == 0 and c == 0, half == 1 and c == 3,
                                    [xTB[half], kB], [LB], signal=(c == 3))
                    self.tt("dve", lgall[:, t, :], L[:, 0:36], brb[:], ALU.add, [LB, kB], rB)
                G = lgall[:, :, 0:4]

                def bc(ap2, n):
                    return ap2.unsqueeze(2).to_broadcast([128, NT, n])

                def rmax(out, in_):
                    P.op("dve", lambda e: e.reduce_max(out=out, in_=in_, axis=AX.X), reads=rB, writes=rB)

                def rsum(out, in_):
                    P.op("dve", lambda e: e.reduce_sum(out=out, in_=in_, axis=AX.X), reads=rB, writes=rB)

                gmax, gsum, gw, m1, m2, dl, w1, d1, d2 = (sv[:, j, :] for j in range(9))
                rmax(gmax, G)
                self.tt("dve", p4[:], G, bc(gmax, 4), ALU.is_equal, rB, rB)
                self.tt("dve", g4[:], G, bc(gmax, 4), ALU.subtract, rB, rB)
                self.act(g4[:], g4[:], AF.Exp, rB, rB)
                rsum(gsum, g4[:])
                self.recip(gw, gsum, rB, rB)
                self.ts("dve", p4[:], p4[:], 1e30, -1e30, ALU.mult, ALU.add, rB, rB)
                self.cp("dve", emA[:], lgall[:, :, 4:36], rB, rB)
                self.tt("dve", emA[:].rearrange("p t (g e) -> p (t g) e", g=4), emA[:].rearrange("p t (g e) -> p (t g) e", g=4),
                        p4[:].rearrange("p t g -> p (t g)").unsqueeze(2).to_broadcast([128, NT * 4, 8]), ALU.add, rB, rB)
                rmax(m1, emA[:])
                self.tt("dve", o1A[:], emA[:], bc(m1, 32), ALU.is_equal, rB, rB)
                self.stt("dve", emA[:], o1A[:], -1e30, emA[:], ALU.mult, ALU.add, rB, rB)
                rmax(m2, emA[:])
                self.tt("dve", o2A[:], emA[:], bc(m2, 32), ALU.is_equal, rB, rB)
                self.tt("dve", dl, m1, m2, ALU.subtract, rB, rB)
                self.act(dl, dl, AF.Sigmoid, rB, rB)
                self.tt("dve", wts2[:, 0:NT, 0], dl, gw, ALU.mult, rB, [rtB])
                self.tt("dve", wts2[:, 0:NT, 1], gw, wts2[:, 0:NT, 0], ALU.subtract, rB + [rtB], [rtB])
                self.tt("dve", AbA[:], o1A[:], o2A[:], ALU.add, rB, rB)
                RK = [(self.bank[4], self.bankB[4]), (self.bank[5], self.bankB[5])]
                TO = [(self.bank[6], self.bankB[6]), (self.bank[7], self.bankB[7])]
                for t in range(NT):
                    (R, RB_), (T_, TB_) = RK[t // 16], TO[t // 16]
                    c0 = (t % 16) * 32
                    self.mm(R[:, c0:c0 + 32], tri[:], AbA[:, t, :], True, True, [kB, tB], [RB_], signal=(t % 16 == 15 or t == NT - 1))
                    self.mm(T_[:, c0:c0 + 32], self.ones_b[:], AbA[:, t, :], True, True, [self.cB, tB], [TB_],
                            signal=(t % 16 == 15 or t == NT - 1))
                for j in range((NT + 15) // 16):
                    n_ = min(16, NT - 16 * j)
                    self.cp("dve", slA[:, 16 * j:16 * j + n_, :], RK[j][0][:, 0:n_ * 32].rearrange("p (t e) -> p t e", e=32), [RK[j][1]], rB)
                    self.cp("dve", emA[:, 16 * j:16 * j + n_, :], TO[j][0][:, 0:n_ * 32].rearrange("p (t e) -> p t e", e=32), [TO[j][1]], rB)
                self.memset("dve", bsA[:, 0, :], 0.0, rB)
                for t in range(1, NT):
                    self.tt("dve", bsA[:, t, :], bsA[:, t - 1, :], emA[:, t - 1, :], ALU.add, rB, rB)
                self.tt("dve", slA[:], slA[:], bsA[:], ALU.add, rB, rB)
                self.ts("dve", emA[:], slA[:], float(C), 1e7, ALU.is_ge, ALU.mult, rB, rB)
                self.tt("dve", slA[:], slA[:], emA[:], ALU.add, rB, rB)
                self.tt("dve", slA[:], slA[:], ecb[:].unsqueeze(1).to_broadcast([128, NT, 32]), ALU.add, rB + [kB], rB)
                self.tt("dve", emA[:], slA[:], o1A[:], ALU.mult, rB, rB)
                rsum(d1, emA[:])
                self.tt("dve", emA[:], slA[:], o2A[:], ALU.mult, rB, rB)
                rsum(d2, emA[:])
                self.cp("dve", didx[:, 0:NT, 0], d1, rB, [rtB])
                self.cp("dve", didx[:, 0:NT, 1], d2, rB + [rtB], [rtB])
                for t in range(NT):
                    for k in range(2):
                        self.P.indirect("scatter", xs, didx[:, t, k:k + 1], xall[:, t, :], NSLOT - 1, reads=[xallB[t], rtB])
                P.barrier()
            with ExitStack() as p2:
                wg = [self.sb(p2, "wg%d" % i, [128, 8, DEXP], BF16) for i in range(2)]
                wu = [self.sb(p2, "wu%d" % i, [128, 8, DEXP], BF16) for i in range(2)]
                wd = [self.sb(p2, "wd%d" % i, [128, 4, D], BF16) for i in range(2)]
                wB = [Buf() for _ in range(2)]
                xe = [self.sb(p2, "xe%d" % i, [128, C // 128, D], BF16) for i in range(2)]
                xeB = [Buf() for _ in range(2)]
                XeT = [self.sb(p2, "XeT%d" % i, [128, 8, C], BF16) for i in range(2)]
                XeTB = [Buf() for _ in range(2)]
                sgb = [self.sb(p2, "msg%d" % i, [128, C], BF16) for i in range(2)]
                sgB = [Buf() for _ in range(2)]
                hdT = self.sb(p2, "hdT", [128, 4, C], BF16)
                hdB = [Buf() for _ in range(4)]
                yo = [self.sb(p2, "yo%d" % i, [128, D]) for i in range(2)]
                yoB = [Buf() for _ in range(2)]
                ysB = Buf()

                def load_expert(e):
                    i = e % 2
                    self.load_w(wg[i], wB[i], d["wg_%d" % l][e])
                    self.load_w(wu[i], wB[i], d["wu_%d" % l][e])
                    self.load_w(wd[i], wB[i], d["wd_%d" % l][e])
                    P.dma("sp", xe[i][:], xs[e * C:(e + 1) * C, :].rearrange("(s p) f -> p s f", p=128), writes=[xeB[i]])

                load_expert(0)
                bi = 0
                yi = 0
                for e in range(NEXP):
                    if e + 1 < NEXP:
                        load_expert(e + 1)
                    i = e % 2
                    for fc in range(8):
                        bk, bB = self.bank[fc % 2], self.bankB[fc % 2]
                        bkb = bk[:].bitcast(BF16)
                        for st in range(C // 128):
                            self.tr(bkb[:, st * 128:(st + 1) * 128], xe[i][:, st, fc * 128:(fc + 1) * 128], self.ident_b[:], [xeB[i], self.cB], [bB],
                                    signal=(st == C // 128 - 1))
                        self.cp("act" if fc % 2 == 0 else "dve", XeT[i][:, fc, :], bkb[:, 0:C], [bB], [XeTB[i]])
                    for fcb in range(4):
                        G, GB = self.bank[2 + 2 * (fcb % 2)], self.bankB[2 + 2 * (fcb % 2)]
                        U, UB = self.bank[3 + 2 * (fcb % 2)], self.bankB[3 + 2 * (fcb % 2)]
                        for k in range(8):
                            self.mm(G[:, 0:C], wg[i][:, k, fcb * 128:(fcb + 1) * 128], XeT[i][:, k, :], k == 0, k == 7, [wB[i], XeTB[i]], [GB])
                        for k in range(8):
                            self.mm(U[:, 0:C], wu[i][:, k, fcb * 128:(fcb + 1) * 128], XeT[i][:, k, :], k == 0, k == 7, [wB[i], XeTB[i]], [UB])
                        sg_, sgB_ = sgb[fcb % 2], sgB[fcb % 2]
                        self.act(sg_[:], G[:, 0:C], AF.Silu, [GB], [sgB_])
                        self.tt("dve", hdT[:, fcb, :], U[:, 0:C], sg_[:], ALU.mult, [UB, sgB_], [hdB[fcb]])
                    for st in range(C // 128):
                        y_, yB_ = yo[yi % 2], yoB[yi % 2]
                        yi += 1
                        for half in range(2):
                            Y, YB = self.bank[6 + bi % 2], self.bankB[6 + bi % 2]
                            bi += 1
                            for fcb in range(4):
                                self.mm(Y[:, :], hdT[:, fcb, st * 128:(st + 1) * 128], wd[i][:, fcb, half * 512:(half + 1) * 512],
                                        fcb == 0, fcb == 3, [hdB[fcb], wB[i]], [YB])
                            self.cp("act" if half == 0 else "dve", y_[:, half * 512:(half + 1) * 512], Y[:, :], [YB], [yB_])
                        P.dma("sp", ys[e * C + st * 128:e * C + (st + 1) * 128, :], y_[:], reads=[yB_], writes=[ysB])
                P.barrier()
            with ExitStack() as p3:
                gbc = self.sb(p3, "gbc2", [128, D])
                gB = Buf()
                ht = [self.sb(p3, "mht%d" % i, [128, D]) for i in range(2)]
                htB = [Buf() for _ in range(2)]
                g1 = [self.sb(p3, "g1_%d" % i, [128, D]) for i in range(2)]
                g2 = [self.sb(p3, "g2_%d" % i, [128, D]) for i in range(2)]
                gtB = [Buf() for _ in range(2)]
                curv = -1
                for t in range(ntile):
                    v = self.variant(t)
                    if v != curv:
                        self.gate_bcast(p3, l, 40, v, gbc, gB)
                        curv = v
                    i = t % 2
                    P.dma("sp", ht[i][:], self.hbuf[t * 128:(t + 1) * 128, :], reads=[self.hB[t]], writes=[htB[i]])
                    self.memset("pool", g1[i][:], 0.0, [gtB[i]])
                    self.memset("pool", g2[i][:], 0.0, [gtB[i]])
                    self.P.indirect("gather", ys, didx[:, t, 0:1], g1[i][:], NSLOT - 1, reads=[rtB], writes=[gtB[i]])
                    self.P.indirect("gather", ys, didx[:, t, 1:2], g2[i][:], NSLOT - 1, reads=[rtB], writes=[gtB[i]])
                    self.ts("dve", g1[i][:], g1[i][:], wts2[:, t, 0:1], None, ALU.mult, None, [gtB[i], rtB], [gtB[i]])
                    self.stt("dve", g1[i][:], g2[i][:], wts2[:, t, 1:2], g1[i][:], ALU.mult, ALU.add, [gtB[i], rtB], [gtB[i]])
                    self.tt("pool", g1[i][:], g1[i][:], gbc[:], ALU.mult, [gtB[i], gB], [gtB[i]])
                    self.tt("dve", g1[i][:], g1[i][:], ht[i][:], ALU.add, [gtB[i], htB[i]], [gtB[i]])
                    P.dma("sp", self.hbuf[t * 128:(t + 1) * 128, :], g1[i][:], reads=[gtB[i], htB[i]], writes=[self.hB[t]])
                P.barrier()

    def phase_final(self, out):
        P, d = self.P, self.d
        with ExitStack() as ph:
            gbc = self.sb(ph, "gfin", [128, D])
            gB = Buf()
            P.dma("sp", gbc[:], d["gfinal"].partition_broadcast(128), writes=[gB])
            ht = [self.sb(ph, "fht%d" % i, [128, D]) for i in range(2)]
            htB = [Buf() for _ in range(2)]
            junk = self.sb(ph, "fjunk", [128, D])
            jB = Buf()
            ss = self.sb(ph, "fss", [128, 16])
            ssB = Buf()
            oB = Buf("out")
            self.memset("dve", ss[:], 0.0, [ssB])
            for t in range(NLAT // 128):
                i = t % 2
                P.dma("sp", ht[i][:], self.hbuf[t * 128:(t + 1) * 128, :], reads=[self.hB[t]], writes=[htB[i]])
                self.act(junk[:], ht[i][:], AF.Square, [htB[i], ssB], [jB, ssB], accum_out=ss[:, t:t + 1])
            self.rsqrt_inplace(ss[:, 0:NLAT // 128], 1.0 / D, EPS, ssB)
            for t in range(NLAT // 128):
                i = t % 2
                P.dma("sp", ht[i][:], self.hbuf[t * 128:(t + 1) * 128, :], reads=[self.hB[t]], writes=[htB[i]])
                self.stt("dve", ht[i][:], ht[i][:], ss[:, t:t + 1], gbc[:], ALU.mult, ALU.mult, [htB[i], ssB, gB], [htB[i]])
                P.dma("sp", out[t * 128:(t + 1) * 128, :], ht[i][:], reads=[htB[i]], writes=[oB])
            P.barrier()

    def build(self):
        nc = self.nc
        L = self.launch
        with ExitStack() as es:
            self.P = Prog(nc, es)
            layers = {"A": [0], "B": [0, 1], "C": [1], "F": [0, 1]}[L]
            self.need = {"A": set(), "B": {(0, "full")}, "C": {(1, "full")}, "F": {(0, "full"), (1, "full")}}[L]
            self.setup(es, layers)
            self.alloc_kv_scratch()
            P = self.P
            h_in = self.din("h_in", [NTOK, D])
            for t in range(NTOK // 128):
                P.dma("sp", self.hbuf[t * 128:(t + 1) * 128, :], h_in[t * 128:(t + 1) * 128, :], writes=[self.hB[t]])
            stop = self.dbg.get("stop")
            if L == "A":
                send = self.dout("send", [SEND_ROWS, TL], BF16)
                self.phase_cond(0)
                self.phase_norm(0, 1, NTOK)
                self.phase_kv(0, send, Buf("send"))
            elif L in ("B", "C"):
                l = 0 if L == "B" else 1
                last = l == 1
                gath = self.din("gath", [NCORES * SEND_ROWS, TL], BF16)
                gathB = Buf("gath")
                dummy = self.dscr("send_dummy", [SEND_ROWS, TL], BF16)
                self.phase_cond(l)
                self.phase_norm(l, 1, NTOK)
                self.phase_kv(l, dummy, Buf("dummy"))
                if stop != "kv":
                    self.phase_mix(l, last, gath, gathB)
                if stop not in ("kv", "mix"):
                    self.phase_moe(l, last)
                if L == "B":
                    if stop is None:
                        send = self.dout("send", [SEND_ROWS, TL], BF16)
                        self.phase_cond(1)
                        self.phase_norm(1, 1, NTOK)
                        self.phase_kv(1, send, Buf("send"))
                    h_out = self.dout("h_out", [NTOK, D])
                    for t in range(NTOK // 128):
                        P.dma("sp", h_out[t * 128:(t + 1) * 128, :], self.hbuf[t * 128:(t + 1) * 128, :], reads=[self.hB[t]])
                else:
                    self.d["gfinal"] = self.din("gfinal", [1, D])
                    out = self.dout("out", [NLAT, D])
                    self.phase_final(out)
            elif L == "F":
                self.d["gfinal"] = self.din("gfinal", [1, D])
                out = self.dout("out", [NLAT, D])
                for l in (0, 1):
                    last = l == 1
                    send = self.dscr("send%d" % l, [SEND_ROWS, TL], BF16)
                    gath = self.dscr("gath%d" % l, [NCORES * SEND_ROWS, TL], BF16)
                    sendB, gathB = Buf("send"), Buf("gath")
                    self.phase_cond(l)
                    self.phase_norm(l, 1, NTOK)
                    self.phase_kv(l, send, sendB)
                    P.collective(send, gath, [sendB], [gathB])
                    self.phase_mix(l, last, gath, gathB)
                    self.phase_moe(l, last)
                self.phase_final(out)
            P.finish()
        return nc


def _tlayout(v):
    v = np.asarray(v, np.float32)
    return np.ascontiguousarray(v.reshape(-1, 128).T)


def _rope_tables(core):
    t = np.arange(TL) + core * TL
    row = (t // GRID_W).astype(np.float32)
    col = (t % GRID_W).astype(np.float32)
    inv = (10000.0 ** (-np.arange(16, dtype=np.float32) / 16)).astype(np.float32)
    ar = row[:, None] * inv
    ac = col[:, None] * inv
    ang = np.concatenate([ar, ar, ac, ac], axis=-1)
    cos = np.cos(ang).astype(np.float32)
    sin = np.sin(ang).astype(np.float32)
    sgn = np.concatenate([-np.ones(16), np.ones(16), -np.ones(16), np.ones(16)]).astype(np.float32)
    cosT = np.concatenate([cos.T, cos.T], axis=0)
    sinT = np.concatenate([(sin * sgn).T, (sin * sgn).T], axis=0)
    return np.ascontiguousarray(cosT), np.ascontiguousarray(sinT)


def _perm_matrix():
    pm = np.zeros((128, 128), np.float32)
    for m in range(128):
        base, dd = (m // 64) * 64, m % 64
        seg, off = dd // 16, dd % 16
        partner = base + (seg ^ 1) * 16 + off
        pm[partner, m] = 1.0
    return pm


def _common_inputs(inp, core):
    c, c_ctx = inp["c"], inp["c_ctx"]
    condT = np.stack([_tlayout(c[0]), _tlayout(c[1]), _tlayout(c_ctx)], axis=-1)
    cosT, sinT = _rope_tables(core)
    selw = np.zeros((128, 16), np.float32)
    if core > 0:
        selw[:, core - 1] = 1.0
    if core < NCORES - 1:
        selw[:, 8 + core + 1] = 1.0
    return {"condT": np.ascontiguousarray(condT), "ident": np.eye(128, dtype=np.float32), "pm": _perm_matrix(),
            "ropec": cosT, "ropes": sinT, "selw": selw,
            "tri": np.triu(np.ones((128, 128), np.float32), 1),
            "ecb": np.ascontiguousarray(np.broadcast_to(np.arange(NEXP, dtype=np.float32) * MOE_CAP, (128, NEXP)))}


def _nabias(rpb, core):
    g = np.arange(2)[:, None, None, None]
    i = np.arange(8)[None, :, None, None]
    a = np.arange(2)[None, None, :, None]
    jq = np.arange(8)[None, None, None, :]
    kr = 16 * core + 8 * g - 4 + 2 * i + a
    qr = 16 * core + 8 * g + jq
    rs = np.clip(qr - 4, 0, 128 - 8)
    vr = (kr >= 0) & (kr < 128) & (kr >= rs) & (kr < rs + 8)
    dri = np.clip(kr - qr + 7, 0, 14)
    kc = np.arange(64)[:, None]
    qc = np.arange(64)[None, :]
    cs = np.clip(qc - 8, 0, 64 - 16)
    vc = (kc >= cs) & (kc < cs + 16)
    dci = np.clip(kc - qc + 15, 0, 30)
    vals = rpb[:, dri[..., None, None], dci[None, None, None, None]]
    ok = vr[..., None, None] & vc[None, None, None, None]
    vals = np.where(ok[None], vals, np.float32(-1e30)).astype(np.float32)
    vals = vals.transpose(1, 0, 2, 3, 5, 4, 6)
    return np.ascontiguousarray(vals.reshape(2, 8, 8, 128, 512))


def _layer_inputs(inp, l, core, full=False):
    out = {
        "w_ada_%d" % l: inp["w_ada"][l], "b_adaT_%d" % l: _tlayout(inp["b_ada"][l]),
        "gmixT_%d" % l: _tlayout(inp["g_norm_mix"][l]), "gffnT_%d" % l: _tlayout(inp["g_norm_ffn"][l]),
        "w_in_%d" % l: inp["w_in"][l],
    }
    if full:
        out.update({
            "dalam_%d" % l: inp["da_lambda"][l].reshape(1, 256), "dasub_%d" % l: inp["da_subln_g"][l].reshape(128, 1),
            "sglng_%d" % l: inp["sg_ln_g"][l].reshape(1, 512), "sglnb_%d" % l: inp["sg_ln_b"][l].reshape(1, 512),
            "sgwT_%d" % l: np.ascontiguousarray(inp["sg_w"][l].transpose(0, 2, 1)), "sgb_%d" % l: inp["sg_b"][l].reshape(1, 512),
            "nabias_%d" % l: _nabias(inp["na_rpb"][l], core),
            "w_branch_%d" % l: inp["w_branch"][l], "w_out_%d" % l: inp["w_out"][l],
            "wr_%d" % l: np.ascontiguousarray(np.concatenate([inp["moe_w_group"][l], inp["moe_w_router"][l]], axis=1)),
            "br_%d" % l: np.concatenate([inp["moe_b_group"][l], inp["moe_b_router"][l]]).reshape(1, 36),
            "wg_%d" % l: inp["moe_w_gate"][l], "wu_%d" % l: inp["moe_w_up"][l], "wd_%d" % l: inp["moe_w_down"][l],
        })
    return out


def _h0(inp, core):
    x, ctx = inp["x"], inp["ctx"]
    return np.ascontiguousarray(np.concatenate([x[0, core * TL:(core + 1) * TL], x[1, core * TL:(core + 1) * TL], ctx[0], ctx[1]],
                                               axis=0).astype(np.float32))


def _run(kb, maps):
    nc = kb.build()
    in_maps = [{k: np.ascontiguousarray(m[k]) for k in kb.in_names} for m in maps]
    res = run_bass_kernel_spmd(nc, in_maps, core_ids=list(range(NCORES)))
    return res.results


FUSED = True


def _assemble(res):
    out = np.empty((2, SEQ, D), np.float32)
    for c in range(NCORES):
        o = np.asarray(res[c]["out"], np.float32)
        out[0, c * TL:(c + 1) * TL] = o[0:TL]
        out[1, c * TL:(c + 1) * TL] = o[TL:2 * TL]
    return out


def kernel(**inputs):
    inp = {k: np.asarray(v) for k, v in inputs.items()}
    common = [_common_inputs(inp, c) for c in range(NCORES)]
    if FUSED:
        kb = KB("F")
        maps = []
        for c in range(NCORES):
            m = dict(common[c])
            m.update(_layer_inputs(inp, 0, c, full=True))
            m.update(_layer_inputs(inp, 1, c, full=True))
            m["h_in"] = _h0(inp, c)
            m["gfinal"] = inp["g_final"].reshape(1, D)
            maps.append(m)
        return _assemble(_run(kb, maps))
    kbA = KB("A")
    maps = []
    for c in range(NCORES):
        m = dict(common[c])
        m.update(_layer_inputs(inp, 0, c))
        m["h_in"] = _h0(inp, c)
        maps.append(m)
    resA = _run(kbA, maps)
    gath0 = np.concatenate([resA[c]["send"] for c in range(NCORES)], axis=0)
    kbB = KB("B")
    for c in range(NCORES):
        maps[c].update(_layer_inputs(inp, 0, c, full=True))
        maps[c].update(_layer_inputs(inp, 1, c))
        maps[c]["gath"] = gath0
    resB = _run(kbB, maps)
    gath1 = np.concatenate([resB[c]["send"] for c in range(NCORES)], axis=0)
    kbC = KB("C")
    maps2 = []
    for c in range(NCORES):
        m = dict(common[c])
        m.update(_layer_inputs(inp, 1, c, full=True))
        m["h_in"] = resB[c]["h_out"]
        m["gath"] = gath1
        m["gfinal"] = inp["g_final"].reshape(1, D)
        maps2.append(m)
    return _assemble(_run(kbC, maps2))
```

```python
import math
from contextlib import ExitStack

import numpy as np
import concourse.bass as bass
import concourse.mybir as mybir
from concourse.bass_utils import run_bass_kernel_spmd

F32 = mybir.dt.float32
BF16 = mybir.dt.bfloat16
AF = mybir.ActivationFunctionType
ALU = mybir.AluOpType
AX = mybir.AxisListType

NCORES = 8
D = 1024
SEQ = 8192
NCTX = 256
GRID_W = 64
TL = 1024
NLAT = 2 * TL
NTOK = NLAT + 2 * NCTX
EPS = 1e-6
OFF_KA, OFF_VA, OFF_KC, OFF_VC, OFF_QA, OFF_QC, OFF_ZB, OFF_GATE = 0, 512, 1024, 1536, 2048, 2560, 3072, 4096
IN_COLS = 7168
NEXP = 32
DEXP = 512
MOE_CAP = 512
SEC_KA, SEC_VA, SEC_KCH, SEC_VCH, SEC_B = 0, 512, 1024, 1280, 1536
SEND_ROWS = 2 * SEC_B


class Buf:
    __slots__ = ("name", "w", "r")

    def __init__(self, name=""):
        self.name = name
        self.w = None
        self.r = {}


class Prog:
    def __init__(self, nc, es, n_dma_sems=(28, 16)):
        self.nc = nc
        self.engs = {"pe": nc.tensor, "act": nc.scalar, "dve": nc.vector, "pool": nc.gpsimd, "sp": nc.sync}
        self.semobj = {}
        self.cnt = {}
        for e in ["pe", "act", "dve", "pool"]:
            self.semobj[e] = es.enter_context(nc.semaphore("s_" + e))
            self.cnt[e] = 0
        self.dpool = {"sp": [], "pool": []}
        for q, n in zip(["sp", "pool"], n_dma_sems):
            for i in range(n):
                k = "d_%s_%d" % (q, i)
                self.semobj[k] = es.enter_context(nc.semaphore(k))
                self.cnt[k] = 0
                self.dpool[q].append(k)
        self.semobj["cc"] = es.enter_context(nc.semaphore("s_cc"))
        self.cnt["cc"] = 0
        self.drr = {"sp": 0, "pool": 0}
        self.waited = {e: {} for e in self.engs}
        self.nins = 0

    def _collect(self, reads, writes):
        need = {}

        def add(k, v):
            if need.get(k, 0) < v:
                need[k] = v

        for b in reads:
            if b.w is not None:
                add(*b.w)
        for b in writes:
            if b.w is not None:
                add(*b.w)
            for k, v in b.r.items():
                add(k, v)
        return need

    def _emit_waits(self, eng, need):
        w = self.waited[eng]
        e = self.engs[eng]
        for k, v in need.items():
            if eng == "pe" and k == "pe":
                continue
            if w.get(k, 0) < v:
                e.wait_ge(self.semobj[k], v)
                w[k] = v
                self.nins += 1

    def _update(self, tok, reads, writes):
        k, v = tok
        for b in reads:
            if b.r.get(k, 0) < v:
                b.r[k] = v
        for b in writes:
            b.w = tok
            b.r = {}

    def op(self, eng, fn, reads=(), writes=(), signal=True):
        self._emit_waits(eng, self._collect(reads, writes))
        ins = fn(self.engs[eng])
        self.nins += 1
        if signal:
            ins.then_inc(self.semobj[eng], 1)
            self.cnt[eng] += 1
            tok = (eng, self.cnt[eng])
        else:
            tok = (eng, self.cnt[eng] + 1)
        self._update(tok, reads, writes)
        return tok

    def dma(self, q, out, in_, reads=(), writes=(), **kw):
        need = self._collect(reads, writes)
        pool = self.dpool[q]
        k = pool[self.drr[q] % len(pool)]
        self.drr[q] += 1
        if self.cnt[k] > 0 and need.get(k, 0) < self.cnt[k]:
            need[k] = self.cnt[k]
        self._emit_waits(q, need)
        ins = self.engs[q].dma_start(out=out, in_=in_, **kw)
        self.nins += 1
        ins.then_inc(self.semobj[k], 16)
        self.cnt[k] += 16
        tok = (k, self.cnt[k])
        self._update(tok, reads, writes)
        return tok

    def indirect(self, kind, dram, idx, sb_ap, bound, reads=(), writes=()):
        q = "pool"
        need = self._collect(reads, writes)
        pool = self.dpool[q]
        k = pool[self.drr[q] % len(pool)]
        self.drr[q] += 1
        if self.cnt[k] > 0 and need.get(k, 0) < self.cnt[k]:
            need[k] = self.cnt[k]
        self._emit_waits(q, need)
        off = bass.IndirectOffsetOnAxis(ap=idx, axis=0)
        if not hasattr(self, "_bregs"):
            self._bregs = {}
        if bound not in self._bregs:
            self._bregs[bound] = self.nc.gpsimd.to_reg(bound)
        bound = self._bregs[bound]
        if kind == "scatter":
            ins = self.nc.gpsimd.indirect_dma_start(out=dram[:, :], out_offset=off, in_=sb_ap, in_offset=None, bounds_check=bound,
                                                    oob_is_err=False)
        else:
            ins = self.nc.gpsimd.indirect_dma_start(out=sb_ap, out_offset=None, in_=dram[:, :], in_offset=off, bounds_check=bound,
                                                    oob_is_err=False)
        self.nins += 1
        ins.then_inc(self.semobj[k], 16)
        self.cnt[k] += 16
        tok = (k, self.cnt[k])
        self._update(tok, reads, writes)
        return tok

    def collective(self, in_ap, out_ap, reads=(), writes=()):
        self._emit_waits("pool", self._collect(reads, writes))
        ins = self.nc.gpsimd.collective_compute("AllGather", ALU.bypass, replica_groups=[list(range(NCORES))],
                                                ins=[in_ap.opt()], outs=[out_ap.opt()])
        self.nins += 1
        ins.then_inc(self.semobj["cc"], 1)
        self.cnt["cc"] += 1
        tok = ("cc", self.cnt["cc"])
        self._update(tok, reads, writes)
        return tok

    def all_counts(self):
        return {k: v for k, v in self.cnt.items() if v > 0}

    def barrier(self):
        need = self.all_counts()
        for e in self.engs:
            self._emit_waits(e, need)

    def finish(self):
        self._emit_waits("sp", self.all_counts())


class KB:
    def __init__(self, launch, dbg=None):
        self.launch = launch
        self.dbg = dbg or {}
        self.nc = bass.Bass("TRN2", target_bir_lowering=False)
        self.in_names = []
        self.out_names = []

    def din(self, name, shape, dt=F32):
        self.in_names.append(name)
        return self.nc.dram_tensor(name, list(shape), dt, kind="ExternalInput").ap()

    def dout(self, name, shape, dt=F32):
        self.out_names.append(name)
        return self.nc.dram_tensor(name, list(shape), dt, kind="ExternalOutput").ap()

    def dscr(self, name, shape, dt=F32):
        return self.nc.dram_tensor(name, list(shape), dt).ap()

    def sb(self, es, name, shape, dt=F32):
        self._uid = getattr(self, "_uid", 0) + 1
        return es.enter_context(self.nc.sbuf_tensor("sb%d_%s" % (self._uid, name), list(shape), dt))

    def mm(self, out, lhsT, rhs, start, stop, rd, wr, signal=None):
        if signal is None:
            signal = stop
        return self.P.op("pe", lambda e: e.matmul(out, lhsT=lhsT, rhs=rhs, start=start, stop=stop), reads=rd, writes=wr,
                         signal=signal)

    def tr(self, out, in_, ident, rd, wr, signal=True):
        return self.P.op("pe", lambda e: e.transpose(out=out, in_=in_, identity=ident), reads=rd, writes=wr, signal=signal)

    def act(self, out, in_, func, rd, wr, bias=None, scale=None, accum_out=None):
        kw = {}
        if bias is not None:
            kw["bias"] = bias
        if scale is not None:
            kw["scale"] = scale
        if accum_out is not None:
            kw["accum_out"] = accum_out
        return self.P.op("act", lambda e: e.activation(out=out, in_=in_, func=func, **kw), reads=rd, writes=wr)

    def tt(self, eng, out, in0, in1, op, rd, wr):
        return self.P.op(eng, lambda e: e.tensor_tensor(out=out, in0=in0, in1=in1, op=op), reads=rd, writes=wr)

    def ts(self, eng, out, in0, s1, s2, op0, op1, rd, wr):
        if op1 is None:
            return self.P.op(eng, lambda e: e.tensor_scalar(out=out, in0=in0, scalar1=s1, scalar2=None, op0=op0), reads=rd,
                             writes=wr)
        return self.P.op(eng, lambda e: e.tensor_scalar(out=out, in0=in0, scalar1=s1, scalar2=s2, op0=op0, op1=op1),
                         reads=rd, writes=wr)

    def stt(self, eng, out, in0, scalar, in1, op0, op1, rd, wr):
        return self.P.op(eng, lambda e: e.scalar_tensor_tensor(out=out, in0=in0, scalar=scalar, in1=in1, op0=op0, op1=op1),
                         reads=rd, writes=wr)

    def cp(self, eng, out, in_, rd, wr):
        if eng == "act":
            return self.P.op("act", lambda e: e.copy(out=out, in_=in_), reads=rd, writes=wr)
        return self.P.op(eng, lambda e: e.tensor_copy(out=out, in_=in_), reads=rd, writes=wr)

    def recip(self, out, in_, rd, wr):
        return self.P.op("dve", lambda e: e.reciprocal(out=out, in_=in_), reads=rd, writes=wr)

    def memset(self, eng, ap, val, wr):
        return self.P.op(eng, lambda e: e.memset(ap, val), writes=wr)

    def rsqrt_inplace(self, ap, mult, add, buf):
        self.ts("dve", ap, ap, mult, add, ALU.mult, ALU.add, [buf], [buf])
        self.act(ap, ap, AF.Sqrt, [buf], [buf])
        self.recip(ap, ap, [buf], [buf])

    def setup(self, es, layers):
        nc = self.nc
        d = {}
        d["condT"] = self.din("condT", [128, 8, 3])
        d["ident"] = self.din("ident", [128, 128])
        d["pm"] = self.din("pm", [128, 128])
        d["ropec"] = self.din("ropec", [128, TL])
        d["ropes"] = self.din("ropes", [128, TL])
        d["selw"] = self.din("selw", [128, 16])
        d["tri"] = self.din("tri", [128, 128])
        d["ecb"] = self.din("ecb", [128, NEXP])
        for l in layers:
            d["w_ada_%d" % l] = self.din("w_ada_%d" % l, [D, 6 * D])
            d["b_adaT_%d" % l] = self.din("b_adaT_%d" % l, [128, 48])
            d["gmixT_%d" % l] = self.din("gmixT_%d" % l, [128, 8])
            d["gffnT_%d" % l] = self.din("gffnT_%d" % l, [128, 8])
            d["w_in_%d" % l] = self.din("w_in_%d" % l, [D, IN_COLS])
            if (l, "full") in self.need:
                d["dalam_%d" % l] = self.din("dalam_%d" % l, [1, 256])
                d["dasub_%d" % l] = self.din("dasub_%d" % l, [128, 1])
                d["sglng_%d" % l] = self.din("sglng_%d" % l, [1, 512])
                d["sglnb_%d" % l] = self.din("sglnb_%d" % l, [1, 512])
                d["sgwT_%d" % l] = self.din("sgwT_%d" % l, [4, 128, 128])
                d["sgb_%d" % l] = self.din("sgb_%d" % l, [1, 512])
                d["nabias_%d" % l] = self.din("nabias_%d" % l, [2, 8, 8, 128, 512])
                d["w_branch_%d" % l] = self.din("w_branch_%d" % l, [3, 512, D])
                d["w_out_%d" % l] = self.din("w_out_%d" % l, [D, D])
                d["wr_%d" % l] = self.din("wr_%d" % l, [D, 36])
                d["br_%d" % l] = self.din("br_%d" % l, [1, 36])
                d["wg_%d" % l] = self.din("wg_%d" % l, [NEXP, D, DEXP])
                d["wu_%d" % l] = self.din("wu_%d" % l, [NEXP, D, DEXP])
                d["wd_%d" % l] = self.din("wd_%d" % l, [NEXP, DEXP, D])
        self.d = d
        P = self.P
        self.bank = [es.enter_context(nc.psum_tensor("bank%d" % i, [128, 512], F32)) for i in range(8)]
        self.bankB = [Buf("bank%d" % i) for i in range(8)]
        self.ident = self.sb(es, "ident_f", [128, 128])
        self.pm = self.sb(es, "pm_f", [128, 128])
        self.ones_f = self.sb(es, "ones_f", [128, 128])
        self.ones_b = self.sb(es, "ones_b", [128, 128], BF16)
        self.ident_b = self.sb(es, "ident_b", [128, 128], BF16)
        self.cB = Buf("consts")
        P.dma("sp", self.ident[:], d["ident"], writes=[self.cB])
        P.dma("sp", self.pm[:], d["pm"], writes=[self.cB])
        self.memset("dve", self.ones_f[:], 1.0, [self.cB])
        self.memset("dve", self.ones_b[:], 1.0, [self.cB])
        self.cp("dve", self.ident_b[:], self.ident[:], [self.cB], [self.cB])
        self.selw = self.sb(es, "selw", [128, 16])
        P.dma("sp", self.selw[:], d["selw"], writes=[self.cB])
        self.condS = self.sb(es, "condS", [128, 8, 3])
        self.condSB = Buf("condS")
        P.dma("sp", self.condS[:], d["condT"], writes=[self.condSB])
        self.act(self.condS[:], self.condS[:], AF.Silu, [self.condSB], [self.condSB])
        self.modT = {}
        self.modTB = {}
        self.s1T = {}
        self.s2T = {}
        for l in layers:
            self.modT[l] = self.sb(es, "modT%d" % l, [128, 48, 3])
            self.s1T[l] = self.sb(es, "s1T%d" % l, [128, 8, 3])
            self.s2T[l] = self.sb(es, "s2T%d" % l, [128, 8, 3])
            self.modTB[l] = Buf("modT%d" % l)
        self.xnT = self.sb(es, "xnT", [128, 8, NTOK], BF16)
        self.xnTB = [Buf("xnT%d" % i) for i in range(NTOK // 128)]
        self.hbuf = self.dscr("hbuf", [NTOK, D])
        self.hB = [Buf("h%d" % i) for i in range(NTOK // 128)]

    @staticmethod
    def variant(tile):
        return 0 if tile < 8 else (1 if tile < 16 else 2)

    def phase_cond(self, l):
        P = self.P
        d = self.d
        with ExitStack() as ph:
            wblk = [self.sb(ph, "wada%d" % i, [128, 8, 512]) for i in range(2)]
            wB = [Buf() for _ in range(2)]
            bT = self.sb(ph, "badaT", [128, 48])
            g1 = self.sb(ph, "g1T", [128, 8])
            g2 = self.sb(ph, "g2T", [128, 8])
            sB = Buf()
            P.dma("sp", bT[:], d["b_adaT_%d" % l], writes=[sB])
            P.dma("sp", g1[:], d["gmixT_%d" % l], writes=[sB])
            P.dma("sp", g2[:], d["gffnT_%d" % l], writes=[sB])
            ps, psB = self.bank[0], self.bankB[0]
            for cb in range(12):
                w, B = wblk[cb % 2], wB[cb % 2]
                P.dma("sp", w[:], d["w_ada_%d" % l][:, cb * 512:(cb + 1) * 512].rearrange("(k p) c -> p k c", p=128),
                      writes=[B])
                for j in range(4):
                    cc = cb * 4 + j
                    for k in range(8):
                        self.mm(ps[:, cc * 3:(cc + 1) * 3], w[:, k, j * 128:(j + 1) * 128], self.condS[:, k, :], k == 0, k == 7,
                                [B, self.condSB], [psB])
            modT, mB = self.modT[l], self.modTB[l]
            self.tt("dve", modT[:], ps[:, 0:144].rearrange("p (c v) -> p c v", v=3),
                    bT[:].unsqueeze(2).to_broadcast([128, 48, 3]), ALU.add, [psB, sB], [mB])
            self.stt("dve", self.s1T[l][:], modT[:, 8:16, :], 1.0, g1[:].unsqueeze(2).to_broadcast([128, 8, 3]), ALU.add,
                     ALU.mult, [mB, sB], [mB])
            self.stt("dve", self.s2T[l][:], modT[:, 32:40, :], 1.0, g2[:].unsqueeze(2).to_broadcast([128, 8, 3]), ALU.add,
                     ALU.mult, [mB, sB], [mB])
            P.barrier()

    def phase_norm(self, l, sub, ntok, route=None):
        P = self.P
        ntile = ntok // 128
        scaleT = self.s1T[l] if sub == 1 else self.s2T[l]
        sh0 = 0 if sub == 1 else 24
        mB = self.modTB[l]
        with ExitStack() as ph:
            ht = [self.sb(ph, "ht%d" % i, [128, D]) for i in range(2)]
            htB = [Buf() for _ in range(2)]
            hn = [self.sb(ph, "hn%d" % i, [128, D]) for i in range(2)]
            hnB = [Buf() for _ in range(2)]
            tmp = [self.sb(ph, "ntmp%d" % i, [128, 4, 128]) for i in range(2)]
            tmpB = [Buf() for _ in range(2)]
            junk = self.sb(ph, "junk", [128, D])
            junkB = Buf()
            ss = self.sb(ph, "ss", [128, 32])
            ssB = Buf()
            self.memset("dve", ss[:], 0.0, [ssB])
            for t in range(ntile):
                i = t % 2
                P.dma("sp", ht[i][:], self.hbuf[t * 128:(t + 1) * 128, :], reads=[self.hB[t]], writes=[htB[i]])
                self.act(junk[:], ht[i][:], AF.Square, [htB[i], ssB], [junkB, ssB], accum_out=ss[:, t:t + 1])
            self.rsqrt_inplace(ss[:, 0:ntile], 1.0 / D, EPS, ssB)
            if route is not None:
                route["begin"](ph)
            for t in range(ntile):
                i = t % 2
                v = self.variant(t)
                P.dma("sp", ht[i][:], self.hbuf[t * 128:(t + 1) * 128, :], reads=[self.hB[t]], writes=[htB[i]])
                self.act(hn[i][:], ht[i][:], AF.Copy, [htB[i], ssB], [hnB[i]], scale=ss[:, t:t + 1])
                for half in range(2):
                    bk, bB = self.bank[half], self.bankB[half]
                    for c in range(4):
                        cc = half * 4 + c
                        self.tr(bk[:, c * 128:(c + 1) * 128], hn[i][:, cc * 128:(cc + 1) * 128], self.ident[:], [hnB[i], self.cB],
                                [bB], signal=(c == 3))
                    c0 = half * 4
                    j = half
                    self.tt("dve", tmp[j][:], bk[:].rearrange("p (c t) -> p c t", c=4),
                            scaleT[:, c0:c0 + 4, v].unsqueeze(2).to_broadcast([128, 4, 128]), ALU.mult, [bB, mB], [tmpB[j]])
                    shift = self.modT[l][:, sh0 + c0:sh0 + c0 + 4, v].unsqueeze(2).to_broadcast([128, 4, 128])
                    if route is None:
                        self.tt("pool", self.xnT[:, c0:c0 + 4, t * 128:(t + 1) * 128], tmp[j][:], shift, ALU.add, [tmpB[j], mB],
                                [self.xnTB[t]])
                    else:
                        self.tt("dve", tmp[j][:], tmp[j][:], shift, ALU.add, [tmpB[j], mB], [tmpB[j]])
                        self.cp("pool", self.xnT[:, c0:c0 + 4, t * 128:(t + 1) * 128], tmp[j][:], [tmpB[j]], [self.xnTB[t]])
                        route["tile"](t, half, tmp[j], tmpB[j])
            P.barrier()

    def load_w(self, dst, dstB, src, nsplit=1):
        n = src.shape[1]
        step = n // nsplit
        for s in range(nsplit):
            self.P.dma("pool", dst[:, :, s * step:(s + 1) * step],
                       src[:, s * step:(s + 1) * step].rearrange("(k p) c -> p k c", p=128), writes=[dstB])

    def proj_fm(self, bank, bankB, W, WB, col0, tok0, ntok, ncol=128):
        rd = [WB] + self.xnTB[tok0 // 128:(tok0 + ntok + 127) // 128]
        for k in range(8):
            self.mm(bank[0:ncol, 0:ntok], W[:, k, col0:col0 + ncol], self.xnT[:, k, tok0:tok0 + ntok], k == 0, k == 7, rd, [bankB])

    def proj_tm(self, bank, bankB, W, WB, col0, ncol, tok0):
        rd = [WB, self.xnTB[tok0 // 128]]
        for k in range(8):
            self.mm(bank[:, 0:ncol], self.xnT[:, k, tok0:tok0 + 128], W[:, k, col0:col0 + ncol], k == 0, k == 7, rd, [bankB])

    def rope_evac(self, ph_state, bank, bankB, rbank, rbankB, dst, dstB, pos0, n):
        st = ph_state
        i = st["i"] = (st.get("i", -1) + 1) % 2
        xs, xsB = st["xs"][i], st["xsB"][i]
        t1, t1B = st["t1"][i], st["t1B"][i]
        self.cp("act", xs[:, 0:n], bank[:, 0:n], [bankB], [xsB])
        self.mm(rbank[:, 0:n], self.pm[:], xs[:, 0:n], True, True, [self.cB, xsB], [rbankB])
        self.tt("dve", t1[:, 0:n], xs[:, 0:n], self.ropec[:, pos0:pos0 + n], ALU.mult, [xsB, self.ropeB], [t1B])
        self.tt("dve", xs[:, 0:n], rbank[:, 0:n], self.ropes[:, pos0:pos0 + n], ALU.mult, [rbankB, self.ropeB, xsB], [xsB])
        self.tt("pool", dst, t1[:, 0:n], xs[:, 0:n], ALU.add, [t1B, xsB], [dstB])

    def rope_state(self, ph):
        self.ropec = self.sb(ph, "ropec", [128, TL])
        self.ropes = self.sb(ph, "ropes", [128, TL])
        self.ropeB = Buf("rope")
        self.P.dma("sp", self.ropec[:], self.d["ropec"], writes=[self.ropeB])
        self.P.dma("sp", self.ropes[:], self.d["ropes"], writes=[self.ropeB])
        return {"xs": [self.sb(ph, "rxs%d" % i, [128, 512]) for i in range(2)], "xsB": [Buf() for _ in range(2)],
                "t1": [self.sb(ph, "rt1%d" % i, [128, 512]) for i in range(2)], "t1B": [Buf() for _ in range(2)]}

    def sec(self, buf, rank, b, sec0, nrows):
        r0 = rank * SEND_ROWS + b * SEC_B + sec0
        return buf[r0:r0 + nrows, :]

    def phase_kv(self, l, send, sendB):
        P = self.P
        d = self.d
        with ExitStack() as ph:
            W = self.sb(ph, "wkv", [128, 8, 2048], BF16)
            WB = Buf()
            self.load_w(W, WB, d["w_in_%d" % l][:, 0:2048], nsplit=4)
            rs = self.rope_state(ph)
            st = [self.sb(ph, "kvst%d" % i, [128, 512], BF16) for i in range(4)]
            stB = [Buf() for _ in range(4)]
            si = 0
            for b in range(2):
                tok0 = b * TL
                ctok0 = NLAT + b * NCTX
                for cc in range(4):
                    for half in range(2):
                        bk, bB = self.bank[si % 2], self.bankB[si % 2]
                        s, sB = st[si % 4], stB[si % 4]
                        self.proj_fm(bk, bB, W, WB, OFF_KA + cc * 128, tok0 + half * 512, 512)
                        self.rope_evac(rs, bk, bB, self.bank[2 + si % 2], self.bankB[2 + si % 2], s[:], sB, half * 512, 512)
                        P.dma("sp", self.sec(send, 0, b, SEC_KA + cc * 128, 128)[:, half * 512:(half + 1) * 512], s[:],
                              reads=[sB], writes=[sendB])
                        si += 1
                    bk, bB = self.bank[si % 2], self.bankB[si % 2]
                    s, sB = st[si % 4], stB[si % 4]
                    self.proj_fm(bk, bB, W, WB, OFF_KA + cc * 128, ctok0, NCTX)
                    self.cp("act", s[:, 0:NCTX], bk[:, 0:NCTX], [bB], [sB])
                    P.dma("sp", self.ckaT[b][cc * 128:(cc + 1) * 128, :], s[:, 0:NCTX], reads=[sB], writes=[self.ckvB])
                    si += 1
                for cc in range(4):
                    for half in range(2):
                        bk, bB = self.bank[si % 2], self.bankB[si % 2]
                        s, sB = st[si % 4], stB[si % 4]
                        self.proj_fm(bk, bB, W, WB, OFF_KC + cc * 128, tok0 + half * 512, 512)
                        self.cp("act", s[:], bk[:], [bB], [sB])
                        P.dma("sp", self.kcT[b][cc * 128:(cc + 1) * 128, half * 512:(half + 1) * 512], s[:], reads=[sB],
                              writes=[self.kvownB])
                        hsec = self.sec(send, 0, b, SEC_KCH, 256).rearrange("a (two t) -> (a two) t", two=2)
                        src = s[:, 0:256] if half == 0 else s[:, 256:512]
                        P.dma("sp", hsec[cc * 128:(cc + 1) * 128, half * 256:(half + 1) * 256], src, reads=[sB], writes=[sendB])
                        si += 1
                    bk, bB = self.bank[si % 2], self.bankB[si % 2]
                    s, sB = st[si % 4], stB[si % 4]
                    self.proj_fm(bk, bB, W, WB, OFF_KC + cc * 128, ctok0, NCTX)
                    self.cp("act", s[:, 0:NCTX], bk[:, 0:NCTX], [bB], [sB])
                    P.dma("sp", self.ckcT[b][cc * 128:(cc + 1) * 128, :], s[:, 0:NCTX], reads=[sB], writes=[self.ckvB])
                    si += 1
                vasec = self.sec(send, 0, b, SEC_VA, 512).rearrange("r c -> (r c)").rearrange("(h t e) -> t h e", h=4, t=TL)
                vchsec = self.sec(send, 0, b, SEC_VCH, 256).rearrange("a (two e) -> (a two) e", two=2)
                for t in range(10):
                    lat = t < 8
                    tk = tok0 + t * 128 if lat else ctok0 + (t - 8) * 128
                    for which in range(2):
                        bk, bB = self.bank[si % 2], self.bankB[si % 2]
                        s, sB = st[si % 4], stB[si % 4]
                        self.proj_tm(bk, bB, W, WB, OFF_VA if which == 0 else OFF_VC, 512, tk)
                        self.cp("act" if which == 0 else "dve", s[:], bk[:], [bB], [sB])
                        if which == 0:
                            if lat:
                                P.dma("sp", vasec[t * 128:(t + 1) * 128, :, :], s[:].rearrange("p (h e) -> p h e", h=4),
                                      reads=[sB], writes=[sendB])
                            else:
                                P.dma("sp", self.cva[b][(t - 8) * 128:(t - 7) * 128, :], s[:], reads=[sB], writes=[self.ckvB])
                        else:
                            if lat:
                                P.dma("sp", self.vc[b][t * 128:(t + 1) * 128, :], s[:], reads=[sB], writes=[self.kvownB])
                                if t < 2:
                                    P.dma("sp", vchsec[t * 128:(t + 1) * 128, :], s[:], reads=[sB], writes=[sendB])
                                elif t >= 6:
                                    P.dma("sp", vchsec[(t - 4) * 128:(t - 3) * 128, :], s[:], reads=[sB], writes=[sendB])
                            else:
                                P.dma("sp", self.cvc[b][(t - 8) * 128:(t - 7) * 128, :], s[:], reads=[sB], writes=[self.ckvB])
                        si += 1
            P.barrier()

    def alloc_kv_scratch(self):
        self.ckaT = [self.dscr("ckaT%d" % b, [512, NCTX], BF16) for b in range(2)]
        self.ckcT = [self.dscr("ckcT%d" % b, [512, NCTX], BF16) for b in range(2)]
        self.cva = [self.dscr("cva%d" % b, [NCTX, 512], BF16) for b in range(2)]
        self.cvc = [self.dscr("cvc%d" % b, [NCTX, 512], BF16) for b in range(2)]
        self.kcT = [self.dscr("kcT%d" % b, [512, TL], BF16) for b in range(2)]
        self.vc = [self.dscr("vc%d" % b, [TL, 512], BF16) for b in range(2)]
        self.ckvB = Buf("ckv")
        self.kvownB = Buf("kvown")

    def bcast_rows(self, ph, vecT_fn, nchunk, dst, dstB, rdB):
        tmpd = [self.sb(ph, "bct%d" % i, [128, 128]) for i in range(2)]
        tB = [Buf() for _ in range(2)]
        for c in range(nchunk):
            i = c % 2
            bk, bB = self.bank[(c // 4) % 2], self.bankB[(c // 4) % 2]
            self.ts("dve", tmpd[i][:], self.ident[:], vecT_fn(c), None, ALU.mult, None, [self.cB] + rdB, [tB[i]])
            self.mm(bk[:, (c % 4) * 128:(c % 4 + 1) * 128], self.ones_f[:], tmpd[i][:], True, True, [self.cB, tB[i]], [bB])
            if c % 4 == 3 or c == nchunk - 1:
                c0 = (c // 4) * 4
                n = (c - c0 + 1) * 128
                self.cp("act", dst[:, c0 * 128:c0 * 128 + n], bk[:, 0:n], [bB], [dstB])

    def merge_setup(self, ph, l, i, K):
        nparts = 512 // K
        Wb = self.sb(ph, "wb%d" % i, [K, nparts, D], BF16)
        WbB = Buf()
        self.P.dma("pool", Wb[:], self.d["w_branch_%d" % l][i].rearrange("(j p) c -> p j c", p=K), writes=[WbB])
        Wg = self.sb(ph, "wgate%d" % i, [128, 8, D], BF16)
        WgB = Buf()
        self.load_w(Wg, WgB, self.d["w_in_%d" % l][:, OFF_GATE + i * D:OFF_GATE + (i + 1) * D], nsplit=2)
        sg = [self.sb(ph, "sgt%d_%d" % (i, j), [128, 512]) for j in range(2)]
        sgB = [Buf() for _ in range(2)]
        return {"Wb": Wb, "WbB": WbB, "Wg": Wg, "WgB": WgB, "K": K, "nparts": nparts, "sg": sg, "sgB": sgB, "n": 0}

    def merge(self, ms, yget, yB, tok0, ntok, first, banks=(6, 7)):
        K, nparts = ms["K"], ms["nparts"]
        for s0 in range(0, ntok, 512):
            n = min(512, ntok - s0)
            t0 = tok0 + s0
            mB = self.mTB[t0 // 128:(t0 + n + 127) // 128]
            for fc in range(8):
                bA, bAB = self.bank[banks[0]], self.bankB[banks[0]]
                bG, bGB = self.bank[banks[1]], self.bankB[banks[1]]
                for j in range(nparts):
                    self.mm(bA[:, 0:n], ms["Wb"][0:K, j, fc * 128:(fc + 1) * 128], yget(j, s0, n), j == 0, j == nparts - 1,
                            [ms["WbB"]] + yB, [bAB])
                self.proj_fm(bG, bGB, ms["Wg"], ms["WgB"], fc * 128, t0, n)
                i = ms["n"] = (ms["n"] + 1) % 2
                sg, sgB = ms["sg"][i], ms["sgB"][i]
                self.act(sg[:, 0:n], bG[:, 0:n], AF.Sigmoid, [bGB], [sgB])
                if first:
                    self.tt("dve", self.mT[:, fc, t0:t0 + n], bA[:, 0:n], sg[:, 0:n], ALU.mult, [bAB, sgB], mB)
                else:
                    self.tt("dve", sg[:, 0:n], bA[:, 0:n], sg[:, 0:n], ALU.mult, [bAB, sgB], [sgB])
                    self.tt("pool", self.mT[:, fc, t0:t0 + n], self.mT[:, fc, t0:t0 + n], sg[:, 0:n], ALU.add, [sgB] + mB, mB)

    def mixer_b(self, l, last):
        P, d = self.P, self.d
        nchunk = (NLAT if last else NTOK) // 128
        with ExitStack() as ph:
            W = self.sb(ph, "wzb", [128, 8, 1024], BF16)
            WB = Buf()
            self.load_w(W, WB, d["w_in_%d" % l][:, OFF_ZB:OFF_ZB + 1024], nsplit=2)
            wsT = self.sb(ph, "wsT", [128, 4, 128], BF16)
            cB = Buf()
            P.dma("pool", wsT[:], d["sgwT_%d" % l].rearrange("g q p -> q g p"), writes=[cB])
            bsbc = self.sb(ph, "bsbc", [128, 512])
            lng = self.sb(ph, "lng", [128, 512])
            lnb = self.sb(ph, "lnb", [128, 512])
            P.dma("sp", bsbc[:], d["sgb_%d" % l].partition_broadcast(128), writes=[cB])
            P.dma("sp", lng[:], d["sglng_%d" % l].partition_broadcast(128), writes=[cB])
            P.dma("sp", lnb[:], d["sglnb_%d" % l].partition_broadcast(128), writes=[cB])
            ms = self.merge_setup(ph, l, 1, 128)
            uT = [self.sb(ph, "uT%d" % i, [128, 512]) for i in range(2)]
            uB = [Buf() for _ in range(2)]
            vs = [self.sb(ph, "vs%d" % i, [128, 512]) for i in range(2)]
            vB = [Buf() for _ in range(2)]
            vvb = [self.sb(ph, "vvb%d" % i, [128, 512], BF16) for i in range(2)]
            vvB = [Buf() for _ in range(2)]
            junk = self.sb(ph, "sgjunk", [128, 512])
            jB = Buf()
            st = self.sb(ph, "sgst", [128, 8])
            stB = Buf()
            ybT = [self.sb(ph, "ybT%d" % i, [128, 4, 512], BF16) for i in range(2)]
            ybB = [Buf() for _ in range(2)]
            blocks = [(0, 4), (4, 4), (8, 4), (12, 4)] + ([] if last else [(16, 2), (18, 2)])
            for bi, (ch0, nch) in enumerate(blocks):
                yb, yB = ybT[bi % 2], ybB[bi % 2]
                for cj in range(nch):
                    ch = ch0 + cj
                    i = ch % 2
                    tok0 = ch * 128
                    bu, buB = self.bank[0 + i], self.bankB[0 + i]
                    bv, bvB = self.bank[2 + i], self.bankB[2 + i]
                    bs, bsB = self.bank[4 + i], self.bankB[4 + i]
                    for g in range(4):
                        rd = [WB, self.xnTB[ch]]
                        for k in range(8):
                            self.mm(bu[:, g * 128:(g + 1) * 128], W[:, k, g * 128:(g + 1) * 128], self.xnT[:, k, tok0:tok0 + 128],
                                    k == 0, k == 7, rd, [buB], signal=(k == 7 and g == 3))
                    self.act(uT[i][:], bu[:], AF.Gelu_apprx_tanh, [buB], [uB[i]])
                    self.proj_tm(bv, bvB, W, WB, 512, 512, tok0)
                    self.memset("pool", st[:], 0.0, [stB])
                    self.act(vs[i][:], bv[:], AF.Gelu_apprx_tanh, [bvB, stB], [vB[i], stB], accum_out=st[:, 0:1])
                    self.ts("dve", st[:, 1:2], st[:, 0:1], -1.0 / 512, None, ALU.mult, None, [stB], [stB])
                    self.ts("dve", vs[i][:], vs[i][:], st[:, 1:2], None, ALU.add, None, [vB[i], stB], [vB[i]])
                    self.act(junk[:], vs[i][:], AF.Square, [vB[i], stB], [jB, stB], accum_out=st[:, 2:3])
                    self.rsqrt_inplace(st[:, 2:3], 1.0 / 512, EPS, stB)
                    self.stt("dve", vs[i][:], vs[i][:], st[:, 2:3], lng[:], ALU.mult, ALU.mult, [vB[i], stB, cB], [vB[i]])
                    self.tt("pool", vvb[i][:], vs[i][:], lnb[:], ALU.add, [vB[i], cB], [vvB[i]])
                    for g in range(4):
                        self.mm(bs[:, g * 128:(g + 1) * 128], vvb[i][:, g * 128:(g + 1) * 128], wsT[:, g, :], True, True,
                                [vvB[i], cB], [bsB], signal=(g == 3))
                    self.tt("dve", vs[i][:], bs[:], bsbc[:], ALU.add, [bsB, cB, vB[i]], [vB[i]])
                    self.tt("pool", yb[:, :, cj * 128:(cj + 1) * 128], vs[i][:].rearrange("p (g t) -> p g t", g=4),
                            uT[i][:].rearrange("p (g t) -> p g t", g=4), ALU.mult, [vB[i], uB[i]], [yB])
                self.merge(ms, lambda j, s0, n, yb=yb: yb[:, j, s0:s0 + n], [yB], ch0 * 128, nch * 128, True)
            P.barrier()

    def mixer_a(self, l, last, gath, gathB):
        P, d = self.P, self.d
        lam_init = 0.8 - 0.6 * math.exp(-0.3 * l)
        with ExitStack() as ph:
            W = self.sb(ph, "wqa", [128, 8, 512], BF16)
            WB = Buf()
            self.load_w(W, WB, d["w_in_%d" % l][:, OFF_QA:OFF_QA + 512])
            rs = self.rope_state(ph)
            ms = self.merge_setup(ph, l, 0, 128)
            lamt = self.sb(ph, "lamt", [128, 256])
            lamp = self.sb(ph, "lamp", [128, 128])
            lams = self.sb(ph, "lams", [128, 8])
            lB = Buf()
            P.dma("sp", lamt[:], d["dalam_%d" % l].partition_broadcast(128), writes=[lB])
            P.dma("sp", lams[:, 4:5], d["dasub_%d" % l], writes=[lB])
            self.tt("dve", lamp[:].rearrange("p (m d) -> p m d", m=2), lamt[:].rearrange("p (m k d) -> p m k d", m=2, k=2)[:, :, 0, :],
                    lamt[:].rearrange("p (m k d) -> p m k d", m=2, k=2)[:, :, 1, :], ALU.mult, [lB], [lB])
            P.op("dve", lambda e: e.reduce_sum(out=lams[:, 0:2], in_=lamp[:].rearrange("p (m d) -> p m d", m=2), axis=AX.X),
                 reads=[lB], writes=[lB])
            self.act(lams[:, 0:2], lams[:, 0:2], AF.Exp, [lB], [lB])
            self.tt("dve", lams[:, 2:3], lams[:, 1:2], lams[:, 0:1], ALU.subtract, [lB], [lB])
            self.ts("dve", lams[:, 3:4], lams[:, 2:3], -lam_init, None, ALU.add, None, [lB], [lB])
            self.ts("dve", lams[:, 5:6], lams[:, 4:5], 1.0 - lam_init, None, ALU.mult, None, [lB], [lB])
            neglam, gsc = lams[:, 3:4], lams[:, 5:6]
            KT = self.sb(ph, "KT", [128, NCTX + SEQ], BF16)
            KTBs = [Buf() for _ in range(1 + NCORES)]
            V = self.sb(ph, "Vh", [128, 66, 128], BF16)
            VBs = [Buf() for _ in range(1 + NCORES)]
            QhT = self.sb(ph, "QhT", [128, TL + NCTX], BF16)
            QB = Buf()
            yaT = self.sb(ph, "yaT", [128, 4, TL + NCTX], BF16)
            yaB = Buf()
            NE = 6
            E = [self.sb(ph, "E%d" % i, [128, 512], BF16) for i in range(NE)]
            EB = [Buf() for _ in range(NE)]
            accD = self.sb(ph, "accD", [128, 2, 512])
            accDB = [Buf() for _ in range(2)]
            r1 = self.sb(ph, "fr1", [128, 512])
            r2 = self.sb(ph, "fr2", [128, 512])
            o = self.sb(ph, "fo", [128, 512])
            sq = self.sb(ph, "fsq", [128, 512])
            fB = Buf()
            for b in range(2):
                for h in range(4):
                    for r in range(NCORES):
                        P.dma("sp", KT[:, NCTX + r * TL:NCTX + (r + 1) * TL], self.sec(gath, r, b, SEC_KA + h * 128, 128),
                              reads=[gathB], writes=[KTBs[1 + r]])
                        vsec = self.sec(gath, r, b, SEC_VA, 512).rearrange("r c -> (r c)").rearrange("(h t e) -> h t e", h=4, t=TL)
                        P.dma("sp", V[:, 2 + r * 8:2 + (r + 1) * 8, :], vsec[h].rearrange("(p j) e -> p j e", j=8),
                              reads=[gathB], writes=[VBs[1 + r]])
                    P.dma("sp", KT[:, 0:NCTX], self.ckaT[b][h * 128:(h + 1) * 128, :], reads=[self.ckvB], writes=[KTBs[0]])
                    P.dma("sp", V[:, 0:2, :], self.cva[b][:, h * 128:(h + 1) * 128].rearrange("(t p) e -> p t e", p=128),
                          reads=[self.ckvB], writes=[VBs[0]])
                    for half in range(2):
                        bk, bB = self.bank[half], self.bankB[half]
                        self.proj_fm(bk, bB, W, WB, h * 128, b * TL + half * 512, 512)
                        self.rope_evac(rs, bk, bB, self.bank[2 + half], self.bankB[2 + half], QhT[:, half * 512:(half + 1) * 512], QB,
                                       half * 512, 512)
                    if not last:
                        self.proj_fm(self.bank[0], self.bankB[0], W, WB, h * 128, NLAT + b * NCTX, NCTX)
                        self.cp("act", QhT[:, TL:TL + NCTX], self.bank[0][:, 0:NCTX], [self.bankB[0]], [QB])
                    lat_tiles = [(t, slice(t * 128, (t + 1) * 128)) for t in range(2)] + [
                        (2 + r * 8 + j, slice(NCTX + r * TL + j, NCTX + (r + 1) * TL, 8)) for r in range(NCORES) for j in range(8)]
                    qblocks = [(0, 512, lat_tiles), (512, 512, lat_tiles)] + ([] if last else [(TL, NCTX, lat_tiles[0:2])])
                    for (q0, n, tiles) in qblocks:
                        O1, O2, D1, D2 = self.bank[4], self.bank[5], self.bank[6], self.bank[7]
                        O1B, O2B, D1B, D2B = self.bankB[4], self.bankB[5], self.bankB[6], self.bankB[7]
                        nt = len(tiles)
                        def emit_s(ti):
                            vt, ksl = tiles[ti]
                            for m in range(2):
                                si = (2 * ti + m) % 4
                                ei = (2 * ti + m) % NE
                                S, SB = self.bank[si], self.bankB[si]
                                self.mm(S[:, 0:n], KT[m * 64:(m + 1) * 64, ksl], QhT[m * 64:(m + 1) * 64, q0:q0 + n], True, True,
                                        [KTBs[0 if vt < 2 else 1 + (vt - 2) // 8], QB], [SB])
                                self.act(E[ei][:, 0:n], S[:, 0:n], AF.Exp, [SB], [EB[ei]], scale=0.125)

                        def emit_pv(ti):
                            vt, ksl = tiles[ti]
                            for m in range(2):
                                ei = (2 * ti + m) % NE
                                Om, OmB = (O1, O1B) if m == 0 else (O2, O2B)
                                self.mm(Om[:, 0:n], V[:, vt, :], E[ei][:, 0:n], ti == 0, ti == nt - 1,
                                        [VBs[0 if vt < 2 else 1 + (vt - 2) // 8], EB[ei]], [OmB])
                                if m == 0:
                                    self.mm(D1[:, 0:n], self.ones_b[:], E[ei][:, 0:n], ti == 0, ti == nt - 1, [self.cB, EB[ei]], [D1B])
                                else:
                                    a = ti % 2
                                    eng = "dve" if a == 0 else "pool"
                                    if ti < 2:
                                        self.cp(eng, accD[:, a, 0:n], E[ei][:, 0:n], [EB[ei]], [accDB[a]])
                                    else:
                                        self.tt(eng, accD[:, a, 0:n], accD[:, a, 0:n], E[ei][:, 0:n], ALU.add, [EB[ei], accDB[a]], [accDB[a]])

                        emit_s(0)
                        for ti in range(nt):
                            if ti + 1 < nt:
                                emit_s(ti + 1)
                            emit_pv(ti)
                        self.mm(D2[:, 0:n], self.ones_f[:], accD[:, 0, 0:n], True, False, [self.cB, accDB[0]], [D2B], signal=False)
                        self.mm(D2[:, 0:n], self.ones_f[:], accD[:, 1, 0:n], False, True, [self.cB, accDB[1]], [D2B])
                        self.recip(r1[:, 0:n], D1[:, 0:n], [D1B], [fB])
                        self.recip(r2[:, 0:n], D2[:, 0:n], [D2B], [fB])
                        self.tt("dve", o[:, 0:n], O1[:, 0:n], r1[:, 0:n], ALU.mult, [O1B, fB], [fB])
                        self.tt("dve", r2[:, 0:n], O2[:, 0:n], r2[:, 0:n], ALU.mult, [O2B, fB], [fB])
                        self.stt("dve", o[:, 0:n], r2[:, 0:n], neglam, o[:, 0:n], ALU.mult, ALU.add, [fB, lB], [fB])
                        self.act(sq[:, 0:n], o[:, 0:n], AF.Square, [fB], [fB])
                        self.mm(D1[:, 0:n], self.ones_f[:], sq[:, 0:n], True, True, [self.cB, fB], [D1B])
                        self.ts("dve", r1[:, 0:n], D1[:, 0:n], 1.0 / 128, EPS, ALU.mult, ALU.add, [D1B, fB], [fB])
                        self.act(r1[:, 0:n], r1[:, 0:n], AF.Sqrt, [fB], [fB])
                        self.recip(r1[:, 0:n], r1[:, 0:n], [fB], [fB])
                        self.stt("dve", yaT[:, h, q0:q0 + n], o[:, 0:n], gsc, r1[:, 0:n], ALU.mult, ALU.mult, [fB, lB], [yaB])
                self.merge(ms, lambda j, s0, n: yaT[:, j, s0:s0 + n], [yaB], b * TL, TL, False, banks=(0, 1))
                if not last:
                    self.merge(ms, lambda j, s0, n: yaT[:, j, TL + s0:TL + s0 + n], [yaB], NLAT + b * NCTX, NCTX, False, banks=(0, 1))
            P.barrier()

    def mixer_c(self, l, last, gath, gathB):
        P, d = self.P, self.d
        with ExitStack() as ph:
            W = self.sb(ph, "wqc", [128, 8, 512], BF16)
            WB = Buf()
            self.load_w(W, WB, d["w_in_%d" % l][:, OFF_QC:OFF_QC + 512])
            ms = self.merge_setup(ph, l, 2, 64)
            selm = self.sb(ph, "selm", [128, 16, 128], BF16)
            selB = Buf()
            for j in range(16):
                self.ts("dve", selm[:, j, :], self.ident_b[:], self.selw[:, j:j + 1], None, ALU.mult, None, [self.cB], [selB])
            KCx = self.sb(ph, "KCx", [128, 4, 24 * 64], BF16)
            KCB = Buf()
            VCx = self.sb(ph, "VCx", [128, 12, 512], BF16)
            VCB = Buf()
            KCc = self.sb(ph, "KCc", [128, 4, NCTX], BF16)
            VCc = self.sb(ph, "VCc", [128, 2, 512], BF16)
            ccB = Buf()
            QcT = self.sb(ph, "QcT", [128, 4, TL + NCTX], BF16)
            QB = Buf()
            ycT = [self.sb(ph, "ycT%d" % i, [64, 8, 512], BF16) for i in range(1)]
            ycB = [Buf() for _ in range(1)]
            E = [self.sb(ph, "Ec%d" % i, [128, 512], BF16) for i in range(4)]
            EB = [Buf() for _ in range(4)]
            NBT = 8
            bt = [self.sb(ph, "nabt%d" % i, [128, 512], BF16) for i in range(NBT)]
            btB = [Buf() for _ in range(NBT)]
            nqb = 2
            bias_uses = [(g_, hd_, i_) for b_ in range(2) for g_ in range(nqb) for hd_ in range(8) for i_ in range(8)]
            bias_state = {"k": 0}

            def bias_prefetch(k):
                if k < len(bias_uses):
                    g_, hd_, i_ = bias_uses[k]
                    P.dma("pool", bt[k % NBT][:], d["nabias_%d" % l][g_, hd_, i_], writes=[btB[k % NBT]])

            for k in range(NBT):
                bias_prefetch(k)
            rr = [self.sb(ph, "ncr%d" % i, [64, 512]) for i in range(2)]
            rrB = [Buf() for _ in range(2)]
            cand = [self.sb(ph, "cand%d" % i, [128, 8, 512], BF16) for i in range(1)]
            candB = [Buf() for _ in range(1)]
            yi = 0
            for b in range(2):
                P.dma("sp", KCx[:, :, 256:256 + TL], self.kcT[b].rearrange("(c p) t -> p c t", p=128), reads=[self.kvownB], writes=[KCB])
                P.dma("sp", VCx[:, 2:10, :], self.vc[b].rearrange("(t p) e -> p t e", p=128), reads=[self.kvownB], writes=[VCB])
                P.dma("sp", KCc[:], self.ckcT[b].rearrange("(c p) t -> p c t", p=128), reads=[self.ckvB], writes=[ccB])
                P.dma("sp", VCc[:], self.cvc[b].rearrange("(t p) e -> p t e", p=128), reads=[self.ckvB], writes=[ccB])
                ci = 0
                for c in range(4):
                    cd, cdB = cand[0], candB[0]
                    ci += 1
                    for r in range(NCORES):
                        hsec = self.sec(gath, r, b, SEC_KCH, 256).rearrange("a (two t) -> (a two) t", two=2)
                        P.dma("sp", cd[:, r, :], hsec[c * 128:(c + 1) * 128, :], reads=[gathB], writes=[cdB])
                    for side in range(2):
                        bk, bB = self.bank[side], self.bankB[side]
                        for r in range(NCORES):
                            src = cd[:, r, 256:512] if side == 0 else cd[:, r, 0:256]
                            self.mm(bk[:, 0:256], selm[:, side * 8 + r, :], src, r == 0, r == NCORES - 1, [selB, cdB], [bB])
                        dst = KCx[:, c, 0:256] if side == 0 else KCx[:, c, 256 + TL:512 + TL]
                        self.cp("act", dst, bk[:, 0:256], [bB], [KCB])
                for side in range(2):
                    for a in range(2):
                        cd, cdB = cand[0], candB[0]
                        ci += 1
                        for r in range(NCORES):
                            vsec = self.sec(gath, r, b, SEC_VCH, 256).rearrange("a (two e) -> (a two) e", two=2)
                            t0 = (256 + a * 128) if side == 0 else a * 128
                            P.dma("sp", cd[:, r, :], vsec[t0:t0 + 128, :], reads=[gathB], writes=[cdB])
                        bk, bB = self.bank[2 + a], self.bankB[2 + a]
                        for r in range(NCORES):
                            self.mm(bk[:, :], selm[:, side * 8 + r, :], cd[:, r, :], r == 0, r == NCORES - 1, [selB, cdB], [bB])
                        self.cp("dve", VCx[:, (0 if side == 0 else 10) + a, :], bk[:, :], [bB], [VCB])
                for c in range(4):
                    for half in range(2):
                        bk, bB = self.bank[(2 * c + half) % 4], self.bankB[(2 * c + half) % 4]
                        self.proj_fm(bk, bB, W, WB, c * 128, b * TL + half * 512, 512)
                        self.act(QcT[:, c, half * 512:(half + 1) * 512], bk[:, :], AF.Copy, [bB], [QB], scale=0.125)
                    if not last:
                        bk, bB = self.bank[c % 4], self.bankB[c % 4]
                        self.proj_fm(bk, bB, W, WB, c * 128, NLAT + b * NCTX, NCTX)
                        self.act(QcT[:, c, TL:TL + NCTX], bk[:, 0:NCTX], AF.Copy, [bB], [QB], scale=0.125)
                ctx_tiles = [("c", t) for t in range(2)]
                qblocks = [(0, 512, [("w", 0 + i) for i in range(8)] + ctx_tiles, 0),
                           (512, 512, [("w", 4 + i) for i in range(8)] + ctx_tiles, 1)]
                if not last:
                    qblocks.append((TL, NCTX, ctx_tiles, None))
                si = 0
                for (q0, n, tiles, g) in qblocks:
                    yc, yB = ycT[0], ycB[0]
                    yi += 1
                    for hd in range(8):
                        c, po = hd // 2, (hd % 2) * 64
                        O, OB = self.bank[4 + 2 * (hd % 2)], self.bankB[4 + 2 * (hd % 2)]
                        Dn, DB = self.bank[5 + 2 * (hd % 2)], self.bankB[5 + 2 * (hd % 2)]
                        nt = len(tiles)
                        def emit_s(ti, sidx):
                            kind, j = tiles[ti]
                            S, SB = self.bank[sidx % 4], self.bankB[sidx % 4]
                            e, eB = E[sidx % 4], EB[sidx % 4]
                            if kind == "w":
                                i = j - 4 * g
                                k = bias_state["k"]
                                assert bias_uses[k] == (g, hd, i)
                                btile, btB_ = bt[k % NBT], btB[k % NBT]
                                self.mm(S[:, 0:n], KCx[po:po + 64, c, j * 128:(j + 1) * 128], QcT[po:po + 64, c, q0:q0 + n], True, False,
                                        [KCB, QB], [SB], signal=False)
                                self.mm(S[:, 0:n], self.ident_b[:], btile[:, 0:n], False, True, [self.cB, btB_], [SB])
                                bias_prefetch(k + NBT)
                                bias_state["k"] = k + 1
                            else:
                                self.mm(S[:, 0:n], KCc[po:po + 64, c, j * 128:(j + 1) * 128], QcT[po:po + 64, c, q0:q0 + n], True, True,
                                        [ccB, QB], [SB])
                            self.act(e[:, 0:n], S[:, 0:n], AF.Exp, [SB], [eB])

                        def emit_pv(ti, sidx):
                            kind, j = tiles[ti]
                            e, eB = E[sidx % 4], EB[sidx % 4]
                            if kind == "w":
                                vap, vrd = VCx[:, j, hd * 64:(hd + 1) * 64], VCB
                            else:
                                vap, vrd = VCc[:, j, hd * 64:(hd + 1) * 64], ccB
                            self.mm(O[0:64, 0:n], vap, e[:, 0:n], ti == 0, ti == nt - 1, [vrd, eB], [OB])
                            self.mm(Dn[0:64, 0:n], self.ones_b[:, 0:64], e[:, 0:n], ti == 0, ti == nt - 1, [self.cB, eB], [DB])

                        emit_s(0, si)
                        for ti in range(nt):
                            if ti + 1 < nt:
                                emit_s(ti + 1, si + ti + 1)
                            emit_pv(ti, si + ti)
                        si += nt
                        r_, rB_ = rr[hd % 2], rrB[hd % 2]
                        self.recip(r_[:, 0:n], Dn[0:64, 0:n], [DB], [rB_])
                        self.tt("dve", yc[:, hd, 0:n], O[0:64, 0:n], r_[:, 0:n], ALU.mult, [OB, rB_], [yB])
                    tok0 = b * TL + q0 if g is not None else NLAT + b * NCTX
                    self.merge(ms, lambda j, s0, n_, yc=yc: yc[0:64, j, s0:s0 + n_], [yB], tok0, n, False, banks=(0, 1))
            P.barrier()

    def gate_bcast(self, ph, l, c0, v, dst, dstB):
        self.bcast_rows(ph, lambda c: self.modT[l][:, c0 + c, v:v + 1], 8, dst, dstB, [self.modTB[l]])

    def out_proj(self, l, last):
        P, d = self.P, self.d
        ntile = (NLAT if last else NTOK) // 128
        with ExitStack() as ph:
            W = self.sb(ph, "wout", [128, 8, D], BF16)
            WB = Buf()
            self.load_w(W, WB, d["w_out_%d" % l][:, :], nsplit=2)
            gbc = self.sb(ph, "gbc", [128, D])
            gB = Buf()
            ht = [self.sb(ph, "oht%d" % i, [128, D]) for i in range(2)]
            htB = [Buf() for _ in range(2)]
            hn = [self.sb(ph, "ohn%d" % i, [128, D]) for i in range(2)]
            hnB = [Buf() for _ in range(2)]
            curv = -1
            for t in range(ntile):
                v = self.variant(t)
                if v != curv:
                    self.gate_bcast(ph, l, 16, v, gbc, gB)
                    curv = v
                i = t % 2
                P.dma("sp", ht[i][:], self.hbuf[t * 128:(t + 1) * 128, :], reads=[self.hB[t]], writes=[htB[i]])
                for half in range(2):
                    bk, bB = self.bank[2 + 2 * i + half], self.bankB[2 + 2 * i + half]
                    for fc in range(8):
                        self.mm(bk[:, :], self.mT[:, fc, t * 128:(t + 1) * 128], W[:, fc, half * 512:(half + 1) * 512], fc == 0, fc == 7,
                                [self.mTB[t], WB], [bB])
                    self.tt("dve", hn[i][:, half * 512:(half + 1) * 512], bk[:, :], gbc[:, half * 512:(half + 1) * 512], ALU.mult,
                            [bB, gB], [hnB[i]])
                self.tt("pool", hn[i][:], hn[i][:], ht[i][:], ALU.add, [hnB[i], htB[i]], [hnB[i]])
                P.dma("sp", self.hbuf[t * 128:(t + 1) * 128, :], hn[i][:], reads=[hnB[i]], writes=[self.hB[t]])
            P.barrier()

    def phase_mix(self, l, last, gath, gathB):
        with ExitStack() as ph:
            self.mT = self.sb(ph, "mT", [128, 8, NTOK], BF16)
            self.mTB = [Buf("mT%d" % i) for i in range(NTOK // 128)]
            self.mixer_b(l, last)
            self.mixer_a(l, last, gath, gathB)
            self.mixer_c(l, last, gath, gathB)
            self.out_proj(l, last)

    def phase_moe_dense(self, l, last):
        P, d = self.P, self.d
        ntok = NLAT if last else NTOK
        ntile = ntok // 128
        with ExitStack() as ph:
            wts = self.sb(ph, "wts", [128, NTOK // 128, NEXP])
            wtsB = Buf()
            rstate = {}

            def r_begin(ph2):
                rstate["wrT"] = self.sb(ph2, "wrT", [128, 8, 36])
                rstate["br"] = self.sb(ph2, "brbc", [128, 36])
                rstate["B"] = Buf()
                P.dma("sp", rstate["wrT"][:], d["wr_%d" % l].rearrange("(k p) n -> p k n", p=128), writes=[rstate["B"]])
                P.dma("sp", rstate["br"][:], d["br_%d" % l].partition_broadcast(128), writes=[rstate["B"]])
                rstate["lg"] = self.sb(ph2, "rlg", [128, 36])
                rstate["em"] = self.sb(ph2, "rem", [128, 32])
                rstate["oh"] = self.sb(ph2, "roh", [128, 32])
                rstate["sm"] = self.sb(ph2, "rsm", [128, 16])
                rstate["ge"] = self.sb(ph2, "rge", [128, 4])
                rstate["tB"] = Buf()

            def r_tile(t, half, tmp, tmpB):
                L, LB = self.bank[2], self.bankB[2]
                for c in range(4):
                    self.mm(L[:, 0:36], tmp[:, c, :], rstate["wrT"][:, half * 4 + c, :], half == 0 and c == 0, half == 1 and c == 3,
                            [tmpB, rstate["B"]], [LB], signal=(c == 3))
                if half == 0:
                    return
                lg, em, oh, sm, ge, tB = rstate["lg"], rstate["em"], rstate["oh"], rstate["sm"], rstate["ge"], rstate["tB"]
                rB = [tB]
                self.tt("dve", lg[:], L[:, 0:36], rstate["br"][:], ALU.add, [LB, rstate["B"]], rB)
                P.op("dve", lambda e: e.reduce_max(out=sm[:, 0:1], in_=lg[:, 0:4], axis=AX.X), reads=rB, writes=rB)
                self.ts("dve", oh[:, 0:4], lg[:, 0:4], sm[:, 0:1], None, ALU.is_equal, None, rB, rB)
                self.ts("dve", sm[:, 1:2], sm[:, 0:1], -1.0, None, ALU.mult, None, rB, rB)
                self.memset("dve", sm[:, 2:3], 0.0, rB)
                self.act(ge[:], lg[:, 0:4], AF.Exp, rB, rB, bias=sm[:, 1:2], accum_out=sm[:, 2:3])
                self.recip(sm[:, 3:4], sm[:, 2:3], rB, rB)
                self.ts("dve", oh[:, 4:8], oh[:, 0:4], 1e30, -1e30, ALU.mult, ALU.add, rB, rB)
                self.tt("dve", em[:].rearrange("p (g e) -> p g e", g=4), lg[:, 4:36].rearrange("p (g e) -> p g e", g=4),
                        oh[:, 4:8].unsqueeze(2).to_broadcast([128, 4, 8]), ALU.add, rB, rB)
                P.op("dve", lambda e: e.reduce_max(out=sm[:, 4:5], in_=em[:], axis=AX.X), reads=rB, writes=rB)
                self.ts("dve", oh[:], em[:], sm[:, 4:5], None, ALU.is_equal, None, rB, rB)
                self.stt("dve", em[:], oh[:], -1e30, em[:], ALU.mult, ALU.add, rB, rB)
                P.op("dve", lambda e: e.reduce_max(out=sm[:, 5:6], in_=em[:], axis=AX.X), reads=rB, writes=rB)
                self.tt("dve", sm[:, 6:7], sm[:, 4:5], sm[:, 5:6], ALU.subtract, rB, rB)
                self.act(sm[:, 6:7], sm[:, 6:7], AF.Sigmoid, rB, rB)
                self.tt("dve", sm[:, 7:8], sm[:, 6:7], sm[:, 3:4], ALU.mult, rB, rB)
                self.tt("dve", sm[:, 8:9], sm[:, 3:4], sm[:, 7:8], ALU.subtract, rB, rB)
                self.ts("dve", wts[:, t, :], oh[:], sm[:, 7:8], None, ALU.mult, None, rB, [wtsB])
                self.ts("dve", oh[:], em[:], sm[:, 5:6], None, ALU.is_equal, None, rB, rB)
                self.stt("dve", wts[:, t, :], oh[:], sm[:, 8:9], wts[:, t, :], ALU.mult, ALU.add, rB + [wtsB], [wtsB])

            self.phase_norm(l, 2, ntok, route={"begin": r_begin, "tile": r_tile})

            acc = self.sb(ph, "acc", [128, ntile, D])
            accB = [Buf() for _ in range(ntile)]
            for t in range(ntile):
                self.memset("pool", acc[:, t, :], 0.0, [accB[t]])
            wg = [self.sb(ph, "wg%d" % i, [128, 8, DEXP], BF16) for i in range(2)]
            wu = [self.sb(ph, "wu%d" % i, [128, 8, DEXP], BF16) for i in range(2)]
            wd = [self.sb(ph, "wd%d" % i, [128, 4, D], BF16) for i in range(2)]
            wB = [Buf() for _ in range(2)]
            sgb = [self.sb(ph, "msg%d" % i, [128, 512], BF16) for i in range(2)]
            sgB = [Buf() for _ in range(2)]
            hdT = self.sb(ph, "hdT", [128, 4, 512], BF16)
            hdB = [Buf() for _ in range(4)]

            def load_expert(e):
                i = e % 2
                self.load_w(wg[i], wB[i], d["wg_%d" % l][e])
                self.load_w(wu[i], wB[i], d["wu_%d" % l][e])
                self.load_w(wd[i], wB[i], d["wd_%d" % l][e])

            load_expert(0)
            bi = 0
            for e in range(NEXP):
                if e + 1 < NEXP:
                    load_expert(e + 1)
                i = e % 2
                for tb in range(ntok // 512):
                    tok0 = tb * 512
                    for fcb in range(4):
                        G, GB = self.bank[2 * (fcb % 2)], self.bankB[2 * (fcb % 2)]
                        U, UB = self.bank[2 * (fcb % 2) + 1], self.bankB[2 * (fcb % 2) + 1]
                        self.proj_fm(G, GB, wg[i], wB[i], fcb * 128, tok0, 512)
                        self.proj_fm(U, UB, wu[i], wB[i], fcb * 128, tok0, 512)
                        sg_, sgB_ = sgb[fcb % 2], sgB[fcb % 2]
                        self.act(sg_[:], G[:, :], AF.Silu, [GB], [sgB_])
                        self.tt("dve", hdT[:, fcb, :], U[:, :], sg_[:], ALU.mult, [UB, sgB_], [hdB[fcb]])
                    for tt_ in range(4):
                        t = tb * 4 + tt_
                        for half in range(2):
                            Y, YB = self.bank[4 + bi % 4], self.bankB[4 + bi % 4]
                            bi += 1
                            for fcb in range(4):
                                self.mm(Y[:, :], hdT[:, fcb, tt_ * 128:(tt_ + 1) * 128], wd[i][:, fcb, half * 512:(half + 1) * 512],
                                        fcb == 0, fcb == 3, [hdB[fcb], wB[i]], [YB])
                            self.stt("dve", acc[:, t, half * 512:(half + 1) * 512], Y[:, :], wts[:, t, e:e + 1],
                                     acc[:, t, half * 512:(half + 1) * 512], ALU.mult, ALU.add, [YB, wtsB, accB[t]], [accB[t]])
            gbc = self.sb(ph, "gbc2", [128, D])
            gB = Buf()
            ht = [self.sb(ph, "mht%d" % i, [128, D]) for i in range(2)]
            htB = [Buf() for _ in range(2)]
            curv = -1
            for t in range(ntile):
                v = self.variant(t)
                if v != curv:
                    self.gate_bcast(ph, l, 40, v, gbc, gB)
                    curv = v
                i = t % 2
                P.dma("sp", ht[i][:], self.hbuf[t * 128:(t + 1) * 128, :], reads=[self.hB[t]], writes=[htB[i]])
                self.tt("pool", acc[:, t, :], acc[:, t, :], gbc[:], ALU.mult, [accB[t], gB], [accB[t]])
                self.tt("dve", acc[:, t, :], acc[:, t, :], ht[i][:], ALU.add, [accB[t], htB[i]], [accB[t]])
                P.dma("sp", self.hbuf[t * 128:(t + 1) * 128, :], acc[:, t, :], reads=[accB[t], htB[i]], writes=[self.hB[t]])
            P.barrier()

    def phase_moe(self, l, last):
        P, d = self.P, self.d
        ntok = NLAT if last else NTOK
        ntile = ntok // 128
        C = MOE_CAP
        NSLOT = NEXP * C
        I32 = mybir.dt.int32
        xs = self.dscr("xs%d" % l, [NSLOT, D], BF16)
        ys = self.dscr("ys%d" % l, [NSLOT, D], F32)
        scaleT = self.s2T[l]
        mB = self.modTB[l]
        with ExitStack() as ph:
            didx = self.sb(ph, "didx", [128, NTOK // 128, 2], I32)
            wts2 = self.sb(ph, "wts2", [128, NTOK // 128, 2])
            rtB = Buf()
            with ExitStack() as p1:
                tri = self.sb(p1, "tri", [128, 128], BF16)
                ecb = self.sb(p1, "ecb", [128, NEXP])
                wrT = self.sb(p1, "wrT", [128, 8, 36])
                brb = self.sb(p1, "brbc", [128, 36])
                kB = Buf()
                P.dma("pool", tri[:], d["tri"], writes=[kB])
                P.dma("sp", ecb[:], d["ecb"], writes=[kB])
                P.dma("sp", wrT[:], d["wr_%d" % l].rearrange("(k p) n -> p k n", p=128), writes=[kB])
                P.dma("sp", brb[:], d["br_%d" % l].partition_broadcast(128), writes=[kB])
                base = self.sb(p1, "rbase", [128, NEXP])
                baseB = Buf()
                self.memset("dve", base[:], 0.0, [baseB])
                sbc = self.sb(p1, "sbc", [128, D])
                shbc = self.sb(p1, "shbc", [128, D])
                bcB = Buf()
                ht = [self.sb(p1, "ht%d" % i, [128, D]) for i in range(2)]
                htB = [Buf() for _ in range(2)]
                xf = [self.sb(p1, "xf%d" % i, [128, D]) for i in range(2)]
                xfB = [Buf() for _ in range(2)]
                xtm = [self.sb(p1, "xtm%d" % i, [128, D], BF16) for i in range(3)]
                xtmB = [Buf() for _ in range(3)]
                xT = [self.sb(p1, "xT%d" % i, [128, 4, 128]) for i in range(2)]
                xTB = [Buf() for _ in range(2)]
                junk = self.sb(p1, "junk", [128, D])
                junkB = Buf()
                ss = self.sb(p1, "ss", [128, 32])
                ssB = Buf()
                lg = self.sb(p1, "rlg", [128, 36])
                em = self.sb(p1, "rem", [128, 32])
                oh1 = self.sb(p1, "roh1", [128, 32])
                oh2 = self.sb(p1, "roh2", [128, 32])
                ohg = self.sb(p1, "rohg", [128, 8])
                Ab = self.sb(p1, "rAb", [128, 32], BF16)
                slot = self.sb(p1, "rslot", [128, 32])
                tmp32 = self.sb(p1, "rtmp32", [128, 32])
                sm = self.sb(p1, "rsm", [128, 16])
                ge = self.sb(p1, "rge", [128, 4])
                tB = Buf()
                rB = [tB]
                NT = ntile
                xall = self.sb(p1, "xall", [128, NT, D], BF16)
                xallB = [Buf() for _ in range(NT)]
                lgall = self.sb(p1, "lgall", [128, NT, 36])
                emA = self.sb(p1, "emA", [128, NT, 32])
                o1A = self.sb(p1, "o1A", [128, NT, 32])
                o2A = self.sb(p1, "o2A", [128, NT, 32])
                slA = self.sb(p1, "slA", [128, NT, 32])
                bsA = self.sb(p1, "bsA", [128, NT, 32])
                AbA = self.sb(p1, "AbA", [128, NT, 32], BF16)
                g4 = self.sb(p1, "g4", [128, NT, 4])
                p4 = self.sb(p1, "p4", [128, NT, 4])
                sv = self.sb(p1, "sv", [128, 12, NT])
                self.memset("dve", ss[:], 0.0, [ssB])
                for t in range(ntile):
                    i = t % 2
                    P.dma("sp", ht[i][:], self.hbuf[t * 128:(t + 1) * 128, :], reads=[self.hB[t]], writes=[htB[i]])
                    self.act(junk[:], ht[i][:], AF.Square, [htB[i], ssB], [junkB, ssB], accum_out=ss[:, t:t + 1])
                self.rsqrt_inplace(ss[:, 0:ntile], 1.0 / D, EPS, ssB)
                curv = -1
                for t in range(ntile):
                    i = t % 2
                    v = self.variant(t)
                    if v != curv:
                        self.bcast_rows(p1, lambda c, v=v: scaleT[:, c, v:v + 1], 8, sbc, bcB, [mB])
                        self.bcast_rows(p1, lambda c, v=v: self.modT[l][:, 24 + c, v:v + 1], 8, shbc, bcB, [mB])
                        curv = v
                    P.dma("sp", ht[i][:], self.hbuf[t * 128:(t + 1) * 128, :], reads=[self.hB[t]], writes=[htB[i]])
                    self.act(xf[i][:], ht[i][:], AF.Copy, [htB[i], ssB], [xfB[i]], scale=ss[:, t:t + 1])
                    self.tt("dve", xf[i][:], xf[i][:], sbc[:], ALU.mult, [xfB[i], bcB], [xfB[i]])
                    self.tt("pool", xf[i][:], xf[i][:], shbc[:], ALU.add, [xfB[i], bcB], [xfB[i]])
                    self.cp("pool", xall[:, t, :], xf[i][:], [xfB[i]], [xallB[t]])
                    L, LB = self.bank[2 + t % 2], self.bankB[2 + t % 2]
                    for half in range(2):
                        bk, bB = self.bank[half], self.bankB[half]
                        for c in range(4):
                            cc = half * 4 + c
                            self.tr(bk[:, c * 128:(c + 1) * 128], xf[i][:, cc * 128:(cc + 1) * 128], self.ident[:], [xfB[i], self.cB], [bB],
                                    signal=(c == 3))
                        self.cp("act", xT[half][:], bk[:].rearrange("p (c t) -> p c t", c=4), [bB], [xTB[half]])
                        for c in range(4):
                            self.mm(L[:, 0:36], xT[half][:, c, :], wrT[:, half * 4 + c, :], half == 0 and c == 0, half == 1 and c == 3,
                                    [xTB[half], kB], [LB], signal=(c == 3))
                    self.tt("dve", lgall[:, t, :], L[:, 0:36], brb[:], ALU.add, [LB, kB], rB)
                G = lgall[:, :, 0:4]

                def bc(ap2, n):
                    return ap2.unsqueeze(2).to_broadcast([128, NT, n])

                def rmax(out, in_):
                    P.op("dve", lambda e: e.reduce_max(out=out, in_=in_, axis=AX.X), reads=rB, writes=rB)

                def rsum(out, in_):
                    P.op("dve", lambda e: e.reduce_sum(out=out, in_=in_, axis=AX.X), reads=rB, writes=rB)

                gmax, gsum, gw, m1, m2, dl, w1, d1, d2 = (sv[:, j, :] for j in range(9))
                rmax(gmax, G)
                self.tt("dve", p4[:], G, bc(gmax, 4), ALU.is_equal, rB, rB)
                self.tt("dve", g4[:], G, bc(gmax, 4), ALU.subtract, rB, rB)
                self.act(g4[:], g4[:], AF.Exp, rB, rB)
                rsum(gsum, g4[:])
                self.recip(gw, gsum, rB, rB)
                self.ts("dve", p4[:], p4[:], 1e30, -1e30, ALU.mult, ALU.add, rB, rB)
                self.cp("dve", emA[:], lgall[:, :, 4:36], rB, rB)
                self.tt("dve", emA[:].rearrange("p t (g e) -> p (t g) e", g=4), emA[:].rearrange("p t (g e) -> p (t g) e", g=4),
                        p4[:].rearrange("p t g -> p (t g)").unsqueeze(2).to_broadcast([128, NT * 4, 8]), ALU.add, rB, rB)
                rmax(m1, emA[:])
                self.tt("dve", o1A[:], emA[:], bc(m1, 32), ALU.is_equal, rB, rB)
                self.stt("dve", emA[:], o1A[:], -1e30, emA[:], ALU.mult, ALU.add, rB, rB)
                rmax(m2, emA[:])
                self.tt("dve", o2A[:], emA[:], bc(m2, 32), ALU.is_equal, rB, rB)
                self.tt("dve", dl, m1, m2, ALU.subtract, rB, rB)
                self.act(dl, dl, AF.Sigmoid, rB, rB)
                self.tt("dve", wts2[:, 0:NT, 0], dl, gw, ALU.mult, rB, [rtB])
                self.tt("dve", wts2[:, 0:NT, 1], gw, wts2[:, 0:NT, 0], ALU.subtract, rB + [rtB], [rtB])
                self.tt("dve", AbA[:], o1A[:], o2A[:], ALU.add, rB, rB)
                RK = [(self.bank[4], self.bankB[4]), (self.bank[5], self.bankB[5])]
                TO = [(self.bank[6], self.bankB[6]), (self.bank[7], self.bankB[7])]
                for t in range(NT):
                    (R, RB_), (T_, TB_) = RK[t // 16], TO[t // 16]
                    c0 = (t % 16) * 32
                    self.mm(R[:, c0:c0 + 32], tri[:], AbA[:, t, :], True, True, [kB, tB], [RB_], signal=(t % 16 == 15 or t == NT - 1))
                    self.mm(T_[:, c0:c0 + 32], self.ones_b[:], AbA[:, t, :], True, True, [self.cB, tB], [TB_],
                            signal=(t % 16 == 15 or t == NT - 1))
                for j in range((NT + 15) // 16):
                    n_ = min(16, NT - 16 * j)
                    self.cp("dve", slA[:, 16 * j:16 * j + n_, :], RK[j][0][:, 0:n_ * 32].rearrange("p (t e) -> p t e", e=32), [RK[j][1]], rB)
                    self.cp("dve", emA[:, 16 * j:16 * j + n_, :], TO[j][0][:, 0:n_ * 32].rearrange("p (t e) -> p t e", e=32), [TO[j][1]], rB)
                self.memset("dve", bsA[:, 0, :], 0.0, rB)
                for t in range(1, NT):
                    self.tt("dve", bsA[:, t, :], bsA[:, t - 1, :], emA[:, t - 1, :], ALU.add, rB, rB)
                self.tt("dve", slA[:], slA[:], bsA[:], ALU.add, rB, rB)
                self.ts("dve", emA[:], slA[:], float(C), 1e7, ALU.is_ge, ALU.mult, rB, rB)
                self.tt("dve", slA[:], slA[:], emA[:], ALU.add, rB, rB)
                self.tt("dve", slA[:], slA[:], ecb[:].unsqueeze(1).to_broadcast([128, NT, 32]), ALU.add, rB + [kB], rB)
                self.tt("dve", emA[:], slA[:], o1A[:], ALU.mult, rB, rB)
                rsum(d1, emA[:])
                self.tt("dve", emA[:], slA[:], o2A[:], ALU.mult, rB, rB)
                rsum(d2, emA[:])
                self.cp("dve", didx[:, 0:NT, 0], d1, rB, [rtB])
                self.cp("dve", didx[:, 0:NT, 1], d2, rB + [rtB], [rtB])
                for t in range(NT):
                    for k in range(2):
                        self.P.indirect("scatter", xs, didx[:, t, k:k + 1], xall[:, t, :], NSLOT - 1, reads=[xallB[t], rtB])
                P.barrier()
            with ExitStack() as p2:
                wg = [self.sb(p2, "wg%d" % i, [128, 8, DEXP], BF16) for i in range(2)]
                wu = [self.sb(p2, "wu%d" % i, [128, 8, DEXP], BF16) for i in range(2)]
                wd = [self.sb(p2, "wd%d" % i, [128, 4, D], BF16) for i in range(2)]
                wB = [Buf() for _ in range(2)]
                xe = [self.sb(p2, "xe%d" % i, [128, C // 128, D], BF16) for i in range(2)]
                xeB = [Buf() for _ in range(2)]
                XeT = [self.sb(p2, "XeT%d" % i, [128, 8, C], BF16) for i in range(2)]
                XeTB = [Buf() for _ in range(2)]
                sgb = [self.sb(p2, "msg%d" % i, [128, C], BF16) for i in range(2)]
                sgB = [Buf() for _ in range(2)]
                hdT = self.sb(p2, "hdT", [128, 4, C], BF16)
                hdB = [Buf() for _ in range(4)]
                yo = [self.sb(p2, "yo%d" % i, [128, D]) for i in range(2)]
                yoB = [Buf() for _ in range(2)]
                ysB = Buf()

                def load_expert(e):
                    i = e % 2
                    self.load_w(wg[i], wB[i], d["wg_%d" % l][e])
                    self.load_w(wu[i], wB[i], d["wu_%d" % l][e])
                    self.load_w(wd[i], wB[i], d["wd_%d" % l][e])
                    P.dma("sp", xe[i][:], xs[e * C:(e + 1) * C, :].rearrange("(s p) f -> p s f", p=128), writes=[xeB[i]])

                load_expert(0)
                bi = 0
                yi = 0
                for e in range(NEXP):
                    if e + 1 < NEXP:
                        load_expert(e + 1)
                    i = e % 2
                    for fc in range(8):
                        bk, bB = self.bank[fc % 2], self.bankB[fc % 2]
                        bkb = bk[:].bitcast(BF16)
                        for st in range(C // 128):
                            self.tr(bkb[:, st * 128:(st + 1) * 128], xe[i][:, st, fc * 128:(fc + 1) * 128], self.ident_b[:], [xeB[i], self.cB], [bB],
                                    signal=(st == C // 128 - 1))
                        self.cp("act" if fc % 2 == 0 else "dve", XeT[i][:, fc, :], bkb[:, 0:C], [bB], [XeTB[i]])
                    for fcb in range(4):
                        G, GB = self.bank[2 + 2 * (fcb % 2)], self.bankB[2 + 2 * (fcb % 2)]
                        U, UB = self.bank[3 + 2 * (fcb % 2)], self.bankB[3 + 2 * (fcb % 2)]
                        for k in range(8):
                            self.mm(G[:, 0:C], wg[i][:, k, fcb * 128:(fcb + 1) * 128], XeT[i][:, k, :], k == 0, k == 7, [wB[i], XeTB[i]], [GB])
                        for k in range(8):
                            self.mm(U[:, 0:C], wu[i][:, k, fcb * 128:(fcb + 1) * 128], XeT[i][:, k, :], k == 0, k == 7, [wB[i], XeTB[i]], [UB])
                        sg_, sgB_ = sgb[fcb % 2], sgB[fcb % 2]
                        self.act(sg_[:], G[:, 0:C], AF.Silu, [GB], [sgB_])
                        self.tt("dve", hdT[:, fcb, :], U[:, 0:C], sg_[:], ALU.mult, [UB, sgB_], [hdB[fcb]])
                    for st in range(C // 128):
                        y_, yB_ = yo[yi % 2], yoB[yi % 2]
                        yi += 1
                        for half in range(2):
                            Y, YB = self.bank[6 + bi % 2], self.bankB[6 + bi % 2]
                            bi += 1
                            for fcb in range(4):
                                self.mm(Y[:, :], hdT[:, fcb, st * 128:(st + 1) * 128], wd[i][:, fcb, half * 512:(half + 1) * 512],
                                        fcb == 0, fcb == 3, [hdB[fcb], wB[i]], [YB])
                            self.cp("act" if half == 0 else "dve", y_[:, half * 512:(half + 1) * 512], Y[:, :], [YB], [yB_])
                        P.dma("sp", ys[e * C + st * 128:e * C + (st + 1) * 128, :], y_[:], reads=[yB_], writes=[ysB])
                P.barrier()
            with ExitStack() as p3:
                gbc = self.sb(p3, "gbc2", [128, D])
                gB = Buf()
                ht = [self.sb(p3, "mht%d" % i, [128, D]) for i in range(2)]
                htB = [Buf() for _ in range(2)]
                g1 = [self.sb(p3, "g1_%d" % i, [128, D]) for i in range(2)]
                g2 = [self.sb(p3, "g2_%d" % i, [128, D]) for i in range(2)]
                gtB = [Buf() for _ in range(2)]
                curv = -1
                for t in range(ntile):
                    v = self.variant(t)
                    if v != curv:
                        self.gate_bcast(p3, l, 40, v, gbc, gB)
                        curv = v
                    i = t % 2
                    P.dma("sp", ht[i][:], self.hbuf[t * 128:(t + 1) * 128, :], reads=[self.hB[t]], writes=[htB[i]])
                    self.memset("pool", g1[i][:], 0.0, [gtB[i]])
                    self.memset("pool", g2[i][:], 0.0, [gtB[i]])
                    self.P.indirect("gather", ys, didx[:, t, 0:1], g1[i][:], NSLOT - 1, reads=[rtB], writes=[gtB[i]])
                    self.P.indirect("gather", ys, didx[:, t, 1:2], g2[i][:], NSLOT - 1, reads=[rtB], writes=[gtB[i]])
                    self.ts("dve", g1[i][:], g1[i][:], wts2[:, t, 0:1], None, ALU.mult, None, [gtB[i], rtB], [gtB[i]])
                    self.stt("dve", g1[i][:], g2[i][:], wts2[:, t, 1:2], g1[i][:], ALU.mult, ALU.add, [gtB[i], rtB], [gtB[i]])
                    self.tt("pool", g1[i][:], g1[i][:], gbc[:], ALU.mult, [gtB[i], gB], [gtB[i]])
                    self.tt("dve", g1[i][:], g1[i][:], ht[i][:], ALU.add, [gtB[i], htB[i]], [gtB[i]])
                    P.dma("sp", self.hbuf[t * 128:(t + 1) * 128, :], g1[i][:], reads=[gtB[i], htB[i]], writes=[self.hB[t]])
                P.barrier()

    def phase_final(self, out):
        P, d = self.P, self.d
        with ExitStack() as ph:
            gbc = self.sb(ph, "gfin", [128, D])
            gB = Buf()
            P.dma("sp", gbc[:], d["gfinal"].partition_broadcast(128), writes=[gB])
            ht = [self.sb(ph, "fht%d" % i, [128, D]) for i in range(2)]
            htB = [Buf() for _ in range(2)]
            junk = self.sb(ph, "fjunk", [128, D])
            jB = Buf()
            ss = self.sb(ph, "fss", [128, 16])
            ssB = Buf()
            oB = Buf("out")
            self.memset("dve", ss[:], 0.0, [ssB])
            for t in range(NLAT // 128):
                i = t % 2
                P.dma("sp", ht[i][:], self.hbuf[t * 128:(t + 1) * 128, :], reads=[self.hB[t]], writes=[htB[i]])
                self.act(junk[:], ht[i][:], AF.Square, [htB[i], ssB], [jB, ssB], accum_out=ss[:, t:t + 1])
            self.rsqrt_inplace(ss[:, 0:NLAT // 128], 1.0 / D, EPS, ssB)
            for t in range(NLAT // 128):
                i = t % 2
                P.dma("sp", ht[i][:], self.hbuf[t * 128:(t + 1) * 128, :], reads=[self.hB[t]], writes=[htB[i]])
                self.stt("dve", ht[i][:], ht[i][:], ss[:, t:t + 1], gbc[:], ALU.mult, ALU.mult, [htB[i], ssB, gB], [htB[i]])
                P.dma("sp", out[t * 128:(t + 1) * 128, :], ht[i][:], reads=[htB[i]], writes=[oB])
            P.barrier()

    def build(self):
        nc = self.nc
        L = self.launch
        with ExitStack() as es:
            self.P = Prog(nc, es)
            layers = {"A": [0], "B": [0, 1], "C": [1], "F": [0, 1]}[L]
            self.need = {"A": set(), "B": {(0, "full")}, "C": {(1, "full")}, "F": {(0, "full"), (1, "full")}}[L]
            self.setup(es, layers)
            self.alloc_kv_scratch()
            P = self.P
            h_in = self.din("h_in", [NTOK, D])
            for t in range(NTOK // 128):
                P.dma("sp", self.hbuf[t * 128:(t + 1) * 128, :], h_in[t * 128:(t + 1) * 128, :], writes=[self.hB[t]])
            stop = self.dbg.get("stop")
            if L == "A":
                send = self.dout("send", [SEND_ROWS, TL], BF16)
                self.phase_cond(0)
                self.phase_norm(0, 1, NTOK)
                self.phase_kv(0, send, Buf("send"))
            elif L in ("B", "C"):
                l = 0 if L == "B" else 1
                last = l == 1
                gath = self.din("gath", [NCORES * SEND_ROWS, TL], BF16)
                gathB = Buf("gath")
                dummy = self.dscr("send_dummy", [SEND_ROWS, TL], BF16)
                self.phase_cond(l)
                self.phase_norm(l, 1, NTOK)
                self.phase_kv(l, dummy, Buf("dummy"))
                if stop != "kv":
                    self.phase_mix(l, last, gath, gathB)
                if stop not in ("kv", "mix"):
                    self.phase_moe(l, last)
                if L == "B":
                    if stop is None:
                        send = self.dout("send", [SEND_ROWS, TL], BF16)
                        self.phase_cond(1)
                        self.phase_norm(1, 1, NTOK)
                        self.phase_kv(1, send, Buf("send"))
                    h_out = self.dout("h_out", [NTOK, D])
                    for t in range(NTOK // 128):
                        P.dma("sp", h_out[t * 128:(t + 1) * 128, :], self.hbuf[t * 128:(t + 1) * 128, :], reads=[self.hB[t]])
                else:
                    self.d["gfinal"] = self.din("gfinal", [1, D])
                    out = self.dout("out", [NLAT, D])
                    self.phase_final(out)
            elif L == "F":
                self.d["gfinal"] = self.din("gfinal", [1, D])
                out = self.dout("out", [NLAT, D])
                for l in (0, 1):
                    last = l == 1
                    send = self.dscr("send%d" % l, [SEND_ROWS, TL], BF16)
                    gath = self.dscr("gath%d" % l, [NCORES * SEND_ROWS, TL], BF16)
                    sendB, gathB = Buf("send"), Buf("gath")
                    self.phase_cond(l)
                    self.phase_norm(l, 1, NTOK)
                    self.phase_kv(l, send, sendB)
                    P.collective(send, gath, [sendB], [gathB])
                    self.phase_mix(l, last, gath, gathB)
                    self.phase_moe(l, last)
                self.phase_final(out)
            P.finish()
        return nc


def _tlayout(v):
    v = np.asarray(v, np.float32)
    return np.ascontiguousarray(v.reshape(-1, 128).T)


def _rope_tables(core):
    t = np.arange(TL) + core * TL
    row = (t // GRID_W).astype(np.float32)
    col = (t % GRID_W).astype(np.float32)
    inv = (10000.0 ** (-np.arange(16, dtype=np.float32) / 16)).astype(np.float32)
    ar = row[:, None] * inv
    ac = col[:, None] * inv
    ang = np.concatenate([ar, ar, ac, ac], axis=-1)
    cos = np.cos(ang).astype(np.float32)
    sin = np.sin(ang).astype(np.float32)
    sgn = np.concatenate([-np.ones(16), np.ones(16), -np.ones(16), np.ones(16)]).astype(np.float32)
    cosT = np.concatenate([cos.T, cos.T], axis=0)
    sinT = np.concatenate([(sin * sgn).T, (sin * sgn).T], axis=0)
    return np.ascontiguousarray(cosT), np.ascontiguousarray(sinT)


def _perm_matrix():
    pm = np.zeros((128, 128), np.float32)
    for m in range(128):
        base, dd = (m // 64) * 64, m % 64
        seg, off = dd // 16, dd % 16
        partner = base + (seg ^ 1) * 16 + off
        pm[partner, m] = 1.0
    return pm


def _common_inputs(inp, core):
    c, c_ctx = inp["c"], inp["c_ctx"]
    condT = np.stack([_tlayout(c[0]), _tlayout(c[1]), _tlayout(c_ctx)], axis=-1)
    cosT, sinT = _rope_tables(core)
    selw = np.zeros((128, 16), np.float32)
    if core > 0:
        selw[:, core - 1] = 1.0
    if core < NCORES - 1:
        selw[:, 8 + core + 1] = 1.0
    return {"condT": np.ascontiguousarray(condT), "ident": np.eye(128, dtype=np.float32), "pm": _perm_matrix(),
            "ropec": cosT, "ropes": sinT, "selw": selw,
            "tri": np.triu(np.ones((128, 128), np.float32), 1),
            "ecb": np.ascontiguousarray(np.broadcast_to(np.arange(NEXP, dtype=np.float32) * MOE_CAP, (128, NEXP)))}


def _nabias(rpb, core):
    g = np.arange(2)[:, None, None, None]
    i = np.arange(8)[None, :, None, None]
    a = np.arange(2)[None, None, :, None]
    jq = np.arange(8)[None, None, None, :]
    kr = 16 * core + 8 * g - 4 + 2 * i + a
    qr = 16 * core + 8 * g + jq
    rs = np.clip(qr - 4, 0, 128 - 8)
    vr = (kr >= 0) & (kr < 128) & (kr >= rs) & (kr < rs + 8)
    dri = np.clip(kr - qr + 7, 0, 14)
    kc = np.arange(64)[:, None]
    qc = np.arange(64)[None, :]
    cs = np.clip(qc - 8, 0, 64 - 16)
    vc = (kc >= cs) & (kc < cs + 16)
    dci = np.clip(kc - qc + 15, 0, 30)
    vals = rpb[:, dri[..., None, None], dci[None, None, None, None]]
    ok = vr[..., None, None] & vc[None, None, None, None]
    vals = np.where(ok[None], vals, np.float32(-1e30)).astype(np.float32)
    vals = vals.transpose(1, 0, 2, 3, 5, 4, 6)
    return np.ascontiguousarray(vals.reshape(2, 8, 8, 128, 512))


def _layer_inputs(inp, l, core, full=False):
    out = {
        "w_ada_%d" % l: inp["w_ada"][l], "b_adaT_%d" % l: _tlayout(inp["b_ada"][l]),
        "gmixT_%d" % l: _tlayout(inp["g_norm_mix"][l]), "gffnT_%d" % l: _tlayout(inp["g_norm_ffn"][l]),
        "w_in_%d" % l: inp["w_in"][l],
    }
    if full:
        out.update({
            "dalam_%d" % l: inp["da_lambda"][l].reshape(1, 256), "dasub_%d" % l: inp["da_subln_g"][l].reshape(128, 1),
            "sglng_%d" % l: inp["sg_ln_g"][l].reshape(1, 512), "sglnb_%d" % l: inp["sg_ln_b"][l].reshape(1, 512),
            "sgwT_%d" % l: np.ascontiguousarray(inp["sg_w"][l].transpose(0, 2, 1)), "sgb_%d" % l: inp["sg_b"][l].reshape(1, 512),
            "nabias_%d" % l: _nabias(inp["na_rpb"][l], core),
            "w_branch_%d" % l: inp["w_branch"][l], "w_out_%d" % l: inp["w_out"][l],
            "wr_%d" % l: np.ascontiguousarray(np.concatenate([inp["moe_w_group"][l], inp["moe_w_router"][l]], axis=1)),
            "br_%d" % l: np.concatenate([inp["moe_b_group"][l], inp["moe_b_router"][l]]).reshape(1, 36),
            "wg_%d" % l: inp["moe_w_gate"][l], "wu_%d" % l: inp["moe_w_up"][l], "wd_%d" % l: inp["moe_w_down"][l],
        })
    return out


def _h0(inp, core):
    x, ctx = inp["x"], inp["ctx"]
    return np.ascontiguousarray(np.concatenate([x[0, core * TL:(core + 1) * TL], x[1, core * TL:(core + 1) * TL], ctx[0], ctx[1]],
                                               axis=0).astype(np.float32))


def _run(kb, maps):
    nc = kb.build()
    in_maps = [{k: np.ascontiguousarray(m[k]) for k in kb.in_names} for m in maps]
    res = run_bass_kernel_spmd(nc, in_maps, core_ids=list(range(NCORES)))
    return res.results


FUSED = True


def _assemble(res):
    out = np.empty((2, SEQ, D), np.float32)
    for c in range(NCORES):
        o = np.asarray(res[c]["out"], np.float32)
        out[0, c * TL:(c + 1) * TL] = o[0:TL]
        out[1, c * TL:(c + 1) * TL] = o[TL:2 * TL]
    return out


def kernel(**inputs):
    inp = {k: np.asarray(v) for k, v in inputs.items()}
    common = [_common_inputs(inp, c) for c in range(NCORES)]
    if FUSED:
        kb = KB("F")
        maps = []
        for c in range(NCORES):
            m = dict(common[c])
            m.update(_layer_inputs(inp, 0, c, full=True))
            m.update(_layer_inputs(inp, 1, c, full=True))
            m["h_in"] = _h0(inp, c)
            m["gfinal"] = inp["g_final"].reshape(1, D)
            maps.append(m)
        return _assemble(_run(kb, maps))
    kbA = KB("A")
    maps = []
    for c in range(NCORES):
        m = dict(common[c])
        m.update(_layer_inputs(inp, 0, c))
        m["h_in"] = _h0(inp, c)
        maps.append(m)
    resA = _run(kbA, maps)
    gath0 = np.concatenate([resA[c]["send"] for c in range(NCORES)], axis=0)
    kbB = KB("B")
    for c in range(NCORES):
        maps[c].update(_layer_inputs(inp, 0, c, full=True))
        maps[c].update(_layer_inputs(inp, 1, c))
        maps[c]["gath"] = gath0
    resB = _run(kbB, maps)
    gath1 = np.concatenate([resB[c]["send"] for c in range(NCORES)], axis=0)
    kbC = KB("C")
    maps2 = []
    for c in range(NCORES):
        m = dict(common[c])
        m.update(_layer_inputs(inp, 1, c, full=True))
        m["h_in"] = resB[c]["h_out"]
        m["gath"] = gath1
        m["gfinal"] = inp["g_final"].reshape(1, D)
        maps2.append(m)
    return _assemble(_run(kbC, maps2))
```

```python
import math
from contextlib import ExitStack

import numpy as np
import concourse.bass as bass
import concourse.mybir as mybir
from concourse.bass_utils import run_bass_kernel_spmd

F32 = mybir.dt.float32
BF16 = mybir.dt.bfloat16
AF = mybir.ActivationFunctionType
ALU = mybir.AluOpType
AX = mybir.AxisListType

NCORES = 8
D = 1024
SEQ = 8192
NCTX = 256
GRID_W = 64
TL = 1024
NLAT = 2 * TL
NTOK = NLAT + 2 * NCTX
EPS = 1e-6
OFF_KA, OFF_VA, OFF_KC, OFF_VC, OFF_QA, OFF_QC, OFF_ZB, OFF_GATE = 0, 512, 1024, 1536, 2048, 2560, 3072, 4096
IN_COLS = 7168
NEXP = 32
DEXP = 512
MOE_CAP = 512
SEC_KA, SEC_VA, SEC_KCH, SEC_VCH, SEC_B = 0, 512, 1024, 1280, 1536
SEND_ROWS = 2 * SEC_B


class Buf:
    __slots__ = ("name", "w", "r")

    def __init__(self, name=""):
        self.name = name
        self.w = None
        self.r = {}


class Prog:
    def __init__(self, nc, es, n_dma_sems=(28, 16)):
        self.nc = nc
        self.engs = {"pe": nc.tensor, "act": nc.scalar, "dve": nc.vector, "pool": nc.gpsimd, "sp": nc.sync}
        self.semobj = {}
        self.cnt = {}
        for e in ["pe", "act", "dve", "pool"]:
            self.semobj[e] = es.enter_context(nc.semaphore("s_" + e))
            self.cnt[e] = 0
        self.dpool = {"sp": [], "pool": []}
        for q, n in zip(["sp", "pool"], n_dma_sems):
            for i in range(n):
                k = "d_%s_%d" % (q, i)
                self.semobj[k] = es.enter_context(nc.semaphore(k))
                self.cnt[k] = 0
                self.dpool[q].append(k)
        self.semobj["cc"] = es.enter_context(nc.semaphore("s_cc"))
        self.cnt["cc"] = 0
        self.drr = {"sp": 0, "pool": 0}
        self.waited = {e: {} for e in self.engs}
        self.nins = 0

    def _collect(self, reads, writes):
        need = {}

        def add(k, v):
            if need.get(k, 0) < v:
                need[k] = v

        for b in reads:
            if b.w is not None:
                add(*b.w)
        for b in writes:
            if b.w is not None:
                add(*b.w)
            for k, v in b.r.items():
                add(k, v)
        return need

    def _emit_waits(self, eng, need):
        w = self.waited[eng]
        e = self.engs[eng]
        for k, v in need.items():
            if eng == "pe" and k == "pe":
                continue
            if w.get(k, 0) < v:
                e.wait_ge(self.semobj[k], v)
                w[k] = v
                self.nins += 1

    def _update(self, tok, reads, writes):
        k, v = tok
        for b in reads:
            if b.r.get(k, 0) < v:
                b.r[k] = v
        for b in writes:
            b.w = tok
            b.r = {}

    def op(self, eng, fn, reads=(), writes=(), signal=True):
        self._emit_waits(eng, self._collect(reads, writes))
        ins = fn(self.engs[eng])
        self.nins += 1
        if signal:
            ins.then_inc(self.semobj[eng], 1)
            self.cnt[eng] += 1
            tok = (eng, self.cnt[eng])
        else:
            tok = (eng, self.cnt[eng] + 1)
        self._update(tok, reads, writes)
        return tok

    def dma(self, q, out, in_, reads=(), writes=(), **kw):
        need = self._collect(reads, writes)
        pool = self.dpool[q]
        k = pool[self.drr[q] % len(pool)]
        self.drr[q] += 1
        if self.cnt[k] > 0 and need.get(k, 0) < self.cnt[k]:
            need[k] = self.cnt[k]
        self._emit_waits(q, need)
        ins = self.engs[q].dma_start(out=out, in_=in_, **kw)
        self.nins += 1
        ins.then_inc(self.semobj[k], 16)
        self.cnt[k] += 16
        tok = (k, self.cnt[k])
        self._update(tok, reads, writes)
        return tok

    def indirect(self, kind, dram, idx, sb_ap, bound, reads=(), writes=()):
        q = "pool"
        need = self._collect(reads, writes)
        pool = self.dpool[q]
        k = pool[self.drr[q] % len(pool)]
        self.drr[q] += 1
        if self.cnt[k] > 0 and need.get(k, 0) < self.cnt[k]:
            need[k] = self.cnt[k]
        self._emit_waits(q, need)
        off = bass.IndirectOffsetOnAxis(ap=idx, axis=0)
        if not hasattr(self, "_bregs"):
            self._bregs = {}
        if bound not in self._bregs:
            self._bregs[bound] = self.nc.gpsimd.to_reg(bound)
        bound = self._bregs[bound]
        if kind == "scatter":
            ins = self.nc.gpsimd.indirect_dma_start(out=dram[:, :], out_offset=off, in_=sb_ap, in_offset=None, bounds_check=bound,
                                                    oob_is_err=False)
        else:
            ins = self.nc.gpsimd.indirect_dma_start(out=sb_ap, out_offset=None, in_=dram[:, :], in_offset=off, bounds_check=bound,
                                                    oob_is_err=False)
        self.nins += 1
        ins.then_inc(self.semobj[k], 16)
        self.cnt[k] += 16
        tok = (k, self.cnt[k])
        self._update(tok, reads, writes)
        return tok

    def collective(self, in_ap, out_ap, reads=(), writes=()):
        self._emit_waits("pool", self._collect(reads, writes))
        ins = self.nc.gpsimd.collective_compute("AllGather", ALU.bypass, replica_groups=[list(range(NCORES))],
                                                ins=[in_ap.opt()], outs=[out_ap.opt()])
        self.nins += 1
        ins.then_inc(self.semobj["cc"], 1)
        self.cnt["cc"] += 1
        tok = ("cc", self.cnt["cc"])
        self._update(tok, reads, writes)
        return tok

    def all_counts(self):
        return {k: v for k, v in self.cnt.items() if v > 0}

    def barrier(self):
        need = self.all_counts()
        for e in self.engs:
            self._emit_waits(e, need)

    def finish(self):
        self._emit_waits("sp", self.all_counts())


class KB:
    def __init__(self, launch, dbg=None):
        self.launch = launch
        self.dbg = dbg or {}
        self.nc = bass.Bass("TRN2", target_bir_lowering=False)
        self.in_names = []
        self.out_names = []

    def din(self, name, shape, dt=F32):
        self.in_names.append(name)
        return self.nc.dram_tensor(name, list(shape), dt, kind="ExternalInput").ap()

    def dout(self, name, shape, dt=F32):
        self.out_names.append(name)
        return self.nc.dram_tensor(name, list(shape), dt, kind="ExternalOutput").ap()

    def dscr(self, name, shape, dt=F32):
        return self.nc.dram_tensor(name, list(shape), dt).ap()

    def sb(self, es, name, shape, dt=F32):
        self._uid = getattr(self, "_uid", 0) + 1
        return es.enter_context(self.nc.sbuf_tensor("sb%d_%s" % (self._uid, name), list(shape), dt))

    def mm(self, out, lhsT, rhs, start, stop, rd, wr, signal=None):
        if signal is None:
            signal = stop
        return self.P.op("pe", lambda e: e.matmul(out, lhsT=lhsT, rhs=rhs, start=start, stop=stop), reads=rd, writes=wr,
                         signal=signal)

    def tr(self, out, in_, ident, rd, wr, signal=True):
        return self.P.op("pe", lambda e: e.transpose(out=out, in_=in_, identity=ident), reads=rd, writes=wr, signal=signal)

    def act(self, out, in_, func, rd, wr, bias=None, scale=None, accum_out=None):
        kw = {}
        if bias is not None:
            kw["bias"] = bias
        if scale is not None:
            kw["scale"] = scale
        if accum_out is not None:
            kw["accum_out"] = accum_out
        return self.P.op("act", lambda e: e.activation(out=out, in_=in_, func=func, **kw), reads=rd, writes=wr)

    def tt(self, eng, out, in0, in1, op, rd, wr):
        return self.P.op(eng, lambda e: e.tensor_tensor(out=out, in0=in0, in1=in1, op=op), reads=rd, writes=wr)

    def ts(self, eng, out, in0, s1, s2, op0, op1, rd, wr):
        if op1 is None:
            return self.P.op(eng, lambda e: e.tensor_scalar(out=out, in0=in0, scalar1=s1, scalar2=None, op0=op0), reads=rd,
                             writes=wr)
        return self.P.op(eng, lambda e: e.tensor_scalar(out=out, in0=in0, scalar1=s1, scalar2=s2, op0=op0, op1=op1),
                         reads=rd, writes=wr)

    def stt(self, eng, out, in0, scalar, in1, op0, op1, rd, wr):
        return self.P.op(eng, lambda e: e.scalar_tensor_tensor(out=out, in0=in0, scalar=scalar, in1=in1, op0=op0, op1=op1),
                         reads=rd, writes=wr)

    def cp(self, eng, out, in_, rd, wr):
        if eng == "act":
            return self.P.op("act", lambda e: e.copy(out=out, in_=in_), reads=rd, writes=wr)
        return self.P.op(eng, lambda e: e.tensor_copy(out=out, in_=in_), reads=rd, writes=wr)

    def recip(self, out, in_, rd, wr):
        return self.P.op("dve", lambda e: e.reciprocal(out=out, in_=in_), reads=rd, writes=wr)

    def memset(self, eng, ap, val, wr):
        return self.P.op(eng, lambda e: e.memset(ap, val), writes=wr)

    def rsqrt_inplace(self, ap, mult, add, buf):
        self.ts("dve", ap, ap, mult, add, ALU.mult, ALU.add, [buf], [buf])
        self.act(ap, ap, AF.Sqrt, [buf], [buf])
        self.recip(ap, ap, [buf], [buf])

    def setup(self, es, layers):
        nc = self.nc
        d = {}
        d["condT"] = self.din("condT", [128, 8, 3])
        d["ident"] = self.din("ident", [128, 128])
        d["pm"] = self.din("pm", [128, 128])
        d["ropec"] = self.din("ropec", [128, TL])
        d["ropes"] = self.din("ropes", [128, TL])
        d["selw"] = self.din("selw", [128, 16])
        d["tri"] = self.din("tri", [128, 128])
        d["ecb"] = self.din("ecb", [128, NEXP])
        for l in layers:
            d["w_ada_%d" % l] = self.din("w_ada_%d" % l, [D, 6 * D])
            d["b_adaT_%d" % l] = self.din("b_adaT_%d" % l, [128, 48])
            d["gmixT_%d" % l] = self.din("gmixT_%d" % l, [128, 8])
            d["gffnT_%d" % l] = self.din("gffnT_%d" % l, [128, 8])
            d["w_in_%d" % l] = self.din("w_in_%d" % l, [D, IN_COLS])
            if (l, "full") in self.need:
                d["dalam_%d" % l] = self.din("dalam_%d" % l, [1, 256])
                d["dasub_%d" % l] = self.din("dasub_%d" % l, [128, 1])
                d["sglng_%d" % l] = self.din("sglng_%d" % l, [1, 512])
                d["sglnb_%d" % l] = self.din("sglnb_%d" % l, [1, 512])
                d["sgwT_%d" % l] = self.din("sgwT_%d" % l, [4, 128, 128])
                d["sgb_%d" % l] = self.din("sgb_%d" % l, [1, 512])
                d["nabias_%d" % l] = self.din("nabias_%d" % l, [2, 8, 8, 128, 512])
                d["w_branch_%d" % l] = self.din("w_branch_%d" % l, [3, 512, D])
                d["w_out_%d" % l] = self.din("w_out_%d" % l, [D, D])
                d["wr_%d" % l] = self.din("wr_%d" % l, [D, 36])
                d["br_%d" % l] = self.din("br_%d" % l, [1, 36])
                d["wg_%d" % l] = self.din("wg_%d" % l, [NEXP, D, DEXP])
                d["wu_%d" % l] = self.din("wu_%d" % l, [NEXP, D, DEXP])
                d["wd_%d" % l] = self.din("wd_%d" % l, [NEXP, DEXP, D])
        self.d = d
        P = self.P
        self.bank = [es.enter_context(nc.psum_tensor("bank%d" % i, [128, 512], F32)) for i in range(8)]
        self.bankB = [Buf("bank%d" % i) for i in range(8)]
        self.ident = self.sb(es, "ident_f", [128, 128])
        self.pm = self.sb(es, "pm_f", [128, 128])
        self.ones_f = self.sb(es, "ones_f", [128, 128])
        self.ones_b = self.sb(es, "ones_b", [128, 128], BF16)
        self.ident_b = self.sb(es, "ident_b", [128, 128], BF16)
        self.cB = Buf("consts")
        P.dma("sp", self.ident[:], d["ident"], writes=[self.cB])
        P.dma("sp", self.pm[:], d["pm"], writes=[self.cB])
        self.memset("dve", self.ones_f[:], 1.0, [self.cB])
        self.memset("dve", self.ones_b[:], 1.0, [self.cB])
        self.cp("dve", self.ident_b[:], self.ident[:], [self.cB], [self.cB])
        self.selw = self.sb(es, "selw", [128, 16])
        P.dma("sp", self.selw[:], d["selw"], writes=[self.cB])
        self.condS = self.sb(es, "condS", [128, 8, 3])
        self.condSB = Buf("condS")
        P.dma("sp", self.condS[:], d["condT"], writes=[self.condSB])
        self.act(self.condS[:], self.condS[:], AF.Silu, [self.condSB], [self.condSB])
        self.modT = {}
        self.modTB = {}
        self.s1T = {}
        self.s2T = {}
        for l in layers:
            self.modT[l] = self.sb(es, "modT%d" % l, [128, 48, 3])
            self.s1T[l] = self.sb(es, "s1T%d" % l, [128, 8, 3])
            self.s2T[l] = self.sb(es, "s2T%d" % l, [128, 8, 3])
            self.modTB[l] = Buf("modT%d" % l)
        self.xnT = self.sb(es, "xnT", [128, 8, NTOK], BF16)
        self.xnTB = [Buf("xnT%d" % i) for i in range(NTOK // 128)]
        self.hbuf = self.dscr("hbuf", [NTOK, D])
        self.hB = [Buf("h%d" % i) for i in range(NTOK // 128)]

    @staticmethod
    def variant(tile):
        return 0 if tile < 8 else (1 if tile < 16 else 2)

    def phase_cond(self, l):
        P = self.P
        d = self.d
        with ExitStack() as ph:
            wblk = [self.sb(ph, "wada%d" % i, [128, 8, 512]) for i in range(2)]
            wB = [Buf() for _ in range(2)]
            bT = self.sb(ph, "badaT", [128, 48])
            g1 = self.sb(ph, "g1T", [128, 8])
            g2 = self.sb(ph, "g2T", [128, 8])
            sB = Buf()
            P.dma("sp", bT[:], d["b_adaT_%d" % l], writes=[sB])
            P.dma("sp", g1[:], d["gmixT_%d" % l], writes=[sB])
            P.dma("sp", g2[:], d["gffnT_%d" % l], writes=[sB])
            ps, psB = self.bank[0], self.bankB[0]
            for cb in range(12):
                w, B = wblk[cb % 2], wB[cb % 2]
                P.dma("sp", w[:], d["w_ada_%d" % l][:, cb * 512:(cb + 1) * 512].rearrange("(k p) c -> p k c", p=128),
                      writes=[B])
                for j in range(4):
                    cc = cb * 4 + j
                    for k in range(8):
                        self.mm(ps[:, cc * 3:(cc + 1) * 3], w[:, k, j * 128:(j + 1) * 128], self.condS[:, k, :], k == 0, k == 7,
                                [B, self.condSB], [psB])
            modT, mB = self.modT[l], self.modTB[l]
            self.tt("dve", modT[:], ps[:, 0:144].rearrange("p (c v) -> p c v", v=3),
                    bT[:].unsqueeze(2).to_broadcast([128, 48, 3]), ALU.add, [psB, sB], [mB])
            self.stt("dve", self.s1T[l][:], modT[:, 8:16, :], 1.0, g1[:].unsqueeze(2).to_broadcast([128, 8, 3]), ALU.add,
                     ALU.mult, [mB, sB], [mB])
            self.stt("dve", self.s2T[l][:], modT[:, 32:40, :], 1.0, g2[:].unsqueeze(2).to_broadcast([128, 8, 3]), ALU.add,
                     ALU.mult, [mB, sB], [mB])
            P.barrier()

    def phase_norm(self, l, sub, ntok, route=None):
        P = self.P
        ntile = ntok // 128
        scaleT = self.s1T[l] if sub == 1 else self.s2T[l]
        sh0 = 0 if sub == 1 else 24
        mB = self.modTB[l]
        with ExitStack() as ph:
            ht = [self.sb(ph, "ht%d" % i, [128, D]) for i in range(2)]
            htB = [Buf() for _ in range(2)]
            hn = [self.sb(ph, "hn%d" % i, [128, D]) for i in range(2)]
            hnB = [Buf() for _ in range(2)]
            tmp = [self.sb(ph, "ntmp%d" % i, [128, 4, 128]) for i in range(2)]
            tmpB = [Buf() for _ in range(2)]
            junk = self.sb(ph, "junk", [128, D])
            junkB = Buf()
            ss = self.sb(ph, "ss", [128, 32])
            ssB = Buf()
            self.memset("dve", ss[:], 0.0, [ssB])
            for t in range(ntile):
                i = t % 2
                P.dma("sp", ht[i][:], self.hbuf[t * 128:(t + 1) * 128, :], reads=[self.hB[t]], writes=[htB[i]])
                self.act(junk[:], ht[i][:], AF.Square, [htB[i], ssB], [junkB, ssB], accum_out=ss[:, t:t + 1])
            self.rsqrt_inplace(ss[:, 0:ntile], 1.0 / D, EPS, ssB)
            if route is not None:
                route["begin"](ph)
            for t in range(ntile):
                i = t % 2
                v = self.variant(t)
                P.dma("sp", ht[i][:], self.hbuf[t * 128:(t + 1) * 128, :], reads=[self.hB[t]], writes=[htB[i]])
                self.act(hn[i][:], ht[i][:], AF.Copy, [htB[i], ssB], [hnB[i]], scale=ss[:, t:t + 1])
                for half in range(2):
                    bk, bB = self.bank[half], self.bankB[half]
                    for c in range(4):
                        cc = half * 4 + c
                        self.tr(bk[:, c * 128:(c + 1) * 128], hn[i][:, cc * 128:(cc + 1) * 128], self.ident[:], [hnB[i], self.cB],
                                [bB], signal=(c == 3))
                    c0 = half * 4
                    j = half
                    self.tt("dve", tmp[j][:], bk[:].rearrange("p (c t) -> p c t", c=4),
                            scaleT[:, c0:c0 + 4, v].unsqueeze(2).to_broadcast([128, 4, 128]), ALU.mult, [bB, mB], [tmpB[j]])
                    shift = self.modT[l][:, sh0 + c0:sh0 + c0 + 4, v].unsqueeze(2).to_broadcast([128, 4, 128])
                    if route is None:
                        self.tt("pool", self.xnT[:, c0:c0 + 4, t * 128:(t + 1) * 128], tmp[j][:], shift, ALU.add, [tmpB[j], mB],
                                [self.xnTB[t]])
                    else:
                        self.tt("dve", tmp[j][:], tmp[j][:], shift, ALU.add, [tmpB[j], mB], [tmpB[j]])
                        self.cp("pool", self.xnT[:, c0:c0 + 4, t * 128:(t + 1) * 128], tmp[j][:], [tmpB[j]], [self.xnTB[t]])
                        route["tile"](t, half, tmp[j], tmpB[j])
            P.barrier()

    def load_w(self, dst, dstB, src, nsplit=1):
        n = src.shape[1]
        step = n // nsplit
        for s in range(nsplit):
            self.P.dma("pool", dst[:, :, s * step:(s + 1) * step],
                       src[:, s * step:(s + 1) * step].rearrange("(k p) c -> p k c", p=128), writes=[dstB])

    def proj_fm(self, bank, bankB, W, WB, col0, tok0, ntok, ncol=128):
        rd = [WB] + self.xnTB[tok0 // 128:(tok0 + ntok + 127) // 128]
        for k in range(8):
            self.mm(bank[0:ncol, 0:ntok], W[:, k, col0:col0 + ncol], self.xnT[:, k, tok0:tok0 + ntok], k == 0, k == 7, rd, [bankB])

    def proj_tm(self, bank, bankB, W, WB, col0, ncol, tok0):
        rd = [WB, self.xnTB[tok0 // 128]]
        for k in range(8):
            self.mm(bank[:, 0:ncol], self.xnT[:, k, tok0:tok0 + 128], W[:, k, col0:col0 + ncol], k == 0, k == 7, rd, [bankB])

    def rope_evac(self, ph_state, bank, bankB, rbank, rbankB, dst, dstB, pos0, n):
        st = ph_state
        i = st["i"] = (st.get("i", -1) + 1) % 2
        xs, xsB = st["xs"][i], st["xsB"][i]
        t1, t1B = st["t1"][i], st["t1B"][i]
        self.cp("act", xs[:, 0:n], bank[:, 0:n], [bankB], [xsB])
        self.mm(rbank[:, 0:n], self.pm[:], xs[:, 0:n], True, True, [self.cB, xsB], [rbankB])
        self.tt("dve", t1[:, 0:n], xs[:, 0:n], self.ropec[:, pos0:pos0 + n], ALU.mult, [xsB, self.ropeB], [t1B])
        self.tt("dve", xs[:, 0:n], rbank[:, 0:n], self.ropes[:, pos0:pos0 + n], ALU.mult, [rbankB, self.ropeB, xsB], [xsB])
        self.tt("pool", dst, t1[:, 0:n], xs[:, 0:n], ALU.add, [t1B, xsB], [dstB])

    def rope_state(self, ph):
        self.ropec = self.sb(ph, "ropec", [128, TL])
        self.ropes = self.sb(ph, "ropes", [128, TL])
        self.ropeB = Buf("rope")
        self.P.dma("sp", self.ropec[:], self.d["ropec"], writes=[self.ropeB])
        self.P.dma("sp", self.ropes[:], self.d["ropes"], writes=[self.ropeB])
        return {"xs": [self.sb(ph, "rxs%d" % i, [128, 512]) for i in range(2)], "xsB": [Buf() for _ in range(2)],
                "t1": [self.sb(ph, "rt1%d" % i, [128, 512]) for i in range(2)], "t1B": [Buf() for _ in range(2)]}

    def sec(self, buf, rank, b, sec0, nrows):
        r0 = rank * SEND_ROWS + b * SEC_B + sec0
        return buf[r0:r0 + nrows, :]

    def phase_kv(self, l, send, sendB):
        P = self.P
        d = self.d
        with ExitStack() as ph:
            W = self.sb(ph, "wkv", [128, 8, 2048], BF16)
            WB = Buf()
            self.load_w(W, WB, d["w_in_%d" % l][:, 0:2048], nsplit=4)
            rs = self.rope_state(ph)
            st = [self.sb(ph, "kvst%d" % i, [128, 512], BF16) for i in range(4)]
            stB = [Buf() for _ in range(4)]
            si = 0
            for b in range(2):
                tok0 = b * TL
                ctok0 = NLAT + b * NCTX
                for cc in range(4):
                    for half in range(2):
                        bk, bB = self.bank[si % 2], self.bankB[si % 2]
                        s, sB = st[si % 4], stB[si % 4]
                        self.proj_fm(bk, bB, W, WB, OFF_KA + cc * 128, tok0 + half * 512, 512)
                        self.rope_evac(rs, bk, bB, self.bank[2 + si % 2], self.bankB[2 + si % 2], s[:], sB, half * 512, 512)
                        P.dma("sp", self.sec(send, 0, b, SEC_KA + cc * 128, 128)[:, half * 512:(half + 1) * 512], s[:],
                              reads=[sB], writes=[sendB])
                        si += 1
                    bk, bB = self.bank[si % 2], self.bankB[si % 2]
                    s, sB = st[si % 4], stB[si % 4]
                    self.proj_fm(bk, bB, W, WB, OFF_KA + cc * 128, ctok0, NCTX)
                    self.cp("act", s[:, 0:NCTX], bk[:, 0:NCTX], [bB], [sB])
                    P.dma("sp", self.ckaT[b][cc * 128:(cc + 1) * 128, :], s[:, 0:NCTX], reads=[sB], writes=[self.ckvB])
                    si += 1
                for cc in range(4):
                    for half in range(2):
                        bk, bB = self.bank[si % 2], self.bankB[si % 2]
                        s, sB = st[si % 4], stB[si % 4]
                        self.proj_fm(bk, bB, W, WB, OFF_KC + cc * 128, tok0 + half * 512, 512)
                        self.cp("act", s[:], bk[:], [bB], [sB])
                        P.dma("sp", self.kcT[b][cc * 128:(cc + 1) * 128, half * 512:(half + 1) * 512], s[:], reads=[sB],
                              writes=[self.kvownB])
                        hsec = self.sec(send, 0, b, SEC_KCH, 256).rearrange("a (two t) -> (a two) t", two=2)
                        src = s[:, 0:256] if half == 0 else s[:, 256:512]
                        P.dma("sp", hsec[cc * 128:(cc + 1) * 128, half * 256:(half + 1) * 256], src, reads=[sB], writes=[sendB])
                        si += 1
                    bk, bB = self.bank[si % 2], self.bankB[si % 2]
                    s, sB = st[si % 4], stB[si % 4]
                    self.proj_fm(bk, bB, W, WB, OFF_KC + cc * 128, ctok0, NCTX)
                    self.cp("act", s[:, 0:NCTX], bk[:, 0:NCTX], [bB], [sB])
                    P.dma("sp", self.ckcT[b][cc * 128:(cc + 1) * 128, :], s[:, 0:NCTX], reads=[sB], writes=[self.ckvB])
                    si += 1
                vasec = self.sec(send, 0, b, SEC_VA, 512).rearrange("r c -> (r c)").rearrange("(h t e) -> t h e", h=4, t=TL)
                vchsec = self.sec(send, 0, b, SEC_VCH, 256).rearrange("a (two e) -> (a two) e", two=2)
                for t in range(10):
                    lat = t < 8
                    tk = tok0 + t * 128 if lat else ctok0 + (t - 8) * 128
                    for which in range(2):
                        bk, bB = self.bank[si % 2], self.bankB[si % 2]
                        s, sB = st[si % 4], stB[si % 4]
                        self.proj_tm(bk, bB, W, WB, OFF_VA if which == 0 else OFF_VC, 512, tk)
                        self.cp("act" if which == 0 else "dve", s[:], bk[:], [bB], [sB])
                        if which == 0:
                            if lat:
                                P.dma("sp", vasec[t * 128:(t + 1) * 128, :, :], s[:].rearrange("p (h e) -> p h e", h=4),
                                      reads=[sB], writes=[sendB])
                            else:
                                P.dma("sp", self.cva[b][(t - 8) * 128:(t - 7) * 128, :], s[:], reads=[sB], writes=[self.ckvB])
                        else:
                            if lat:
                                P.dma("sp", self.vc[b][t * 128:(t + 1) * 128, :], s[:], reads=[sB], writes=[self.kvownB])
                                if t < 2:
                                    P.dma("sp", vchsec[t * 128:(t + 1) * 128, :], s[:], reads=[sB], writes=[sendB])
                                elif t >= 6:
                                    P.dma("sp", vchsec[(t - 4) * 128:(t - 3) * 128, :], s[:], reads=[sB], writes=[sendB])
                            else:
                                P.dma("sp", self.cvc[b][(t - 8) * 128:(t - 7) * 128, :], s[:], reads=[sB], writes=[self.ckvB])
                        si += 1
            P.barrier()

    def alloc_kv_scratch(self):
        self.ckaT = [self.dscr("ckaT%d" % b, [512, NCTX], BF16) for b in range(2)]
        self.ckcT = [self.dscr("ckcT%d" % b, [512, NCTX], BF16) for b in range(2)]
        self.cva = [self.dscr("cva%d" % b, [NCTX, 512], BF16) for b in range(2)]
        self.cvc = [self.dscr("cvc%d" % b, [NCTX, 512], BF16) for b in range(2)]
        self.kcT = [self.dscr("kcT%d" % b, [512, TL], BF16) for b in range(2)]
        self.vc = [self.dscr("vc%d" % b, [TL, 512], BF16) for b in range(2)]
        self.ckvB = Buf("ckv")
        self.kvownB = Buf("kvown")

    def bcast_rows(self, ph, vecT_fn, nchunk, dst, dstB, rdB):
        tmpd = [self.sb(ph, "bct%d" % i, [128, 128]) for i in range(2)]
        tB = [Buf() for _ in range(2)]
        for c in range(nchunk):
            i = c % 2
            bk, bB = self.bank[(c // 4) % 2], self.bankB[(c // 4) % 2]
            self.ts("dve", tmpd[i][:], self.ident[:], vecT_fn(c), None, ALU.mult, None, [self.cB] + rdB, [tB[i]])
            self.mm(bk[:, (c % 4) * 128:(c % 4 + 1) * 128], self.ones_f[:], tmpd[i][:], True, True, [self.cB, tB[i]], [bB])
            if c % 4 == 3 or c == nchunk - 1:
                c0 = (c // 4) * 4
                n = (c - c0 + 1) * 128
                self.cp("act", dst[:, c0 * 128:c0 * 128 + n], bk[:, 0:n], [bB], [dstB])

    def merge_setup(self, ph, l, i, K):
        nparts = 512 // K
        Wb = self.sb(ph, "wb%d" % i, [K, nparts, D], BF16)
        WbB = Buf()
        self.P.dma("pool", Wb[:], self.d["w_branch_%d" % l][i].rearrange("(j p) c -> p j c", p=K), writes=[WbB])
        Wg = self.sb(ph, "wgate%d" % i, [128, 8, D], BF16)
        WgB = Buf()
        self.load_w(Wg, WgB, self.d["w_in_%d" % l][:, OFF_GATE + i * D:OFF_GATE + (i + 1) * D], nsplit=2)
        sg = [self.sb(ph, "sgt%d_%d" % (i, j), [128, 512]) for j in range(2)]
        sgB = [Buf() for _ in range(2)]
        return {"Wb": Wb, "WbB": WbB, "Wg": Wg, "WgB": WgB, "K": K, "nparts": nparts, "sg": sg, "sgB": sgB, "n": 0}

    def merge(self, ms, yget, yB, tok0, ntok, first, banks=(6, 7), banks2=(4, 5)):
        K, nparts = ms["K"], ms["nparts"]
        for s0 in range(0, ntok, 512):
            n = min(512, ntok - s0)
            t0 = tok0 + s0
            mB = self.mTB[t0 // 128:(t0 + n + 127) // 128]
            for fc in range(8):
                pair = banks if fc % 2 == 0 else (banks2 if banks2 is not None else banks)
                bA, bAB = self.bank[pair[0]], self.bankB[pair[0]]
                bG, bGB = self.bank[pair[1]], self.bankB[pair[1]]
                for j in range(nparts):
                    self.mm(bA[:, 0:n], ms["Wb"][0:K, j, fc * 128:(fc + 1) * 128], yget(j, s0, n), j == 0, j == nparts - 1,
                            [ms["WbB"]] + yB, [bAB])
                self.proj_fm(bG, bGB, ms["Wg"], ms["WgB"], fc * 128, t0, n)
                i = ms["n"] = (ms["n"] + 1) % 2
                sg, sgB = ms["sg"][i], ms["sgB"][i]
                self.act(sg[:, 0:n], bG[:, 0:n], AF.Sigmoid, [bGB], [sgB])
                if first:
                    self.tt("dve", self.mT[:, fc, t0:t0 + n], bA[:, 0:n], sg[:, 0:n], ALU.mult, [bAB, sgB], mB)
                else:
                    self.tt("dve", sg[:, 0:n], bA[:, 0:n], sg[:, 0:n], ALU.mult, [bAB, sgB], [sgB])
                    self.tt("pool", self.mT[:, fc, t0:t0 + n], self.mT[:, fc, t0:t0 + n], sg[:, 0:n], ALU.add, [sgB] + mB, mB)

    def mixer_b(self, l, last):
        P, d = self.P, self.d
        nchunk = (NLAT if last else NTOK) // 128
        with ExitStack() as ph:
            W = self.sb(ph, "wzb", [128, 8, 1024], BF16)
            WB = Buf()
            self.load_w(W, WB, d["w_in_%d" % l][:, OFF_ZB:OFF_ZB + 1024], nsplit=2)
            wsT = self.sb(ph, "wsT", [128, 4, 128], BF16)
            cB = Buf()
            P.dma("pool", wsT[:], d["sgwT_%d" % l].rearrange("g q p -> q g p"), writes=[cB])
            bsbc = self.sb(ph, "bsbc", [128, 512])
            lng = self.sb(ph, "lng", [128, 512])
            lnb = self.sb(ph, "lnb", [128, 512])
            P.dma("sp", bsbc[:], d["sgb_%d" % l].partition_broadcast(128), writes=[cB])
            P.dma("sp", lng[:], d["sglng_%d" % l].partition_broadcast(128), writes=[cB])
            P.dma("sp", lnb[:], d["sglnb_%d" % l].partition_broadcast(128), writes=[cB])
            ms = self.merge_setup(ph, l, 1, 128)
            uT = [self.sb(ph, "uT%d" % i, [128, 512]) for i in range(2)]
            uB = [Buf() for _ in range(2)]
            vs = [self.sb(ph, "vs%d" % i, [128, 512]) for i in range(2)]
            vB = [Buf() for _ in range(2)]
            vvb = [self.sb(ph, "vvb%d" % i, [128, 512], BF16) for i in range(2)]
            vvB = [Buf() for _ in range(2)]
            junk = self.sb(ph, "sgjunk", [128, 512])
            jB = Buf()
            st = self.sb(ph, "sgst", [128, 8])
            stB = Buf()
            ybT = [self.sb(ph, "ybT%d" % i, [128, 4, 512], BF16) for i in range(2)]
            ybB = [Buf() for _ in range(2)]
            blocks = [(0, 4), (4, 4), (8, 4), (12, 4)] + ([] if last else [(16, 2), (18, 2)])
            for bi, (ch0, nch) in enumerate(blocks):
                yb, yB = ybT[bi % 2], ybB[bi % 2]
                for cj in range(nch):
                    ch = ch0 + cj
                    i = ch % 2
                    tok0 = ch * 128
                    bu, buB = self.bank[0 + i], self.bankB[0 + i]
                    bv, bvB = self.bank[2 + i], self.bankB[2 + i]
                    bs, bsB = self.bank[4 + i], self.bankB[4 + i]
                    for g in range(4):
                        rd = [WB, self.xnTB[ch]]
                        for k in range(8):
                            self.mm(bu[:, g * 128:(g + 1) * 128], W[:, k, g * 128:(g + 1) * 128], self.xnT[:, k, tok0:tok0 + 128],
                                    k == 0, k == 7, rd, [buB], signal=(k == 7 and g == 3))
                    self.act(uT[i][:], bu[:], AF.Gelu_apprx_tanh, [buB], [uB[i]])
                    self.proj_tm(bv, bvB, W, WB, 512, 512, tok0)
                    self.memset("pool", st[:], 0.0, [stB])
                    self.act(vs[i][:], bv[:], AF.Gelu_apprx_tanh, [bvB, stB], [vB[i], stB], accum_out=st[:, 0:1])
                    self.ts("dve", st[:, 1:2], st[:, 0:1], -1.0 / 512, None, ALU.mult, None, [stB], [stB])
                    self.ts("dve", vs[i][:], vs[i][:], st[:, 1:2], None, ALU.add, None, [vB[i], stB], [vB[i]])
                    self.act(junk[:], vs[i][:], AF.Square, [vB[i], stB], [jB, stB], accum_out=st[:, 2:3])
                    self.rsqrt_inplace(st[:, 2:3], 1.0 / 512, EPS, stB)
                    self.stt("dve", vs[i][:], vs[i][:], st[:, 2:3], lng[:], ALU.mult, ALU.mult, [vB[i], stB, cB], [vB[i]])
                    self.tt("pool", vvb[i][:], vs[i][:], lnb[:], ALU.add, [vB[i], cB], [vvB[i]])
                    for g in range(4):
                        self.mm(bs[:, g * 128:(g + 1) * 128], vvb[i][:, g * 128:(g + 1) * 128], wsT[:, g, :], True, True,
                                [vvB[i], cB], [bsB], signal=(g == 3))
                    self.tt("dve", vs[i][:], bs[:], bsbc[:], ALU.add, [bsB, cB, vB[i]], [vB[i]])
                    self.tt("pool", yb[:, :, cj * 128:(cj + 1) * 128], vs[i][:].rearrange("p (g t) -> p g t", g=4),
                            uT[i][:].rearrange("p (g t) -> p g t", g=4), ALU.mult, [vB[i], uB[i]], [yB])
                self.merge(ms, lambda j, s0, n, yb=yb: yb[:, j, s0:s0 + n], [yB], ch0 * 128, nch * 128, True)
            P.barrier()

    def mixer_a(self, l, last, gath, gathB):
        P, d = self.P, self.d
        lam_init = 0.8 - 0.6 * math.exp(-0.3 * l)
        with ExitStack() as ph:
            W = self.sb(ph, "wqa", [128, 8, 512], BF16)
            WB = Buf()
            self.load_w(W, WB, d["w_in_%d" % l][:, OFF_QA:OFF_QA + 512])
            rs = self.rope_state(ph)
            ms = self.merge_setup(ph, l, 0, 128)
            lamt = self.sb(ph, "lamt", [128, 256])
            lamp = self.sb(ph, "lamp", [128, 128])
            lams = self.sb(ph, "lams", [128, 8])
            lB = Buf()
            P.dma("sp", lamt[:], d["dalam_%d" % l].partition_broadcast(128), writes=[lB])
            P.dma("sp", lams[:, 4:5], d["dasub_%d" % l], writes=[lB])
            self.tt("dve", lamp[:].rearrange("p (m d) -> p m d", m=2), lamt[:].rearrange("p (m k d) -> p m k d", m=2, k=2)[:, :, 0, :],
                    lamt[:].rearrange("p (m k d) -> p m k d", m=2, k=2)[:, :, 1, :], ALU.mult, [lB], [lB])
            P.op("dve", lambda e: e.reduce_sum(out=lams[:, 0:2], in_=lamp[:].rearrange("p (m d) -> p m d", m=2), axis=AX.X),
                 reads=[lB], writes=[lB])
            self.act(lams[:, 0:2], lams[:, 0:2], AF.Exp, [lB], [lB])
            self.tt("dve", lams[:, 2:3], lams[:, 1:2], lams[:, 0:1], ALU.subtract, [lB], [lB])
            self.ts("dve", lams[:, 3:4], lams[:, 2:3], -lam_init, None, ALU.add, None, [lB], [lB])
            self.ts("dve", lams[:, 5:6], lams[:, 4:5], 1.0 - lam_init, None, ALU.mult, None, [lB], [lB])
            neglam, gsc = lams[:, 3:4], lams[:, 5:6]
            KT = self.sb(ph, "KT", [128, NCTX + SEQ], BF16)
            KTBs = [Buf() for _ in range(1 + NCORES)]
            V = self.sb(ph, "Vh", [128, 66, 128], BF16)
            VBs = [Buf() for _ in range(1 + NCORES)]
            QhT = self.sb(ph, "QhT", [128, TL + NCTX], BF16)
            QB = Buf()
            yaT = self.sb(ph, "yaT", [128, 4, TL + NCTX], BF16)
            yaB = Buf()
            NE = 6
            E = [self.sb(ph, "E%d" % i, [128, 512], BF16) for i in range(NE)]
            EB = [Buf() for _ in range(NE)]
            accD = self.sb(ph, "accD", [128, 2, 512])
            accDB = [Buf() for _ in range(2)]
            r1 = self.sb(ph, "fr1", [128, 512])
            r2 = self.sb(ph, "fr2", [128, 512])
            o = self.sb(ph, "fo", [128, 512])
            sq = self.sb(ph, "fsq", [128, 512])
            fB = Buf()
            for b in range(2):
                for h in range(4):
                    for r in range(NCORES):
                        P.dma("sp", KT[:, NCTX + r * TL:NCTX + (r + 1) * TL], self.sec(gath, r, b, SEC_KA + h * 128, 128),
                              reads=[gathB], writes=[KTBs[1 + r]])
                        vsec = self.sec(gath, r, b, SEC_VA, 512).rearrange("r c -> (r c)").rearrange("(h t e) -> h t e", h=4, t=TL)
                        P.dma("sp", V[:, 2 + r * 8:2 + (r + 1) * 8, :], vsec[h].rearrange("(p j) e -> p j e", j=8),
                              reads=[gathB], writes=[VBs[1 + r]])
                    P.dma("sp", KT[:, 0:NCTX], self.ckaT[b][h * 128:(h + 1) * 128, :], reads=[self.ckvB], writes=[KTBs[0]])
                    P.dma("sp", V[:, 0:2, :], self.cva[b][:, h * 128:(h + 1) * 128].rearrange("(t p) e -> p t e", p=128),
                          reads=[self.ckvB], writes=[VBs[0]])
                    for half in range(2):
                        bk, bB = self.bank[half], self.bankB[half]
                        self.proj_fm(bk, bB, W, WB, h * 128, b * TL + half * 512, 512)
                        self.rope_evac(rs, bk, bB, self.bank[2 + half], self.bankB[2 + half], QhT[:, half * 512:(half + 1) * 512], QB,
                                       half * 512, 512)
                    if not last:
                        self.proj_fm(self.bank[0], self.bankB[0], W, WB, h * 128, NLAT + b * NCTX, NCTX)
                        self.cp("act", QhT[:, TL:TL + NCTX], self.bank[0][:, 0:NCTX], [self.bankB[0]], [QB])
                    lat_tiles = [(t, slice(t * 128, (t + 1) * 128)) for t in range(2)] + [
                        (2 + r * 8 + j, slice(NCTX + r * TL + j, NCTX + (r + 1) * TL, 8)) for r in range(NCORES) for j in range(8)]
                    qblocks = [(0, 512, lat_tiles), (512, 512, lat_tiles)] + ([] if last else [(TL, NCTX, lat_tiles[0:2])])
                    for (q0, n, tiles) in qblocks:
                        O1, O2, D1, D2 = self.bank[4], self.bank[5], self.bank[6], self.bank[7]
                        O1B, O2B, D1B, D2B = self.bankB[4], self.bankB[5], self.bankB[6], self.bankB[7]
                        nt = len(tiles)
                        def emit_s(ti):
                            vt, ksl = tiles[ti]
                            for m in range(2):
                                si = (2 * ti + m) % 4
                                ei = (2 * ti + m) % NE
                                S, SB = self.bank[si], self.bankB[si]
                                self.mm(S[:, 0:n], KT[m * 64:(m + 1) * 64, ksl], QhT[m * 64:(m + 1) * 64, q0:q0 + n], True, True,
                                        [KTBs[0 if vt < 2 else 1 + (vt - 2) // 8], QB], [SB])
                                self.act(E[ei][:, 0:n], S[:, 0:n], AF.Exp, [SB], [EB[ei]], scale=0.125)

                        def emit_pv(ti):
                            vt, ksl = tiles[ti]
                            for m in range(2):
                                ei = (2 * ti + m) % NE
                                Om, OmB = (O1, O1B) if m == 0 else (O2, O2B)
                                self.mm(Om[:, 0:n], V[:, vt, :], E[ei][:, 0:n], ti == 0, ti == nt - 1,
                                        [VBs[0 if vt < 2 else 1 + (vt - 2) // 8], EB[ei]], [OmB])
                                if m == 0:
                                    self.mm(D1[:, 0:n], self.ones_b[:], E[ei][:, 0:n], ti == 0, ti == nt - 1, [self.cB, EB[ei]], [D1B])
                                else:
                                    a = ti % 2
                                    eng = "dve" if a == 0 else "pool"
                                    if ti < 2:
                                        self.cp(eng, accD[:, a, 0:n], E[ei][:, 0:n], [EB[ei]], [accDB[a]])
                                    else:
                                        self.tt(eng, accD[:, a, 0:n], accD[:, a, 0:n], E[ei][:, 0:n], ALU.add, [EB[ei], accDB[a]], [accDB[a]])

                        emit_s(0)
                        for ti in range(nt):
                            if ti + 1 < nt:
                                emit_s(ti + 1)
                            emit_pv(ti)
                        self.mm(D2[:, 0:n], self.ones_f[:], accD[:, 0, 0:n], True, False, [self.cB, accDB[0]], [D2B], signal=False)
                        self.mm(D2[:, 0:n], self.ones_f[:], accD[:, 1, 0:n], False, True, [self.cB, accDB[1]], [D2B])
                        self.recip(r1[:, 0:n], D1[:, 0:n], [D1B], [fB])
                        self.recip(r2[:, 0:n], D2[:, 0:n], [D2B], [fB])
                        self.tt("dve", o[:, 0:n], O1[:, 0:n], r1[:, 0:n], ALU.mult, [O1B, fB], [fB])
                        self.tt("dve", r2[:, 0:n], O2[:, 0:n], r2[:, 0:n], ALU.mult, [O2B, fB], [fB])
                        self.stt("dve", o[:, 0:n], r2[:, 0:n], neglam, o[:, 0:n], ALU.mult, ALU.add, [fB, lB], [fB])
                        self.act(sq[:, 0:n], o[:, 0:n], AF.Square, [fB], [fB])
                        self.mm(D1[:, 0:n], self.ones_f[:], sq[:, 0:n], True, True, [self.cB, fB], [D1B])
                        self.ts("dve", r1[:, 0:n], D1[:, 0:n], 1.0 / 128, EPS, ALU.mult, ALU.add, [D1B, fB], [fB])
                        self.act(r1[:, 0:n], r1[:, 0:n], AF.Sqrt, [fB], [fB])
                        self.recip(r1[:, 0:n], r1[:, 0:n], [fB], [fB])
                        self.stt("dve", yaT[:, h, q0:q0 + n], o[:, 0:n], gsc, r1[:, 0:n], ALU.mult, ALU.mult, [fB, lB], [yaB])
                self.merge(ms, lambda j, s0, n: yaT[:, j, s0:s0 + n], [yaB], b * TL, TL, False, banks=(0, 1), banks2=(2, 3))
                if not last:
                    self.merge(ms, lambda j, s0, n: yaT[:, j, TL + s0:TL + s0 + n], [yaB], NLAT + b * NCTX, NCTX, False, banks=(0, 1), banks2=(2, 3))
            P.barrier()

    def mixer_c(self, l, last, gath, gathB):
        P, d = self.P, self.d
        with ExitStack() as ph:
            W = self.sb(ph, "wqc", [128, 8, 512], BF16)
            WB = Buf()
            self.load_w(W, WB, d["w_in_%d" % l][:, OFF_QC:OFF_QC + 512])
            ms = self.merge_setup(ph, l, 2, 64)
            selm = self.sb(ph, "selm", [128, 16, 128], BF16)
            selB = Buf()
            for j in range(16):
                self.ts("dve", selm[:, j, :], self.ident_b[:], self.selw[:, j:j + 1], None, ALU.mult, None, [self.cB], [selB])
            KCx = self.sb(ph, "KCx", [128, 4, 24 * 64], BF16)
            KCB = Buf()
            VCx = self.sb(ph, "VCx", [128, 12, 512], BF16)
            VCB = Buf()
            KCc = self.sb(ph, "KCc", [128, 4, NCTX], BF16)
            VCc = self.sb(ph, "VCc", [128, 2, 512], BF16)
            ccB = Buf()
            QcT = self.sb(ph, "QcT", [128, 4, TL + NCTX], BF16)
            QB = Buf()
            ycT = [self.sb(ph, "ycT%d" % i, [64, 8, 512], BF16) for i in range(1)]
            ycB = [Buf() for _ in range(1)]
            E = [self.sb(ph, "Ec%d" % i, [128, 512], BF16) for i in range(4)]
            EB = [Buf() for _ in range(4)]
            NBT = 4
            bt = [self.sb(ph, "nabt%d" % i, [128, 512], BF16) for i in range(NBT)]
            btB = [Buf() for _ in range(NBT)]
            nqb = 2
            bias_uses = [(g_, hd_, i_) for b_ in range(2) for g_ in range(nqb) for hd_ in range(8) for i_ in range(8)]
            bias_state = {"k": 0}

            def bias_prefetch(k):
                if k < len(bias_uses):
                    g_, hd_, i_ = bias_uses[k]
                    P.dma("sp", bt[k % NBT][:], self.nab16[l][g_, hd_, i_], reads=[self.nabB[l]], writes=[btB[k % NBT]])

            for k in range(NBT):
                bias_prefetch(k)
            rr = [self.sb(ph, "ncr%d" % i, [64, 512]) for i in range(2)]
            rrB = [Buf() for _ in range(2)]
            cand = [self.sb(ph, "cand%d" % i, [128, 8, 512], BF16) for i in range(2)]
            candB = [Buf() for _ in range(2)]
            yi = 0
            for b in range(2):
                P.dma("sp", KCx[:, :, 256:256 + TL], self.kcT[b].rearrange("(c p) t -> p c t", p=128), reads=[self.kvownB], writes=[KCB])
                P.dma("sp", VCx[:, 2:10, :], self.vc[b].rearrange("(t p) e -> p t e", p=128), reads=[self.kvownB], writes=[VCB])
                P.dma("sp", KCc[:], self.ckcT[b].rearrange("(c p) t -> p c t", p=128), reads=[self.ckvB], writes=[ccB])
                P.dma("sp", VCc[:], self.cvc[b].rearrange("(t p) e -> p t e", p=128), reads=[self.ckvB], writes=[ccB])
                ci = 0
                for c in range(4):
                    cd, cdB = cand[ci % 2], candB[ci % 2]
                    ci += 1
                    for r in range(NCORES):
                        hsec = self.sec(gath, r, b, SEC_KCH, 256).rearrange("a (two t) -> (a two) t", two=2)
                        P.dma("sp", cd[:, r, :], hsec[c * 128:(c + 1) * 128, :], reads=[gathB], writes=[cdB])
                    for side in range(2):
                        bk, bB = self.bank[side], self.bankB[side]
                        for r in range(NCORES):
                            src = cd[:, r, 256:512] if side == 0 else cd[:, r, 0:256]
                            self.mm(bk[:, 0:256], selm[:, side * 8 + r, :], src, r == 0, r == NCORES - 1, [selB, cdB], [bB])
                        dst = KCx[:, c, 0:256] if side == 0 else KCx[:, c, 256 + TL:512 + TL]
                        self.cp("act", dst, bk[:, 0:256], [bB], [KCB])
                for side in range(2):
                    for a in range(2):
                        cd, cdB = cand[ci % 2], candB[ci % 2]
                        ci += 1
                        for r in range(NCORES):
                            vsec = self.sec(gath, r, b, SEC_VCH, 256).rearrange("a (two e) -> (a two) e", two=2)
                            t0 = (256 + a * 128) if side == 0 else a * 128
                            P.dma("sp", cd[:, r, :], vsec[t0:t0 + 128, :], reads=[gathB], writes=[cdB])
                        bk, bB = self.bank[2 + a], self.bankB[2 + a]
                        for r in range(NCORES):
                            self.mm(bk[:, :], selm[:, side * 8 + r, :], cd[:, r, :], r == 0, r == NCORES - 1, [selB, cdB], [bB])
                        self.cp("dve", VCx[:, (0 if side == 0 else 10) + a, :], bk[:, :], [bB], [VCB])
                for c in range(4):
                    for half in range(2):
                        bk, bB = self.bank[(2 * c + half) % 4], self.bankB[(2 * c + half) % 4]
                        self.proj_fm(bk, bB, W, WB, c * 128, b * TL + half * 512, 512)
                        self.act(QcT[:, c, half * 512:(half + 1) * 512], bk[:, :], AF.Copy, [bB], [QB], scale=0.125)
                    if not last:
                        bk, bB = self.bank[c % 4], self.bankB[c % 4]
                        self.proj_fm(bk, bB, W, WB, c * 128, NLAT + b * NCTX, NCTX)
                        self.act(QcT[:, c, TL:TL + NCTX], bk[:, 0:NCTX], AF.Copy, [bB], [QB], scale=0.125)
                ctx_tiles = [("c", t) for t in range(2)]
                qblocks = [(0, 512, [("w", 0 + i) for i in range(8)] + ctx_tiles, 0),
                           (512, 512, [("w", 4 + i) for i in range(8)] + ctx_tiles, 1)]
                if not last:
                    qblocks.append((TL, NCTX, ctx_tiles, None))
                si = 0
                for (q0, n, tiles, g) in qblocks:
                    yc, yB = ycT[0], ycB[0]
                    yi += 1
                    for hd in range(8):
                        c, po = hd // 2, (hd % 2) * 64
                        O, OB = self.bank[4 + 2 * (hd % 2)], self.bankB[4 + 2 * (hd % 2)]
                        Dn, DB = self.bank[5 + 2 * (hd % 2)], self.bankB[5 + 2 * (hd % 2)]
                        nt = len(tiles)
                        def emit_s(ti, sidx):
                            kind, j = tiles[ti]
                            S, SB = self.bank[sidx % 4], self.bankB[sidx % 4]
                            e, eB = E[sidx % 4], EB[sidx % 4]
                            if kind == "w":
                                i = j - 4 * g
                                k = bias_state["k"]
                                assert bias_uses[k] == (g, hd, i)
                                btile, btB_ = bt[k % NBT], btB[k % NBT]
                                self.mm(S[:, 0:n], KCx[po:po + 64, c, j * 128:(j + 1) * 128], QcT[po:po + 64, c, q0:q0 + n], True, False,
                                        [KCB, QB], [SB], signal=False)
                                self.mm(S[:, 0:n], self.ident_b[:], btile[:, 0:n], False, True, [self.cB, btB_], [SB])
                                bias_prefetch(k + NBT)
                                bias_state["k"] = k + 1
                            else:
                                self.mm(S[:, 0:n], KCc[po:po + 64, c, j * 128:(j + 1) * 128], QcT[po:po + 64, c, q0:q0 + n], True, True,
                                        [ccB, QB], [SB])
                            self.act(e[:, 0:n], S[:, 0:n], AF.Exp, [SB], [eB])

                        def emit_pv(ti, sidx):
                            kind, j = tiles[ti]
                            e, eB = E[sidx % 4], EB[sidx % 4]
                            if kind == "w":
                                vap, vrd = VCx[:, j, hd * 64:(hd + 1) * 64], VCB
                            else:
                                vap, vrd = VCc[:, j, hd * 64:(hd + 1) * 64], ccB
                            self.mm(O[0:64, 0:n], vap, e[:, 0:n], ti == 0, ti == nt - 1, [vrd, eB], [OB])
                            self.mm(Dn[0:64, 0:n], self.ones_b[:, 0:64], e[:, 0:n], ti == 0, ti == nt - 1, [self.cB, eB], [DB])

                        emit_s(0, si)
                        for ti in range(nt):
                            if ti + 1 < nt:
                                emit_s(ti + 1, si + ti + 1)
                            emit_pv(ti, si + ti)
                        si += nt
                        r_, rB_ = rr[hd % 2], rrB[hd % 2]
                        self.recip(r_[:, 0:n], Dn[0:64, 0:n], [DB], [rB_])
                        self.tt("dve", yc[:, hd, 0:n], O[0:64, 0:n], r_[:, 0:n], ALU.mult, [OB, rB_], [yB])
                    tok0 = b * TL + q0 if g is not None else NLAT + b * NCTX
                    self.merge(ms, lambda j, s0, n_, yc=yc: yc[0:64, j, s0:s0 + n_], [yB], tok0, n, False, banks=(0, 1), banks2=(2, 3))
            P.barrier()

    def prep_nabias(self, l):
        if not hasattr(self, "nab16"):
            self.nab16, self.nabB = {}, {}
        self.nab16[l] = self.dscr("nab16_%d" % l, [2, 8, 8, 128, 512], BF16)
        self.nabB[l] = Buf("nab16")
        for g in range(2):
            for hd in range(8):
                self.P.dma("pool", self.nab16[l][g, hd], self.d["nabias_%d" % l][g, hd], writes=[self.nabB[l]])

    def gate_bcast(self, ph, l, c0, v, dst, dstB):
        self.bcast_rows(ph, lambda c: self.modT[l][:, c0 + c, v:v + 1], 8, dst, dstB, [self.modTB[l]])

    def out_proj(self, l, last):
        P, d = self.P, self.d
        ntile = (NLAT if last else NTOK) // 128
        with ExitStack() as ph:
            W = self.sb(ph, "wout", [128, 8, D], BF16)
            WB = Buf()
            self.load_w(W, WB, d["w_out_%d" % l][:, :], nsplit=2)
            gbc = self.sb(ph, "gbc", [128, D])
            gB = Buf()
            ht = [self.sb(ph, "oht%d" % i, [128, D]) for i in range(2)]
            htB = [Buf() for _ in range(2)]
            hn = [self.sb(ph, "ohn%d" % i, [128, D]) for i in range(2)]
            hnB = [Buf() for _ in range(2)]
            curv = -1
            for t in range(ntile):
                v = self.variant(t)
                if v != curv:
                    self.gate_bcast(ph, l, 16, v, gbc, gB)
                    curv = v
                i = t % 2
                P.dma("sp", ht[i][:], self.hbuf[t * 128:(t + 1) * 128, :], reads=[self.hB[t]], writes=[htB[i]])
                for half in range(2):
                    bk, bB = self.bank[2 + 2 * i + half], self.bankB[2 + 2 * i + half]
                    for fc in range(8):
                        self.mm(bk[:, :], self.mT[:, fc, t * 128:(t + 1) * 128], W[:, fc, half * 512:(half + 1) * 512], fc == 0, fc == 7,
                                [self.mTB[t], WB], [bB])
                    self.tt("dve", hn[i][:, half * 512:(half + 1) * 512], bk[:, :], gbc[:, half * 512:(half + 1) * 512], ALU.mult,
                            [bB, gB], [hnB[i]])
                self.tt("pool", hn[i][:], hn[i][:], ht[i][:], ALU.add, [hnB[i], htB[i]], [hnB[i]])
                P.dma("sp", self.hbuf[t * 128:(t + 1) * 128, :], hn[i][:], reads=[hnB[i]], writes=[self.hB[t]])
            P.barrier()

    def phase_mix(self, l, last, gath, gathB):
        with ExitStack() as ph:
            self.mT = self.sb(ph, "mT", [128, 8, NTOK], BF16)
            self.mTB = [Buf("mT%d" % i) for i in range(NTOK // 128)]
            self.mixer_b(l, last)
            self.mixer_a(l, last, gath, gathB)
            self.mixer_c(l, last, gath, gathB)
            self.out_proj(l, last)

    def phase_moe_dense(self, l, last):
        P, d = self.P, self.d
        ntok = NLAT if last else NTOK
        ntile = ntok // 128
        with ExitStack() as ph:
            wts = self.sb(ph, "wts", [128, NTOK // 128, NEXP])
            wtsB = Buf()
            rstate = {}

            def r_begin(ph2):
                rstate["wrT"] = self.sb(ph2, "wrT", [128, 8, 36])
                rstate["br"] = self.sb(ph2, "brbc", [128, 36])
                rstate["B"] = Buf()
                P.dma("sp", rstate["wrT"][:], d["wr_%d" % l].rearrange("(k p) n -> p k n", p=128), writes=[rstate["B"]])
                P.dma("sp", rstate["br"][:], d["br_%d" % l].partition_broadcast(128), writes=[rstate["B"]])
                rstate["lg"] = self.sb(ph2, "rlg", [128, 36])
                rstate["em"] = self.sb(ph2, "rem", [128, 32])
                rstate["oh"] = self.sb(ph2, "roh", [128, 32])
                rstate["sm"] = self.sb(ph2, "rsm", [128, 16])
                rstate["ge"] = self.sb(ph2, "rge", [128, 4])
                rstate["tB"] = Buf()

            def r_tile(t, half, tmp, tmpB):
                L, LB = self.bank[2], self.bankB[2]
                for c in range(4):
                    self.mm(L[:, 0:36], tmp[:, c, :], rstate["wrT"][:, half * 4 + c, :], half == 0 and c == 0, half == 1 and c == 3,
                            [tmpB, rstate["B"]], [LB], signal=(c == 3))
                if half == 0:
                    return
                lg, em, oh, sm, ge, tB = rstate["lg"], rstate["em"], rstate["oh"], rstate["sm"], rstate["ge"], rstate["tB"]
                rB = [tB]
                self.tt("dve", lg[:], L[:, 0:36], rstate["br"][:], ALU.add, [LB, rstate["B"]], rB)
                P.op("dve", lambda e: e.reduce_max(out=sm[:, 0:1], in_=lg[:, 0:4], axis=AX.X), reads=rB, writes=rB)
                self.ts("dve", oh[:, 0:4], lg[:, 0:4], sm[:, 0:1], None, ALU.is_equal, None, rB, rB)
                self.ts("dve", sm[:, 1:2], sm[:, 0:1], -1.0, None, ALU.mult, None, rB, rB)
                self.memset("dve", sm[:, 2:3], 0.0, rB)
                self.act(ge[:], lg[:, 0:4], AF.Exp, rB, rB, bias=sm[:, 1:2], accum_out=sm[:, 2:3])
                self.recip(sm[:, 3:4], sm[:, 2:3], rB, rB)
                self.ts("dve", oh[:, 4:8], oh[:, 0:4], 1e30, -1e30, ALU.mult, ALU.add, rB, rB)
                self.tt("dve", em[:].rearrange("p (g e) -> p g e", g=4), lg[:, 4:36].rearrange("p (g e) -> p g e", g=4),
                        oh[:, 4:8].unsqueeze(2).to_broadcast([128, 4, 8]), ALU.add, rB, rB)
                P.op("dve", lambda e: e.reduce_max(out=sm[:, 4:5], in_=em[:], axis=AX.X), reads=rB, writes=rB)
                self.ts("dve", oh[:], em[:], sm[:, 4:5], None, ALU.is_equal, None, rB, rB)
                self.stt("dve", em[:], oh[:], -1e30, em[:], ALU.mult, ALU.add, rB, rB)
                P.op("dve", lambda e: e.reduce_max(out=sm[:, 5:6], in_=em[:], axis=AX.X), reads=rB, writes=rB)
                self.tt("dve", sm[:, 6:7], sm[:, 4:5], sm[:, 5:6], ALU.subtract, rB, rB)
                self.act(sm[:, 6:7], sm[:, 6:7], AF.Sigmoid, rB, rB)
                self.tt("dve", sm[:, 7:8], sm[:, 6:7], sm[:, 3:4], ALU.mult, rB, rB)
                self.tt("dve", sm[:, 8:9], sm[:, 3:4], sm[:, 7:8], ALU.subtract, rB, rB)
                self.ts("dve", wts[:, t, :], oh[:], sm[:, 7:8], None, ALU.mult, None, rB, [wtsB])
                self.ts("dve", oh[:], em[:], sm[:, 5:6], None, ALU.is_equal, None, rB, rB)
                self.stt("dve", wts[:, t, :], oh[:], sm[:, 8:9], wts[:, t, :], ALU.mult, ALU.add, rB + [wtsB], [wtsB])

            self.phase_norm(l, 2, ntok, route={"begin": r_begin, "tile": r_tile})

            acc = self.sb(ph, "acc", [128, ntile, D])
            accB = [Buf() for _ in range(ntile)]
            for t in range(ntile):
                self.memset("pool", acc[:, t, :], 0.0, [accB[t]])
            wg = [self.sb(ph, "wg%d" % i, [128, 8, DEXP], BF16) for i in range(2)]
            wu = [self.sb(ph, "wu%d" % i, [128, 8, DEXP], BF16) for i in range(2)]
            wd = [self.sb(ph, "wd%d" % i, [128, 4, D], BF16) for i in range(2)]
            wB = [Buf() for _ in range(2)]
            sgb = [self.sb(ph, "msg%d" % i, [128, 512], BF16) for i in range(2)]
            sgB = [Buf() for _ in range(2)]
            hdT = self.sb(ph, "hdT", [128, 4, 512], BF16)
            hdB = [Buf() for _ in range(4)]

            def load_expert(e):
                i = e % 2
                self.load_w(wg[i], wB[i], d["wg_%d" % l][e])
                self.load_w(wu[i], wB[i], d["wu_%d" % l][e])
                self.load_w(wd[i], wB[i], d["wd_%d" % l][e])

            load_expert(0)
            bi = 0
            for e in range(NEXP):
                if e + 1 < NEXP:
                    load_expert(e + 1)
                i = e % 2
                for tb in range(ntok // 512):
                    tok0 = tb * 512
                    for fcb in range(4):
                        G, GB = self.bank[2 * (fcb % 2)], self.bankB[2 * (fcb % 2)]
                        U, UB = self.bank[2 * (fcb % 2) + 1], self.bankB[2 * (fcb % 2) + 1]
                        self.proj_fm(G, GB, wg[i], wB[i], fcb * 128, tok0, 512)
                        self.proj_fm(U, UB, wu[i], wB[i], fcb * 128, tok0, 512)
                        sg_, sgB_ = sgb[fcb % 2], sgB[fcb % 2]
                        self.act(sg_[:], G[:, :], AF.Silu, [GB], [sgB_])
                        self.tt("dve", hdT[:, fcb, :], U[:, :], sg_[:], ALU.mult, [UB, sgB_], [hdB[fcb]])
                    for tt_ in range(4):
                        t = tb * 4 + tt_
                        for half in range(2):
                            Y, YB = self.bank[4 + bi % 4], self.bankB[4 + bi % 4]
                            bi += 1
                            for fcb in range(4):
                                self.mm(Y[:, :], hdT[:, fcb, tt_ * 128:(tt_ + 1) * 128], wd[i][:, fcb, half * 512:(half + 1) * 512],
                                        fcb == 0, fcb == 3, [hdB[fcb], wB[i]], [YB])
                            self.stt("dve", acc[:, t, half * 512:(half + 1) * 512], Y[:, :], wts[:, t, e:e + 1],
                                     acc[:, t, half * 512:(half + 1) * 512], ALU.mult, ALU.add, [YB, wtsB, accB[t]], [accB[t]])
            gbc = self.sb(ph, "gbc2", [128, D])
            gB = Buf()
            ht = [self.sb(ph, "mht%d" % i, [128, D]) for i in range(2)]
            htB = [Buf() for _ in range(2)]
            curv = -1
            for t in range(ntile):
                v = self.variant(t)
                if v != curv:
                    self.gate_bcast(ph, l, 40, v, gbc, gB)
                    curv = v
                i = t % 2
                P.dma("sp", ht[i][:], self.hbuf[t * 128:(t + 1) * 128, :], reads=[self.hB[t]], writes=[htB[i]])
                self.tt("pool", acc[:, t, :], acc[:, t, :], gbc[:], ALU.mult, [accB[t], gB], [accB[t]])
                self.tt("dve", acc[:, t, :], acc[:, t, :], ht[i][:], ALU.add, [accB[t], htB[i]], [accB[t]])
                P.dma("sp", self.hbuf[t * 128:(t + 1) * 128, :], acc[:, t, :], reads=[accB[t], htB[i]], writes=[self.hB[t]])
            P.barrier()

    def phase_moe(self, l, last):
        P, d = self.P, self.d
        ntok = NLAT if last else NTOK
        ntile = ntok // 128
        C = MOE_CAP
        NSLOT = NEXP * C
        I32 = mybir.dt.int32
        xs = self.dscr("xs%d" % l, [NSLOT, D], BF16)
        ys = self.dscr("ys%d" % l, [NSLOT, D], F32)
        scaleT = self.s2T[l]
        mB = self.modTB[l]
        with ExitStack() as ph:
            didx = self.sb(ph, "didx", [128, NTOK // 128, 2], I32)
            wts2 = self.sb(ph, "wts2", [128, NTOK // 128, 2])
            rtB = Buf()
            with ExitStack() as p1:
                tri = self.sb(p1, "tri", [128, 128], BF16)
                ecb = self.sb(p1, "ecb", [128, NEXP])
                wrT = self.sb(p1, "wrT", [128, 8, 36])
                brb = self.sb(p1, "brbc", [128, 36])
                kB = Buf()
                P.dma("pool", tri[:], d["tri"], writes=[kB])
                P.dma("sp", ecb[:], d["ecb"], writes=[kB])
                P.dma("sp", wrT[:], d["wr_%d" % l].rearrange("(k p) n -> p k n", p=128), writes=[kB])
                P.dma("sp", brb[:], d["br_%d" % l].partition_broadcast(128), writes=[kB])
                base = self.sb(p1, "rbase", [128, NEXP])
                baseB = Buf()
                self.memset("dve", base[:], 0.0, [baseB])
                sbc = self.sb(p1, "sbc", [128, D])
                shbc = self.sb(p1, "shbc", [128, D])
                bcB = Buf()
                ht = [self.sb(p1, "ht%d" % i, [128, D]) for i in range(2)]
                htB = [Buf() for _ in range(2)]
                xf = [self.sb(p1, "xf%d" % i, [128, D]) for i in range(2)]
                xfB = [Buf() for _ in range(2)]
                xtm = [self.sb(p1, "xtm%d" % i, [128, D], BF16) for i in range(3)]
                xtmB = [Buf() for _ in range(3)]
                xT = [self.sb(p1, "xT%d" % i, [128, 4, 128]) for i in range(2)]
                xTB = [Buf() for _ in range(2)]
                junk = self.sb(p1, "junk", [128, D])
                junkB = Buf()
                ss = self.sb(p1, "ss", [128, 32])
                ssB = Buf()
                lg = self.sb(p1, "rlg", [128, 36])
                em = self.sb(p1, "rem", [128, 32])
                oh1 = self.sb(p1, "roh1", [128, 32])
                oh2 = self.sb(p1, "roh2", [128, 32])
                ohg = self.sb(p1, "rohg", [128, 8])
                Ab = self.sb(p1, "rAb", [128, 32], BF16)
                slot = self.sb(p1, "rslot", [128, 32])
                tmp32 = self.sb(p1, "rtmp32", [128, 32])
                sm = self.sb(p1, "rsm", [128, 16])
                ge = self.sb(p1, "rge", [128, 4])
                tB = Buf()
                rB = [tB]
                NT = ntile
                xall = self.sb(p1, "xall", [128, NT, D], BF16)
                xallB = [Buf() for _ in range(NT)]
                lgall = self.sb(p1, "lgall", [128, NT, 36])
                emA = self.sb(p1, "emA", [128, NT, 32])
                o1A = self.sb(p1, "o1A", [128, NT, 32])
                o2A = self.sb(p1, "o2A", [128, NT, 32])
                slA = self.sb(p1, "slA", [128, NT, 32])
                bsA = self.sb(p1, "bsA", [128, NT, 32])
                AbA = self.sb(p1, "AbA", [128, NT, 32], BF16)
                g4 = self.sb(p1, "g4", [128, NT, 4])
                p4 = self.sb(p1, "p4", [128, NT, 4])
                sv = self.sb(p1, "sv", [128, 12, NT])
                self.memset("dve", ss[:], 0.0, [ssB])
                for t in range(ntile):
                    i = t % 2
                    P.dma("sp", ht[i][:], self.hbuf[t * 128:(t + 1) * 128, :], reads=[self.hB[t]], writes=[htB[i]])
                    self.act(junk[:], ht[i][:], AF.Square, [htB[i], ssB], [junkB, ssB], accum_out=ss[:, t:t + 1])
                self.rsqrt_inplace(ss[:, 0:ntile], 1.0 / D, EPS, ssB)
                curv = -1
                for t in range(ntile):
                    i = t % 2
                    v = self.variant(t)
                    if v != curv:
                        self.bcast_rows(p1, lambda c, v=v: scaleT[:, c, v:v + 1], 8, sbc, bcB, [mB])
                        self.bcast_rows(p1, lambda c, v=v: self.modT[l][:, 24 + c, v:v + 1], 8, shbc, bcB, [mB])
                        curv = v
                    P.dma("sp", ht[i][:], self.hbuf[t * 128:(t + 1) * 128, :], reads=[self.hB[t]], writes=[htB[i]])
                    self.act(xf[i][:], ht[i][:], AF.Copy, [htB[i], ssB], [xfB[i]], scale=ss[:, t:t + 1])
                    self.tt("dve", xf[i][:], xf[i][:], sbc[:], ALU.mult, [xfB[i], bcB], [xfB[i]])
                    self.tt("pool", xf[i][:], xf[i][:], shbc[:], ALU.add, [xfB[i], bcB], [xfB[i]])
                    self.cp("pool", xall[:, t, :], xf[i][:], [xfB[i]], [xallB[t]])
                    L, LB = self.bank[2 + t % 2], self.bankB[2 + t % 2]
                    for half in range(2):
                        bk, bB = self.bank[half], self.bankB[half]
                        for c in range(4):
                            cc = half * 4 + c
                            self.tr(bk[:, c * 128:(c + 1) * 128], xf[i][:, cc * 128:(cc + 1) * 128], self.ident[:], [xfB[i], self.cB], [bB],
                                    signal=(c == 3))
                        self.cp("act", xT[half][:], bk[:].rearrange("p (c t) -> p c t", c=4), [bB], [xTB[half]])
                        for c in range(4):
                            self.mm(L[:, 0:36], xT[half][:, c, :], wrT[:, half * 4 + c, :], half == 0 and c == 0, half == 1 and c == 3,
                                    [xTB[half], kB], [LB], signal=(c == 3))
                    self.tt("dve", lgall[:, t, :], L[:, 0:36], brb[:], ALU.add, [LB, kB], rB)
                G = lgall[:, :, 0:4]

                def bc(ap2, n):
                    return ap2.unsqueeze(2).to_broadcast([128, NT, n])

                def rmax(out, in_):
                    P.op("dve", lambda e: e.reduce_max(out=out, in_=in_, axis=AX.X), reads=rB, writes=rB)

                def rsum(out, in_):
                    P.op("dve", lambda e: e.reduce_sum(out=out, in_=in_, axis=AX.X), reads=rB, writes=rB)

                gmax, gsum, gw, m1, m2, dl, w1, d1, d2 = (sv[:, j, :] for j in range(9))
                rmax(gmax, G)
                self.tt("dve", p4[:], G, bc(gmax, 4), ALU.is_equal, rB, rB)
                self.tt("dve", g4[:], G, bc(gmax, 4), ALU.subtract, rB, rB)
                self.act(g4[:], g4[:], AF.Exp, rB, rB)
                rsum(gsum, g4[:])
                self.recip(gw, gsum, rB, rB)
                self.ts("dve", p4[:], p4[:], 1e30, -1e30, ALU.mult, ALU.add, rB, rB)
                self.cp("dve", emA[:], lgall[:, :, 4:36], rB, rB)
                self.tt("dve", emA[:].rearrange("p t (g e) -> p (t g) e", g=4), emA[:].rearrange("p t (g e) -> p (t g) e", g=4),
                        p4[:].rearrange("p t g -> p (t g)").unsqueeze(2).to_broadcast([128, NT * 4, 8]), ALU.add, rB, rB)
                rmax(m1, emA[:])
                self.tt("dve", o1A[:], emA[:], bc(m1, 32), ALU.is_equal, rB, rB)
                self.stt("dve", emA[:], o1A[:], -1e30, emA[:], ALU.mult, ALU.add, rB, rB)
                rmax(m2, emA[:])
                self.tt("dve", o2A[:], emA[:], bc(m2, 32), ALU.is_equal, rB, rB)
                self.tt("dve", dl, m1, m2, ALU.subtract, rB, rB)
                self.act(dl, dl, AF.Sigmoid, rB, rB)
                self.tt("dve", wts2[:, 0:NT, 0], dl, gw, ALU.mult, rB, [rtB])
                self.tt("dve", wts2[:, 0:NT, 1], gw, wts2[:, 0:NT, 0], ALU.subtract, rB + [rtB], [rtB])
                self.tt("dve", AbA[:], o1A[:], o2A[:], ALU.add, rB, rB)
                RK = [(self.bank[4], self.bankB[4]), (self.bank[5], self.bankB[5])]
                TO = [(self.bank[6], self.bankB[6]), (self.bank[7], self.bankB[7])]
                for t in range(NT):
                    (R, RB_), (T_, TB_) = RK[t // 16], TO[t // 16]
                    c0 = (t % 16) * 32
                    self.mm(R[:, c0:c0 + 32], tri[:], AbA[:, t, :], True, True, [kB, tB], [RB_], signal=(t % 16 == 15 or t == NT - 1))
                    self.mm(T_[:, c0:c0 + 32], self.ones_b[:], AbA[:, t, :], True, True, [self.cB, tB], [TB_],
                            signal=(t % 16 == 15 or t == NT - 1))
                for j in range((NT + 15) // 16):
                    n_ = min(16, NT - 16 * j)
                    self.cp("dve", slA[:, 16 * j:16 * j + n_, :], RK[j][0][:, 0:n_ * 32].rearrange("p (t e) -> p t e", e=32), [RK[j][1]], rB)
                    self.cp("dve", emA[:, 16 * j:16 * j + n_, :], TO[j][0][:, 0:n_ * 32].rearrange("p (t e) -> p t e", e=32), [TO[j][1]], rB)
                self.memset("dve", bsA[:, 0, :], 0.0, rB)
                for t in range(1, NT):
                    self.tt("dve", bsA[:, t, :], bsA[:, t - 1, :], emA[:, t - 1, :], ALU.add, rB, rB)
                self.tt("dve", slA[:], slA[:], bsA[:], ALU.add, rB, rB)
                self.ts("dve", emA[:], slA[:], float(C), 1e7, ALU.is_ge, ALU.mult, rB, rB)
                self.tt("dve", slA[:], slA[:], emA[:], ALU.add, rB, rB)
                self.tt("dve", slA[:], slA[:], ecb[:].unsqueeze(1).to_broadcast([128, NT, 32]), ALU.add, rB + [kB], rB)
                self.tt("dve", emA[:], slA[:], o1A[:], ALU.mult, rB, rB)
                rsum(d1, emA[:])
                self.tt("dve", emA[:], slA[:], o2A[:], ALU.mult, rB, rB)
                rsum(d2, emA[:])
                self.cp("dve", didx[:, 0:NT, 0], d1, rB, [rtB])
                self.cp("dve", didx[:, 0:NT, 1], d2, rB + [rtB], [rtB])
                for t in range(NT):
                    for k in range(2):
                        self.P.indirect("scatter", xs, didx[:, t, k:k + 1], xall[:, t, :], NSLOT - 1, reads=[xallB[t], rtB])
                P.barrier()
            with ExitStack() as p2:
                wg = [self.sb(p2, "wg%d" % i, [128, 8, DEXP], BF16) for i in range(2)]
                wu = [self.sb(p2, "wu%d" % i, [128, 8, DEXP], BF16) for i in range(2)]
                wd = [self.sb(p2, "wd%d" % i, [128, 4, D], BF16) for i in range(2)]
                wB = [Buf() for _ in range(2)]
                xe = [self.sb(p2, "xe%d" % i, [128, C // 128, D], BF16) for i in range(2)]
                xeB = [Buf() for _ in range(2)]
                XeT = [self.sb(p2, "XeT%d" % i, [128, 8, C], BF16) for i in range(2)]
                XeTB = [Buf() for _ in range(2)]
                sgb = [self.sb(p2, "msg%d" % i, [128, C], BF16) for i in range(2)]
                sgB = [Buf() for _ in range(2)]
                hdT = self.sb(p2, "hdT", [128, 4, C], BF16)
                hdB = [Buf() for _ in range(4)]
                yo = [self.sb(p2, "yo%d" % i, [128, D]) for i in range(2)]
                yoB = [Buf() for _ in range(2)]
                ysB = Buf()

                def load_expert(e):
                    i = e % 2
                    self.load_w(wg[i], wB[i], d["wg_%d" % l][e])
                    self.load_w(wu[i], wB[i], d["wu_%d" % l][e])
                    self.load_w(wd[i], wB[i], d["wd_%d" % l][e])
                    P.dma("sp", xe[i][:], xs[e * C:(e + 1) * C, :].rearrange("(s p) f -> p s f", p=128), writes=[xeB[i]])

                load_expert(0)
                bi = 0
                yi = 0
                for e in range(NEXP):
                    if e + 1 < NEXP:
                        load_expert(e + 1)
                    i = e % 2
                    for fc in range(8):
                        bk, bB = self.bank[fc % 2], self.bankB[fc % 2]
                        bkb = bk[:].bitcast(BF16)
                        for st in range(C // 128):
                            self.tr(bkb[:, st * 128:(st + 1) * 128], xe[i][:, st, fc * 128:(fc + 1) * 128], self.ident_b[:], [xeB[i], self.cB], [bB],
                                    signal=(st == C // 128 - 1))
                        self.cp("act" if fc % 2 == 0 else "dve", XeT[i][:, fc, :], bkb[:, 0:C], [bB], [XeTB[i]])
                    for fcb in range(4):
                        G, GB = self.bank[2 + 2 * (fcb % 2)], self.bankB[2 + 2 * (fcb % 2)]
                        U, UB = self.bank[3 + 2 * (fcb % 2)], self.bankB[3 + 2 * (fcb % 2)]
                        for k in range(8):
                            self.mm(G[:, 0:C], wg[i][:, k, fcb * 128:(fcb + 1) * 128], XeT[i][:, k, :], k == 0, k == 7, [wB[i], XeTB[i]], [GB])
                        for k in range(8):
                            self.mm(U[:, 0:C], wu[i][:, k, fcb * 128:(fcb + 1) * 128], XeT[i][:, k, :], k == 0, k == 7, [wB[i], XeTB[i]], [UB])
                        sg_, sgB_ = sgb[fcb % 2], sgB[fcb % 2]
                        self.act(sg_[:], G[:, 0:C], AF.Silu, [GB], [sgB_])
                        self.tt("dve", hdT[:, fcb, :], U[:, 0:C], sg_[:], ALU.mult, [UB, sgB_], [hdB[fcb]])
                    for st in range(C // 128):
                        y_, yB_ = yo[yi % 2], yoB[yi % 2]
                        yi += 1
                        for half in range(2):
                            Y, YB = self.bank[6 + bi % 2], self.bankB[6 + bi % 2]
                            bi += 1
                            for fcb in range(4):
                                self.mm(Y[:, :], hdT[:, fcb, st * 128:(st + 1) * 128], wd[i][:, fcb, half * 512:(half + 1) * 512],
                                        fcb == 0, fcb == 3, [hdB[fcb], wB[i]], [YB])
                            self.cp("act" if half == 0 else "dve", y_[:, half * 512:(half + 1) * 512], Y[:, :], [YB], [yB_])
                        P.dma("sp", ys[e * C + st * 128:e * C + (st + 1) * 128, :], y_[:], reads=[yB_], writes=[ysB])
                P.barrier()
            with ExitStack() as p3:
                gbc = self.sb(p3, "gbc2", [128, D])
                gB = Buf()
                NG = 4
                ht = [self.sb(p3, "mht%d" % i, [128, D]) for i in range(NG)]
                htB = [Buf() for _ in range(NG)]
                g1 = [self.sb(p3, "g1_%d" % i, [128, D]) for i in range(NG)]
                g2 = [self.sb(p3, "g2_%d" % i, [128, D]) for i in range(NG)]
                gtB = [Buf() for _ in range(NG)]
                curv = -1
                for t in range(ntile):
                    v = self.variant(t)
                    if v != curv:
                        self.gate_bcast(p3, l, 40, v, gbc, gB)
                        curv = v
                    i = t % NG
                    P.dma("sp", ht[i][:], self.hbuf[t * 128:(t + 1) * 128, :], reads=[self.hB[t]], writes=[htB[i]])
                    self.memset("pool", g1[i][:], 0.0, [gtB[i]])
                    self.memset("pool", g2[i][:], 0.0, [gtB[i]])
                    self.P.indirect("gather", ys, didx[:, t, 0:1], g1[i][:], NSLOT - 1, reads=[rtB], writes=[gtB[i]])
                    self.P.indirect("gather", ys, didx[:, t, 1:2], g2[i][:], NSLOT - 1, reads=[rtB], writes=[gtB[i]])
                    self.ts("dve", g1[i][:], g1[i][:], wts2[:, t, 0:1], None, ALU.mult, None, [gtB[i], rtB], [gtB[i]])
                    self.stt("dve", g1[i][:], g2[i][:], wts2[:, t, 1:2], g1[i][:], ALU.mult, ALU.add, [gtB[i], rtB], [gtB[i]])
                    self.tt("pool", g1[i][:], g1[i][:], gbc[:], ALU.mult, [gtB[i], gB], [gtB[i]])
                    self.tt("dve", g1[i][:], g1[i][:], ht[i][:], ALU.add, [gtB[i], htB[i]], [gtB[i]])
                    P.dma("sp", self.hbuf[t * 128:(t + 1) * 128, :], g1[i][:], reads=[gtB[i], htB[i]], writes=[self.hB[t]])
                P.barrier()

    def phase_final(self, out):
        P, d = self.P, self.d
        with ExitStack() as ph:
            gbc = self.sb(ph, "gfin", [128, D])
            gB = Buf()
            P.dma("sp", gbc[:], d["gfinal"].partition_broadcast(128), writes=[gB])
            ht = [self.sb(ph, "fht%d" % i, [128, D]) for i in range(2)]
            htB = [Buf() for _ in range(2)]
            junk = self.sb(ph, "fjunk", [128, D])
            jB = Buf()
            ss = self.sb(ph, "fss", [128, 16])
            ssB = Buf()
            oB = Buf("out")
            self.memset("dve", ss[:], 0.0, [ssB])
            for t in range(NLAT // 128):
                i = t % 2
                P.dma("sp", ht[i][:], self.hbuf[t * 128:(t + 1) * 128, :], reads=[self.hB[t]], writes=[htB[i]])
                self.act(junk[:], ht[i][:], AF.Square, [htB[i], ssB], [jB, ssB], accum_out=ss[:, t:t + 1])
            self.rsqrt_inplace(ss[:, 0:NLAT // 128], 1.0 / D, EPS, ssB)
            for t in range(NLAT // 128):
                i = t % 2
                P.dma("sp", ht[i][:], self.hbuf[t * 128:(t + 1) * 128, :], reads=[self.hB[t]], writes=[htB[i]])
                self.stt("dve", ht[i][:], ht[i][:], ss[:, t:t + 1], gbc[:], ALU.mult, ALU.mult, [htB[i], ssB, gB], [htB[i]])
                P.dma("sp", out[t * 128:(t + 1) * 128, :], ht[i][:], reads=[htB[i]], writes=[oB])
            P.barrier()

    def build(self):
        nc = self.nc
        L = self.launch
        with ExitStack() as es:
            self.P = Prog(nc, es)
            layers = {"A": [0], "B": [0, 1], "C": [1], "F": [0, 1]}[L]
            self.need = {"A": set(), "B": {(0, "full")}, "C": {(1, "full")}, "F": {(0, "full"), (1, "full")}}[L]
            self.setup(es, layers)
            self.alloc_kv_scratch()
            P = self.P
            h_in = self.din("h_in", [NTOK, D])
            for t in range(NTOK // 128):
                P.dma("sp", self.hbuf[t * 128:(t + 1) * 128, :], h_in[t * 128:(t + 1) * 128, :], writes=[self.hB[t]])
            stop = self.dbg.get("stop")
            if L == "A":
                send = self.dout("send", [SEND_ROWS, TL], BF16)
                self.phase_cond(0)
                self.phase_norm(0, 1, NTOK)
                self.phase_kv(0, send, Buf("send"))
            elif L in ("B", "C"):
                l = 0 if L == "B" else 1
                last = l == 1
                gath = self.din("gath", [NCORES * SEND_ROWS, TL], BF16)
                gathB = Buf("gath")
                dummy = self.dscr("send_dummy", [SEND_ROWS, TL], BF16)
                self.prep_nabias(l)
                self.phase_cond(l)
                self.phase_norm(l, 1, NTOK)
                self.phase_kv(l, dummy, Buf("dummy"))
                if stop != "kv":
                    self.phase_mix(l, last, gath, gathB)
                if stop not in ("kv", "mix"):
                    self.phase_moe(l, last)
                if L == "B":
                    if stop is None:
                        send = self.dout("send", [SEND_ROWS, TL], BF16)
                        self.phase_cond(1)
                        self.phase_norm(1, 1, NTOK)
                        self.phase_kv(1, send, Buf("send"))
                    h_out = self.dout("h_out", [NTOK, D])
                    for t in range(NTOK // 128):
                        P.dma("sp", h_out[t * 128:(t + 1) * 128, :], self.hbuf[t * 128:(t + 1) * 128, :], reads=[self.hB[t]])
                else:
                    self.d["gfinal"] = self.din("gfinal", [1, D])
                    out = self.dout("out", [NLAT, D])
                    self.phase_final(out)
            elif L == "F":
                self.d["gfinal"] = self.din("gfinal", [1, D])
                out = self.dout("out", [NLAT, D])
                for l in (0, 1):
                    last = l == 1
                    send = self.dscr("send%d" % l, [SEND_ROWS, TL], BF16)
                    gath = self.dscr("gath%d" % l, [NCORES * SEND_ROWS, TL], BF16)
                    sendB, gathB = Buf("send"), Buf("gath")
                    self.prep_nabias(l)
                    self.phase_cond(l)
                    self.phase_norm(l, 1, NTOK)
                    self.phase_kv(l, send, sendB)
                    P.collective(send, gath, [sendB], [gathB])
                    self.phase_mix(l, last, gath, gathB)
                    self.phase_moe(l, last)
                self.phase_final(out)
            P.finish()
        return nc


def _tlayout(v):
    v = np.asarray(v, np.float32)
    return np.ascontiguousarray(v.reshape(-1, 128).T)


def _rope_tables(core):
    t = np.arange(TL) + core * TL
    row = (t // GRID_W).astype(np.float32)
    col = (t % GRID_W).astype(np.float32)
    inv = (10000.0 ** (-np.arange(16, dtype=np.float32) / 16)).astype(np.float32)
    ar = row[:, None] * inv
    ac = col[:, None] * inv
    ang = np.concatenate([ar, ar, ac, ac], axis=-1)
    cos = np.cos(ang).astype(np.float32)
    sin = np.sin(ang).astype(np.float32)
    sgn = np.concatenate([-np.ones(16), np.ones(16), -np.ones(16), np.ones(16)]).astype(np.float32)
    cosT = np.concatenate([cos.T, cos.T], axis=0)
    sinT = np.concatenate([(sin * sgn).T, (sin * sgn).T], axis=0)
    return np.ascontiguousarray(cosT), np.ascontiguousarray(sinT)


def _perm_matrix():
    pm = np.zeros((128, 128), np.float32)
    for m in range(128):
        base, dd = (m // 64) * 64, m % 64
        seg, off = dd // 16, dd % 16
        partner = base + (seg ^ 1) * 16 + off
        pm[partner, m] = 1.0
    return pm


def _common_inputs(inp, core):
    c, c_ctx = inp["c"], inp["c_ctx"]
    condT = np.stack([_tlayout(c[0]), _tlayout(c[1]), _tlayout(c_ctx)], axis=-1)
    cosT, sinT = _rope_tables(core)
    selw = np.zeros((128, 16), np.float32)
    if core > 0:
        selw[:, core - 1] = 1.0
    if core < NCORES - 1:
        selw[:, 8 + core + 1] = 1.0
    return {"condT": np.ascontiguousarray(condT), "ident": np.eye(128, dtype=np.float32), "pm": _perm_matrix(),
            "ropec": cosT, "ropes": sinT, "selw": selw,
            "tri": np.triu(np.ones((128, 128), np.float32), 1),
            "ecb": np.ascontiguousarray(np.broadcast_to(np.arange(NEXP, dtype=np.float32) * MOE_CAP, (128, NEXP)))}


def _nabias(rpb, core):
    g = np.arange(2)[:, None, None, None]
    i = np.arange(8)[None, :, None, None]
    a = np.arange(2)[None, None, :, None]
    jq = np.arange(8)[None, None, None, :]
    kr = 16 * core + 8 * g - 4 + 2 * i + a
    qr = 16 * core + 8 * g + jq
    rs = np.clip(qr - 4, 0, 128 - 8)
    vr = (kr >= 0) & (kr < 128) & (kr >= rs) & (kr < rs + 8)
    dri = np.clip(kr - qr + 7, 0, 14)
    kc = np.arange(64)[:, None]
    qc = np.arange(64)[None, :]
    cs = np.clip(qc - 8, 0, 64 - 16)
    vc = (kc >= cs) & (kc < cs + 16)
    dci = np.clip(kc - qc + 15, 0, 30)
    vals = rpb[:, dri[..., None, None], dci[None, None, None, None]]
    ok = vr[..., None, None] & vc[None, None, None, None]
    vals = np.where(ok[None], vals, np.float32(-1e30)).astype(np.float32)
    vals = vals.transpose(1, 0, 2, 3, 5, 4, 6)
    return np.ascontiguousarray(vals.reshape(2, 8, 8, 128, 512))


def _layer_inputs(inp, l, core, full=False):
    out = {
        "w_ada_%d" % l: inp["w_ada"][l], "b_adaT_%d" % l: _tlayout(inp["b_ada"][l]),
        "gmixT_%d" % l: _tlayout(inp["g_norm_mix"][l]), "gffnT_%d" % l: _tlayout(inp["g_norm_ffn"][l]),
        "w_in_%d" % l: inp["w_in"][l],
    }
    if full:
        out.update({
            "dalam_%d" % l: inp["da_lambda"][l].reshape(1, 256), "dasub_%d" % l: inp["da_subln_g"][l].reshape(128, 1),
            "sglng_%d" % l: inp["sg_ln_g"][l].reshape(1, 512), "sglnb_%d" % l: inp["sg_ln_b"][l].reshape(1, 512),
            "sgwT_%d" % l: np.ascontiguousarray(inp["sg_w"][l].transpose(0, 2, 1)), "sgb_%d" % l: inp["sg_b"][l].reshape(1, 512),
            "nabias_%d" % l: _nabias(inp["na_rpb"][l], core),
            "w_branch_%d" % l: inp["w_branch"][l], "w_out_%d" % l: inp["w_out"][l],
            "wr_%d" % l: np.ascontiguousarray(np.concatenate([inp["moe_w_group"][l], inp["moe_w_router"][l]], axis=1)),
            "br_%d" % l: np.concatenate([inp["moe_b_group"][l], inp["moe_b_router"][l]]).reshape(1, 36),
            "wg_%d" % l: inp["moe_w_gate"][l], "wu_%d" % l: inp["moe_w_up"][l], "wd_%d" % l: inp["moe_w_down"][l],
        })
    return out


def _h0(inp, core):
    x, ctx = inp["x"], inp["ctx"]
    return np.ascontiguousarray(np.concatenate([x[0, core * TL:(core + 1) * TL], x[1, core * TL:(core + 1) * TL], ctx[0], ctx[1]],
                                               axis=0).astype(np.float32))


def _run(kb, maps):
    nc = kb.build()
    in_maps = [{k: np.ascontiguousarray(m[k]) for k in kb.in_names} for m in maps]
    res = run_bass_kernel_spmd(nc, in_maps, core_ids=list(range(NCORES)))
    return res.results


FUSED = True


def _assemble(res):
    out = np.empty((2, SEQ, D), np.float32)
    for c in range(NCORES):
        o = np.asarray(res[c]["out"], np.float32)
        out[0, c * TL:(c + 1) * TL] = o[0:TL]
        out[1, c * TL:(c + 1) * TL] = o[TL:2 * TL]
    return out


def kernel(**inputs):
    inp = {k: np.asarray(v) for k, v in inputs.items()}
    common = [_common_inputs(inp, c) for c in range(NCORES)]
    if FUSED:
        kb = KB("F")
        maps = []
        for c in range(NCORES):
            m = dict(common[c])
            m.update(_layer_inputs(inp, 0, c, full=True))
            m.update(_layer_inputs(inp, 1, c, full=True))
            m["h_in"] = _h0(inp, c)
            m["gfinal"] = inp["g_final"].reshape(1, D)
            maps.append(m)
        return _assemble(_run(kb, maps))
    kbA = KB("A")
    maps = []
    for c in range(NCORES):
        m = dict(common[c])
        m.update(_layer_inputs(inp, 0, c))
        m["h_in"] = _h0(inp, c)
        maps.append(m)
    resA = _run(kbA, maps)
    gath0 = np.concatenate([resA[c]["send"] for c in range(NCORES)], axis=0)
    kbB = KB("B")
    for c in range(NCORES):
        maps[c].update(_layer_inputs(inp, 0, c, full=True))
        maps[c].update(_layer_inputs(inp, 1, c))
        maps[c]["gath"] = gath0
    resB = _run(kbB, maps)
    gath1 = np.concatenate([resB[c]["send"] for c in range(NCORES)], axis=0)
    kbC = KB("C")
    maps2 = []
    for c in range(NCORES):
        m = dict(common[c])
        m.update(_layer_inputs(inp, 1, c, full=True))
        m["h_in"] = resB[c]["h_out"]
        m["gath"] = gath1
        m["gfinal"] = inp["g_final"].reshape(1, D)
        maps2.append(m)
    return _assemble(_run(kbC, maps2))
```
